# Optimizing a Trainium2 kernel written in Bass

```python
import jax, jax.numpy as jnp
from jax import lax
import numpy as np

D_MODEL = 1024
BATCH = 4
SEQ = 8192
DEPTH = 2

HEAD_DIM = 64
GLA_HEADS = 4
GDN_HEADS = 4
MOBA_HEADS = 8
GLA_W = GLA_HEADS * HEAD_DIM
GDN_W = GDN_HEADS * HEAD_DIM
MOBA_W = MOBA_HEADS * HEAD_DIM
D_MIX = GLA_W + GDN_W + MOBA_W
GLA_GATE_RANK = 16
GLA_GATE_TAU = 16.0
CHUNK = 64
CONV_WIDTH = 4
MOBA_BLOCK = 256
MOBA_TOPK = 3
MOBA_QCHUNK = 32
ROPE_THETA = 10000.0
D_FF = -(-(8 * D_MODEL) // (3 * 256)) * 256
RMS_EPS = 1e-6
IN_SPLITS = (GLA_W, GLA_W, GLA_W, GLA_W, GLA_GATE_RANK,
             GDN_W, GDN_W, GDN_W, GDN_W, GDN_HEADS, GDN_HEADS,
             MOBA_W, MOBA_W, MOBA_W)
N_IN = sum(IN_SPLITS)

kernel_name = "hybrid_gla_gdn_moba_parallel_heads"

F32 = jnp.float32


def rms_norm(x, w):
    xf = x.astype(F32)
    y = xf * lax.rsqrt(jnp.mean(xf * xf, axis=-1, keepdims=True) + RMS_EPS)
    return (y * w.astype(F32)).astype(x.dtype)


def l2_norm(t):
    return t * lax.rsqrt(jnp.sum(t * t, axis=-1, keepdims=True) + RMS_EPS)


def rope(t, pos):
    half = HEAD_DIM // 2
    inv = ROPE_THETA ** (-jnp.arange(half, dtype=F32) / half)
    ang = pos.astype(F32)[:, None] * inv[None, :]
    cos = jnp.cos(ang)[:, None, :]
    sin = jnp.sin(ang)[:, None, :]
    tf = t.astype(F32)
    t1, t2 = tf[..., :half], tf[..., half:]
    return jnp.concatenate([t1 * cos - t2 * sin, t2 * cos + t1 * sin], -1).astype(t.dtype)


def causal_depthwise_conv(x, w):
    c = x.shape[-1]
    return lax.conv_general_dilated(
        x, w.astype(x.dtype)[:, None, :], window_strides=(1,),
        padding=[(CONV_WIDTH - 1, 0)], dimension_numbers=("NWC", "WIO", "NWC"),
        feature_group_count=c)


def gla_chunked(q, k, v, log_a):
    b_, h_, s_, dk = q.shape
    dv = v.shape[-1]
    n = s_ // CHUNK
    to_chunks = lambda t: jnp.moveaxis(t.reshape(b_, h_, n, CHUNK, t.shape[-1]), 2, 0)
    incl = jnp.tril(jnp.ones((CHUNK, CHUNK), bool))

    def step(state, inp):
        qc, kc, vc, gc = inp
        cum = jnp.cumsum(gc, axis=-2)
        o_inter = jnp.einsum('bhcd,bhde->bhce', qc * jnp.exp(cum), state)
        diff = cum[:, :, :, None, :] - cum[:, :, None, :, :]
        decay = jnp.exp(jnp.where(incl[:, :, None], diff, -jnp.inf))
        attn = jnp.einsum('bhid,bhjd,bhijd->bhij', qc, kc, decay)
        o = o_inter + jnp.einsum('bhij,bhje->bhie', attn, vc)
        last = cum[:, :, -1:, :]
        state = jnp.exp(last[:, :, 0, :])[..., None] * state + jnp.einsum(
            'bhcd,bhce->bhde', kc * jnp.exp(last - cum), vc)
        return state, o

    s0 = jnp.zeros((b_, h_, dk, dv), F32)
    _, o = lax.scan(step, s0, (to_chunks(q), to_chunks(k), to_chunks(v), to_chunks(log_a)))
    return jnp.moveaxis(o, 0, 2).reshape(b_, h_, s_, dv)


def gdn_chunked(q, k, v, g, beta):
    b_, h_, s_, dk = q.shape
    dv = v.shape[-1]
    n = s_ // CHUNK
    c = lambda t: t.reshape((b_, h_, n, CHUNK) + t.shape[3:])
    q, k, v, g, beta = c(q), c(k), c(v), c(g), c(beta)
    g = jnp.cumsum(g, axis=-1)
    incl = jnp.tril(jnp.ones((CHUNK, CHUNK), bool))
    strict = jnp.tril(jnp.ones((CHUNK, CHUNK), bool), -1)
    decay = jnp.exp(jnp.where(incl, g[..., :, None] - g[..., None, :], -jnp.inf))
    k_beta = k * beta[..., None]
    v_beta = v * beta[..., None]
    lower = jnp.where(strict, jnp.einsum('bhnid,bhnjd->bhnij', k_beta, k) * decay, 0.0)
    eye = jnp.eye(CHUNK, dtype=F32)
    rhs = jnp.concatenate([v_beta, k_beta * jnp.exp(g)[..., None]], axis=-1)
    sol = lax.linalg.triangular_solve(eye + lower, rhs, left_side=True, lower=True,
                                      unit_diagonal=True)
    u, w = sol[..., :dv], sol[..., dv:]
    attn = jnp.where(incl, jnp.einsum('bhnid,bhnjd->bhnij', q, k) * decay, 0.0)

    def step(state, inp):
        qi, ki, ui, wi, ai, gi = inp
        v_new = ui - jnp.einsum('bhck,bhkv->bhcv', wi, state)
        o = jnp.einsum('bhck,bhkv->bhcv', qi * jnp.exp(gi)[..., None], state) + \
            jnp.einsum('bhij,bhjv->bhiv', ai, v_new)
        g_last = gi[..., -1:]
        state = state * jnp.exp(g_last)[..., None] + jnp.einsum(
            'bhck,bhcv->bhkv', ki * jnp.exp(g_last - gi)[..., None], v_new)
        return state, o

    xs = tuple(jnp.moveaxis(t, 2, 0) for t in (q, k, u, w, attn, g))
    _, o = lax.scan(step, jnp.zeros((b_, h_, dk, dv), F32), xs)
    return jnp.moveaxis(o, 0, 2).reshape(b_, h_, s_, dv)


def gla_mixer(q, k, v, z, a_low, w_gate, b_gate, norm_w):
    b_, s_, _ = q.shape
    heads = lambda t: t.astype(F32).reshape(b_, s_, GLA_HEADS, HEAD_DIM).transpose(0, 2, 1, 3)
    log_a = jax.nn.log_sigmoid((a_low @ w_gate + b_gate).astype(F32)) / GLA_GATE_TAU
    o = gla_chunked(heads(q) * HEAD_DIM ** -0.5, heads(k), heads(v), heads(log_a))
    o = rms_norm(o.transpose(0, 2, 1, 3), norm_w).reshape(b_, s_, GLA_W)
    return (o * jax.nn.silu(z.astype(F32))).astype(q.dtype)


def gdn_mixer(q, k, v, z, b_raw, a_raw, conv_w, a_log, dt_bias, norm_w):
    b_, s_, _ = q.shape
    qkv = jax.nn.silu(causal_depthwise_conv(jnp.concatenate([q, k, v], -1), conv_w))
    q, k, v = jnp.split(qkv.astype(F32), 3, axis=-1)
    heads = lambda t: t.reshape(b_, s_, GDN_HEADS, HEAD_DIM).transpose(0, 2, 1, 3)
    q = l2_norm(heads(q)) * HEAD_DIM ** -0.5
    k = l2_norm(heads(k))
    beta = jax.nn.sigmoid(b_raw.astype(F32)).transpose(0, 2, 1)
    g = (-jnp.exp(a_log.astype(F32)) *
         jax.nn.softplus(a_raw.astype(F32) + dt_bias.astype(F32))).transpose(0, 2, 1)
    o = gdn_chunked(q, k, heads(v), g, beta)
    o = rms_norm(o.transpose(0, 2, 1, 3), norm_w).reshape(b_, s_, GDN_W)
    return (o * jax.nn.silu(z.astype(F32))).astype(z.dtype)


def moba_mixer(q, k, v):
    b_, s_, _ = q.shape
    pos = jnp.arange(s_)
    heads = lambda t: t.reshape(b_, s_, MOBA_HEADS, HEAD_DIM)
    q = rope(heads(q), pos).transpose(0, 2, 1, 3)
    k = rope(heads(k), pos).transpose(0, 2, 1, 3)
    v = heads(v).transpose(0, 2, 1, 3)
    sp = -(-s_ // MOBA_BLOCK) * MOBA_BLOCK
    padw = ((0, 0), (0, 0), (0, sp - s_), (0, 0))
    q, k, v = jnp.pad(q, padw), jnp.pad(k, padw), jnp.pad(v, padw)
    nb = sp // MOBA_BLOCK
    nq = sp // MOBA_QCHUNK
    topk = min(MOBA_TOPK, nb)
    kb = k.reshape(b_, MOBA_HEADS, nb, MOBA_BLOCK, HEAD_DIM)
    vb = v.reshape(b_, MOBA_HEADS, nb, MOBA_BLOCK, HEAD_DIM)
    k_mean = jnp.mean(kb.astype(F32), axis=-2)
    qch = jnp.moveaxis(q.reshape(b_, MOBA_HEADS, nq, MOBA_QCHUNK, HEAD_DIM), 2, 0)
    bi = jnp.arange(b_)[:, None, None, None]
    hi = jnp.arange(MOBA_HEADS)[None, :, None, None]
    scale = HEAD_DIM ** -0.5

    def one_chunk(args):
        qc, ci = args
        q_pos = ci * MOBA_QCHUNK + jnp.arange(MOBA_QCHUNK)
        own = (ci * MOBA_QCHUNK) // MOBA_BLOCK
        gate = jnp.einsum('bhqd,bhnd->bhqn', qc.astype(F32), k_mean)
        gate = jnp.where(jnp.arange(nb) < own, gate, -jnp.inf)
        _, idx = lax.top_k(gate, topk)
        valid = idx < own
        k_sel = kb[bi, hi, idx]
        v_sel = vb[bi, hi, idx]
        s_sel = jnp.einsum('bhqd,bhqnkd->bhqnk', qc, k_sel).astype(F32) * scale
        s_sel = jnp.where(valid[..., None], s_sel, -jnp.inf).reshape(
            b_, MOBA_HEADS, MOBA_QCHUNK, topk * MOBA_BLOCK)
        k_own = lax.dynamic_index_in_dim(kb, own, axis=2, keepdims=False)
        v_own = lax.dynamic_index_in_dim(vb, own, axis=2, keepdims=False)
        s_own = jnp.einsum('bhqd,bhkd->bhqk', qc, k_own).astype(F32) * scale
        key_pos = own * MOBA_BLOCK + jnp.arange(MOBA_BLOCK)
        s_own = jnp.where(key_pos[None, :] <= q_pos[:, None], s_own, -jnp.inf)
        p = jax.nn.softmax(jnp.concatenate([s_sel, s_own], axis=-1), axis=-1)
        p_sel = p[..., :topk * MOBA_BLOCK].reshape(
            b_, MOBA_HEADS, MOBA_QCHUNK, topk, MOBA_BLOCK).astype(v.dtype)
        p_own = p[..., topk * MOBA_BLOCK:].astype(v.dtype)
        return (jnp.einsum('bhqnk,bhqnkd->bhqd', p_sel, v_sel) +
                jnp.einsum('bhqk,bhkd->bhqd', p_own, v_own))

    o = lax.map(one_chunk, (qch, jnp.arange(nq)))
    o = jnp.moveaxis(o, 0, 2).reshape(b_, MOBA_HEADS, sp, HEAD_DIM)[:, :, :s_]
    return o.transpose(0, 2, 1, 3).reshape(b_, s_, MOBA_W)


def setup_inputs(seed: int = 0) -> dict:
    key = jax.random.key(seed)
    ks = jax.random.split(key, 18)
    nrm = lambda kk, shape: jax.random.normal(kk, shape, F32)
    gain = lambda kk, shape: 1.0 + 0.05 * nrm(kk, shape)
    dt = jnp.exp(jax.random.uniform(ks[12], (DEPTH, GDN_HEADS), F32,
                                    np.log(1e-3), np.log(1e-1)))
    return {
        "x": nrm(ks[0], (BATCH, SEQ, D_MODEL)),
        "norm_mix_pre": gain(ks[1], (DEPTH, D_MODEL)),
        "norm_mix_post": gain(ks[2], (DEPTH, D_MODEL)),
        "norm_ffn_pre": gain(ks[3], (DEPTH, D_MODEL)),
        "norm_ffn_post": gain(ks[4], (DEPTH, D_MODEL)),
        "w_in": nrm(ks[5], (DEPTH, D_MODEL, N_IN)) * D_MODEL ** -0.5,
        "w_o": nrm(ks[6], (DEPTH, D_MIX, D_MODEL)) * D_MIX ** -0.5,
        "gla_w_gate": nrm(ks[7], (DEPTH, GLA_GATE_RANK, GLA_W)) * GLA_GATE_RANK ** -0.5,
        "gla_b_gate": 0.1 * nrm(ks[8], (DEPTH, GLA_W)),
        "gla_norm": gain(ks[9], (DEPTH, HEAD_DIM)),
        "gdn_conv": nrm(ks[10], (DEPTH, CONV_WIDTH, 3 * GDN_W)) * CONV_WIDTH ** -0.5,
        "gdn_a_log": jnp.log(jax.random.uniform(ks[11], (DEPTH, GDN_HEADS), F32, 1.0, 16.0)),
        "gdn_dt_bias": dt + jnp.log(-jnp.expm1(-dt)),
        "gdn_norm": gain(ks[13], (DEPTH, HEAD_DIM)),
        "ffn_w_gate": nrm(ks[14], (DEPTH, D_MODEL, D_FF)) * D_MODEL ** -0.5,
        "ffn_w_up": nrm(ks[15], (DEPTH, D_MODEL, D_FF)) * D_MODEL ** -0.5,
        "ffn_w_down": nrm(ks[16], (DEPTH, D_FF, D_MODEL)) * D_FF ** -0.5,
    }


def reference(x, norm_mix_pre, norm_mix_post, norm_ffn_pre, norm_ffn_post, w_in, w_o,
              gla_w_gate, gla_b_gate, gla_norm, gdn_conv, gdn_a_log, gdn_dt_bias, gdn_norm,
              ffn_w_gate, ffn_w_up, ffn_w_down):
    split_points = [int(p) for p in np.cumsum(IN_SPLITS)[:-1]]
    for l in range(DEPTH):
        h = rms_norm(x, norm_mix_pre[l])
        proj = h @ w_in[l]
        (gq, gk, gv, gz, ga, dq, dk, dv, dz, db, da, mq, mk, mv) = jnp.split(
            proj, split_points, axis=-1)
        o_gla = gla_mixer(gq, gk, gv, gz, ga, gla_w_gate[l], gla_b_gate[l], gla_norm[l])
        o_gdn = gdn_mixer(dq, dk, dv, dz, db, da, gdn_conv[l], gdn_a_log[l],
                          gdn_dt_bias[l], gdn_norm[l])
        o_moba = moba_mixer(mq, mk, mv)
        mix = jnp.concatenate([o_gla, o_gdn, o_moba], axis=-1) @ w_o[l]
        x = x + rms_norm(mix, norm_mix_post[l])
        h = rms_norm(x, norm_ffn_pre[l])
        y = (jax.nn.silu(h @ ffn_w_gate[l]) * (h @ ffn_w_up[l])) @ ffn_w_down[l]
        x = x + rms_norm(y, norm_ffn_post[l])
    return x
```

```python
import numpy as np
import ml_dtypes
import concourse.bass as bass
import concourse.mybir as mybir
from concourse.bass_utils import run_bass_kernel_spmd
from contextlib import ExitStack

F32 = mybir.dt.float32
BF16 = mybir.dt.bfloat16
AF = mybir.ActivationFunctionType
ALU = mybir.AluOpType
AX = mybir.AxisListType
BF = ml_dtypes.bfloat16

D = 1024
DFF = 2816
NFC = DFF // 128
BIG = 30000.0
ENGS = ("pe", "act", "dve", "pool", "sp")


class _Op:
    __slots__ = ("eng", "fn", "deps", "dma", "chan", "idx", "sig", "waits", "need")

    def __init__(self, eng, fn, dma, chan, idx):
        self.eng, self.fn, self.dma, self.chan, self.idx = eng, fn, dma, chan, idx
        self.deps = ()
        self.sig = None
        self.need = False
        self.waits = ()


class Sched:
    def __init__(self):
        self.ops = []
        self.lastw = {}
        self.readers = {}
        self.last_on_eng = {}
        self.last_on_chan = {}

    @staticmethod
    def _norm(keys):
        return [k[:3] if (k[:2] in ("pf", "pb") and len(k) > 2 and k[2].isdigit()) else k for k in keys]

    def add(self, eng, fn, reads=(), writes=(), dma=False, chan=None):
        reads = self._norm(reads)
        writes = self._norm(writes)
        op = _Op(eng, fn, dma, chan if dma else None, len(self.ops))
        deps = set()
        for k in reads:
            w = self.lastw.get(k)
            if w is not None:
                deps.add(w)
        for k in writes:
            w = self.lastw.get(k)
            if w is not None:
                deps.add(w)
            for r in self.readers.get(k, ()):
                deps.add(r)
        op.deps = deps
        for k in reads:
            self.readers.setdefault(k, []).append(op.idx)
        for k in writes:
            self.lastw[k] = op.idx
            self.readers[k] = []
        self.ops.append(op)
        if dma:
            self.last_on_chan[chan] = op.idx
        else:
            self.last_on_eng[eng] = op.idx
        return op

    def barrier(self):
        allprev = set(self.last_on_eng.values()) | set(self.last_on_chan.values())
        for e in ENGS:
            op = _Op(e, None, False, None, len(self.ops))
            op.deps = set(allprev)
            self.ops.append(op)
            self.last_on_eng[e] = op.idx
        self.lastw.clear()
        self.readers.clear()

    def emit(self):
        ops = self.ops
        for op in ops:
            for d in op.deps:
                ops[d].need = True
        cnt = {}
        for op in ops:
            if op.fn is None:
                continue
            if op.dma:
                key = ("chan", op.chan)
                cnt[key] = cnt.get(key, 0) + 16
                op.sig = (key, cnt[key])
            elif op.need:
                key = ("eng", op.eng)
                cnt[key] = cnt.get(key, 0) + 1
                op.sig = (key, cnt[key])
        water = {e: {} for e in ENGS}
        for op in ops:
            need = {}
            for d in op.deps:
                dop = ops[d]
                if dop.sig is None:
                    continue
                k, v = dop.sig
                if need.get(k, 0) < v:
                    need[k] = v
            wm = water[op.eng]
            waits = []
            for k, v in need.items():
                if wm.get(k, 0) < v:
                    wm[k] = v
                    waits.append((k, v))
            op.waits = waits
        return sorted(cnt.keys())

    def run(self, semkeys, sems, block):
        ops = self.ops
        semmap = dict(zip(semkeys, sems))

        def stream(engname):
            def body(e):
                for op in ops:
                    if op.eng != engname:
                        continue
                    for k, v in op.waits:
                        e.wait_ge(semmap[k], v)
                    if op.fn is None:
                        continue
                    ins = op.fn(e)
                    if op.sig is not None:
                        ins.then_inc(semmap[op.sig[0]], 16 if op.dma else 1)
            return body

        block.tensor(stream("pe"))
        block.scalar(stream("act"))
        block.vector(stream("dve"))
        block.gpsimd(stream("pool"))
        block.sync(stream("sp"))


def _win_cols():
    o = {}
    idx = []
    base = {"gq": 0, "gk": 256, "gv": 512, "gz": 768, "ga": 1024, "dq": 1040, "dk": 1296, "dv": 1552,
            "dz": 1808, "db": 2064, "da": 2068, "mq": 2072, "mk": 2584, "mv": 3096}

    def put(name, cols):
        o[name] = len(idx)
        idx.extend(cols)

    put("gq", range(base["gq"], base["gq"] + 256))
    put("gk", range(base["gk"], base["gk"] + 256))
    put("gv", range(base["gv"], base["gv"] + 256))
    put("ga", range(base["ga"], base["ga"] + 16))
    put("dq", range(base["dq"], base["dq"] + 256))
    put("dk", range(base["dk"], base["dk"] + 256))
    put("dv", range(base["dv"], base["dv"] + 256))
    put("dba", range(base["db"], base["db"] + 8))
    swap = lambda b: [b + h * 64 + (d + 32) % 64 for h in range(8) for d in range(64)]
    put("mq", range(base["mq"], base["mq"] + 512))
    put("mqs", swap(base["mq"]))
    put("mk", range(base["mk"], base["mk"] + 512))
    put("mks", swap(base["mk"]))
    put("z", list(range(base["gz"], base["gz"] + 256)) + list(range(base["dz"], base["dz"] + 256)))
    put("mv", range(base["mv"], base["mv"] + 512))
    return np.array(idx, dtype=np.int64), o


WIN_IDX, WOFF = _win_cols()
NCOL = len(WIN_IDX)


def build(S, depth, dbg=False):
    NT = S // 128
    NG = S // 512
    NB = S // 256
    nc = bass.Bass("TRN2", target_bir_lowering=False)
    sc_kind = "ExternalOutput" if dbg else "Internal"

    def din(name, shape, dt):
        return nc.dram_tensor(name, shape, dt, kind="ExternalInput").ap()

    def dscr(name, shape, dt):
        return nc.dram_tensor(name, shape, dt, kind=sc_kind).ap()

    x_in = din("x", [S, D], F32)
    win_d = din("win", [depth, D, NCOL], BF16)
    wga_d = din("wga", [depth, 33, 256], BF16)
    wo_d = din("wo", [depth, D, D], BF16)
    wg_d = din("wg", [depth, D, DFF], BF16)
    wu_d = din("wu", [depth, D, DFF], BF16)
    wd_d = din("wd", [depth, DFF, D], BF16)
    gains_d = din("gains", [depth, 4, D], F32)
    hn_d = din("hn", [depth, 2, 64], F32)
    conv_d = din("conv", [depth, 768, 4], F32)
    gsc_d = din("gsc", [depth, 2, 4], F32)
    c_identb = din("c_identb", [128, 128], BF16)
    c_identf = din("c_identf", [128, 128], F32)
    c_masku = din("c_masku", [128, 128], F32)
    c_negu = din("c_negu", [128, 128], F32)
    c_posl = din("c_posl", [128, 128], F32)
    c_triu = din("c_triu", [128, 128], F32)
    c_trisu = din("c_trisu", [128, 128], F32)
    c_bones = din("c_bones", [128, 128], F32)
    c_rope = din("c_rope", [4, 128, S], F32)
    c_onehot = din("c_onehot", [64, S], BF16)
    c_mb = din("c_mb", [128, 32 * 32], F32)
    c_cm = din("c_cm", [4, 128, 512], BF16)
    y_out = nc.dram_tensor("y", [S, D], F32, kind="ExternalOutput").ap()

    xs = dscr("xs", [S, D], F32)
    x1s = dscr("x1s", [S, D], F32)
    h2s = dscr("h2s", [S, D], BF16)
    glaT = dscr("glaT", [1024, S], F32)
    gdnT = dscr("gdnT", [768, S], BF16)
    gsct = dscr("gsct", [S, 24], F32)
    mqT = dscr("mqT", [512, S], BF16)
    mkT = dscr("mkT", [512, S], BF16)
    mksum = dscr("mksum", [512, 32], F32)
    mvs = dscr("mvs", [S, 512], BF16)
    zs = dscr("zs", [S, 512], F32)
    mix = dscr("mix", [S, D], BF16)

    S_ = Sched()
    uid = [0]
    ARENA_BASE = 16640
    ARENA_CAP = 226000
    arena = [ARENA_BASE]

    def sb(name, shape, dt):
        nbytes = int(np.prod(shape[1:])) * (4 if dt == F32 else 2)
        nbytes = (nbytes + 63) // 64 * 64
        off = arena[0]
        arena[0] += nbytes
        assert arena[0] <= ARENA_CAP, (name, arena[0])
        uid[0] += 1
        return nc.alloc_sbuf_tensor_at(f"{name}_{uid[0]}", shape, dt, offset=off)

    es = ExitStack()
    with es:
        PF = [es.enter_context(nc.psum_tensor(f"pf{i}", [128, 512], F32)) for i in range(6)]
        PB = [es.enter_context(nc.psum_tensor(f"pb{i}", [128, 1024], BF16)) for i in range(2)]

        def op(eng, meth, reads, writes, **kw):
            S_.add(eng, lambda e, m=meth, k=kw: getattr(e, m)(**k), reads=reads, writes=writes)

        def mmg(items, reads, writes):
            def f(e, items=items):
                n = len(items)
                for i, (o, l, r) in enumerate(items):
                    ins = e.matmul(out=o, lhsT=l, rhs=r, start=(i == 0), stop=(i == n - 1))
                return ins
            S_.add("pe", f, reads=reads, writes=writes)

        def trg(items, reads, writes):
            def f(e, items=items):
                for (o, i_, idn) in items:
                    ins = e.transpose(out=o, in_=i_, identity=idn)
                return ins
            S_.add("pe", f, reads=reads, writes=writes)

        def dma(q, out, in_, reads, writes, chan, slow=False):
            if slow:
                S_.add(q, lambda e, o=out, i=in_: e.dma_start(out=o, in_=i, allow_slow_non_contiguous=True),
                       reads=reads, writes=writes, dma=True, chan=chan)
            else:
                S_.add(q, lambda e, o=out, i=in_: e.dma_start(out=o, in_=i), reads=reads, writes=writes, dma=True, chan=chan)

        identb = sb("identb", [128, 128], BF16)
        identf = sb("identf", [128, 128], F32)
        epsc = sb("epsc", [128, 1], F32)
        onec = sb("onec", [128, 1], F32)
        dma("sp", identb[:], c_identb, [], ["identb"], "c0")
        dma("sp", identf[:], c_identf, [], ["identf"], "c1")
        op("dve", "memset", [], ["epsc"], ap=epsc[:], constant=1e-6)
        op("dve", "memset", [], ["onec"], ap=onec[:], constant=1.0)
        GLOBAL_TOP = arena[0]

        def rstd_from_ss(ss_ap, out_ap, n, rk, wk):
            op("act", "activation", rk + ["epsc"], wk, out=out_ap, in_=ss_ap, func=AF.Ln, scale=1.0 / n, bias=epsc[:ss_ap.shape[0], :])
            op("act", "activation", wk, wk, out=out_ap, in_=out_ap, func=AF.Exp, scale=-0.5)

        def phase_A(l, xsrc):
            arena[0] = GLOBAL_TOP
            win = sb("win", [128, 8, NCOL], BF16)
            wga = sb("wga", [33, 256], BF16)
            gpre = sb("gpre", [128, D], F32)
            convw = sb("convw", [128, 6, 4], F32)
            dtb = sb("dtb", [128, 4], F32)
            nA = sb("nA", [128, 4], F32)
            triu = sb("triu", [128, 128], F32)
            trisu = sb("trisu", [128, 128], F32)
            bones = sb("bones", [128, 128], F32)
            for c in range(8):
                dma("sp" if c % 2 == 0 else "pool", win[:, c, :], win_d[l, c * 128:(c + 1) * 128, :], [], [f"win{c}"], f"w{c}")
            WIN = [f"win{c}" for c in range(8)]
            dma("sp", wga[:], wga_d[l], [], ["wga"], "c2")
            dma("sp", gpre[:], gains_d[l, 0:1, :].broadcast_to([128, D]), [], ["gpre"], "c3")
            dma("sp", convw[:], conv_d[l].rearrange("(g p) k -> p g k", p=128), [], ["convw"], "c4")
            dma("sp", dtb[:], gsc_d[l, 1:2, :].broadcast_to([128, 4]), [], ["dtb"], "c5")
            dma("sp", nA[:], gsc_d[l, 0:1, :].broadcast_to([128, 4]), [], ["nA"], "c6")
            dma("sp", triu[:], c_triu, [], ["triu"], "c7")
            dma("sp", trisu[:], c_trisu, [], ["trisu"], "c8")
            dma("sp", bones[:], c_bones, [], ["bones"], "c9")
            op("act", "activation", ["nA"], ["nA"], out=nA[:], in_=nA[:], func=AF.Exp)
            op("dve", "tensor_scalar", ["nA"], ["nA"], out=nA[:], in0=nA[:], scalar1=-1.0, scalar2=None, op0=ALU.mult)

            xt = [sb(f"xt{i}", [128, D], F32) for i in range(2)]
            sq = sb("sq", [128, D], F32)
            hb = [sb(f"hb{i}", [128, D], BF16) for i in range(2)]
            ss = [sb(f"ss{i}", [128, 1], F32) for i in range(2)]
            rs = [sb(f"rs{i}", [128, 1], F32) for i in range(2)]
            hT = [sb(f"hT{i}", [128, 8, 512], BF16) for i in range(2)]
            gaT = sb("gaT", [33, 512], BF16)
            op("dve", "memset", [], ["gaT"], ap=gaT[:], constant=0.0)
            op("dve", "memset", ["gaT"], ["gaT"], ap=gaT[32:33, :], constant=1.0)
            stf = [sb(f"stf{i}", [128, 512], F32) for i in range(3)]
            stb = [sb(f"stb{i}", [128, 512], BF16) for i in range(3)]
            xc = [sb(f"xc{i}", [128, 3 + 512], F32) for i in range(6)]
            cy = [sb(f"cy{i}", [128, 512], F32) for i in range(2)]
            ce = [sb(f"ce{i}", [128, 512], F32) for i in range(2)]
            rope_t = [sb(f"rope{i}", [128, 512], F32) for i in range(4)]
            r1 = [sb(f"r1{i}", [128, 512], F32) for i in range(2)]
            r2 = [sb(f"r2{i}", [128, 512], F32) for i in range(2)]
            ksum = sb("ksum", [128, 4, 32], F32)
            sc = [sb(f"sc{i}", [128, 24], F32) for i in range(2)]
            sct = [sb(f"sct{i}", [128, 8], F32) for i in range(2)]
            op("pool", "memset", [], ["ksum"], ap=ksum[:], constant=0.0)
            for i in range(6):
                op("pool", "memset", [], [f"xc{i}"], ap=xc[i][:, 0:3], constant=0.0)
            nst = [0, 0]
            pfi = [0]

            def nextpf():
                pfi[0] = (pfi[0] + 1) % 4
                return pfi[0], PF[pfi[0]], f"pf{pfi[0]}"

            def load_norm(G):
                hTb = hT[G % 2]
                for tt in range(4):
                    t = G * 4 + tt
                    b = t % 2
                    dma("sp", xt[b][:], xsrc[t * 128:(t + 1) * 128, :], [], [f"xt{b}"], f"xt{b}")
                    op("act", "activation", [f"xt{b}"], ["sq", f"ss{b}"], out=sq[:], in_=xt[b][:], func=AF.Square, accum_out=ss[b][:])
                    rstd_from_ss(ss[b][:], rs[b][:], D, [f"ss{b}"], [f"rs{b}"])
                    op("dve", "scalar_tensor_tensor", [f"xt{b}", f"rs{b}", "gpre"], [f"hb{b}"], out=hb[b][:], in0=xt[b][:],
                       scalar=rs[b][:], in1=gpre[:], op0=ALU.mult, op1=ALU.mult)
                    for half in range(2):
                        trg([(PB[half][:, c * 128:(c + 1) * 128], hb[b][:, (half * 4 + c) * 128:(half * 4 + c + 1) * 128], identb[:])
                             for c in range(4)], [f"hb{b}", "identb"], [f"pb{half}"])
                        dst = hTb[:, half * 4:(half + 1) * 4, tt * 128:(tt + 1) * 128]
                        src = PB[half][:, 0:512].rearrange("p (c t) -> p c t", c=4)
                        if half == 0:
                            op("act", "copy", [f"pb{half}"], [f"hT{G % 2}_{tt}"], out=dst, in_=src)
                        else:
                            op("dve", "tensor_copy", [f"pb{half}"], [f"hT{G % 2}_{tt}"], out=dst, in_=src)

            def fm(G, coff, ncols=128):
                i, p, pk = nextpf()
                hTb = hT[G % 2]
                mmg([(p[0:ncols, :], win[:, c, coff:coff + ncols], hTb[:, c, :]) for c in range(8)],
                    WIN + [f"hT{G % 2}_{tt}" for tt in range(4)], [pk])
                return p, pk

            def stage_f():
                nst[0] = (nst[0] + 1) % 3
                return stf[nst[0]], f"stf{nst[0]}"

            def stage_b():
                nst[1] = (nst[1] + 1) % 3
                return stb[nst[1]], f"stb{nst[1]}"

            for G in range(NG):
                g0 = G * 512
                load_norm(G)
                hk = [f"hT{G % 2}_{tt}" for tt in range(4)]
                for j in range(6):
                    p, pk = fm(G, WOFF["gq"] + j * 128)
                    st, sk = stage_f()
                    op("act", "copy", [pk], [sk], out=st[:], in_=p[:])
                    dma("pool", glaT[j * 128:(j + 1) * 128, g0:g0 + 512], st[:], [sk], [], "o_" + sk)
                p, pk = fm(G, WOFF["ga"], 16)
                op("act", "copy", [pk], ["gaT"], out=gaT[0:16, :], in_=p[0:16, :])
                for j in range(2):
                    i, p2, pk2 = nextpf()
                    mmg([(p2[:], wga[:, j * 128:(j + 1) * 128], gaT[:, :])], ["wga", "gaT"], [pk2])
                    st, sk = stage_f()
                    op("act", "activation", [pk2], [sk], out=st[:], in_=p2[:], func=AF.Exp, scale=-1.0)
                    op("act", "activation", [sk, "onec"], [sk], out=st[:], in_=st[:], func=AF.Ln, bias=onec[:])
                    dma("pool", glaT[768 + j * 128:768 + (j + 1) * 128, g0:g0 + 512], st[:], [sk], [], "o_" + sk)
                for tt in range(4):
                    t = G * 4 + tt
                    b = t % 2
                    i, p, pk = nextpf()
                    mmg([(p[:, 0:8], hT[G % 2][:, c, tt * 128:(tt + 1) * 128], win[:, c, WOFF["dba"]:WOFF["dba"] + 8]) for c in range(8)],
                        WIN + hk, [pk])
                    scb, sck, stt_, stk = sc[b], f"sc{b}", sct[b], f"sct{b}"
                    op("act", "activation", [pk], [stk], out=stt_[:, 0:4], in_=p[:, 0:4], func=AF.Exp, scale=-1.0)
                    op("dve", "tensor_scalar", [stk], [stk], out=stt_[:, 0:4], in0=stt_[:, 0:4], scalar1=1.0, scalar2=None, op0=ALU.add)
                    op("dve", "reciprocal", [stk], [sck], out=scb[:, 4:8], in_=stt_[:, 0:4])
                    op("dve", "tensor_tensor", [pk, "dtb"], [stk], out=stt_[:, 4:8], in0=p[:, 4:8], in1=dtb[:], op=ALU.add)
                    op("act", "activation", [stk], [stk], out=stt_[:, 4:8], in_=stt_[:, 4:8], func=AF.Exp)
                    op("act", "activation", [stk, "onec"], [stk], out=stt_[:, 4:8], in_=stt_[:, 4:8], func=AF.Ln, bias=onec[:])
                    op("dve", "tensor_tensor", [stk, "nA"], [stk], out=stt_[:, 4:8], in0=stt_[:, 4:8], in1=nA[:], op=ALU.mult)
                    i2, pc, pck = nextpf()
                    mmg([(pc[:, 0:4], triu[:], stt_[:, 4:8])], ["triu", stk], [pck])
                    op("act", "copy", [pck], [sck], out=scb[:, 0:4], in_=pc[:, 0:4])
                    op("act", "activation", [pck], [sck], out=scb[:, 8:12], in_=pc[:, 0:4], func=AF.Exp)
                    op("dve", "tensor_scalar", [pck], [sck], out=scb[:, 20:24], in0=pc[:, 0:4], scalar1=-1.0, scalar2=None, op0=ALU.mult)
                    i3, pr, prk = nextpf()
                    mmg([(pr[:, 0:4], trisu[:], stt_[:, 4:8])], ["trisu", stk], [prk])
                    op("act", "activation", [prk], [sck], out=scb[:, 16:20], in_=pr[:, 0:4], func=AF.Exp)
                    op("dve", "tensor_tensor", [sck], [sck], out=scb[:, 12:16], in0=scb[:, 4:8], in1=scb[:, 8:12], op=ALU.mult)
                    dma("pool", gsct[t * 128:(t + 1) * 128, :], scb[:], [sck], [], "o_" + sck)
                for j in range(6):
                    p, pk = fm(G, WOFF["dq"] + j * 128)
                    xk = f"xc{j}"
                    op("act", "copy", [pk], [xk], out=xc[j][:, 3:515], in_=p[:])
                    b = j % 2
                    cyb, cyk, ceb, cek = cy[b], f"cy{b}", ce[b], f"ce{b}"
                    op("dve", "tensor_scalar", [xk, "convw"], [cyk], out=cyb[:], in0=xc[j][:, 3:515], scalar1=convw[:, j, 3:4], scalar2=None, op0=ALU.mult)
                    for i in range(3):
                        op("dve", "scalar_tensor_tensor", [xk, "convw", cyk], [cyk], out=cyb[:], in0=xc[j][:, i:i + 512],
                           scalar=convw[:, j, i:i + 1], in1=cyb[:], op0=ALU.mult, op1=ALU.add)
                    op("pool", "tensor_copy", [xk], [xk], out=xc[j][:, 0:3], in_=xc[j][:, 512:515])
                    op("act", "activation", [cyk], [cek], out=ceb[:], in_=cyb[:], func=AF.Exp, scale=-1.0)
                    op("pool", "tensor_scalar", [cek], [cek], out=ceb[:], in0=ceb[:], scalar1=1.0, scalar2=None, op0=ALU.add)
                    op("dve", "reciprocal", [cek], [cek], out=ceb[:], in_=ceb[:])
                    op("dve", "tensor_tensor", [cek, cyk], [cyk], out=cyb[:], in0=cyb[:], in1=ceb[:], op=ALU.mult)
                    st, sk = stage_b()
                    if j < 4:
                        op("pool", "tensor_tensor", [cyk], [cek], out=ceb[:], in0=cyb[:], in1=cyb[:], op=ALU.mult)
                        i, pn, pnk = nextpf()
                        mmg([(pn[:], bones[:], ceb[:])], ["bones", cek], [pnk])
                        op("act", "activation", [pnk, "epsc"], [cek], out=ceb[:], in_=pn[:], func=AF.Ln, bias=epsc[:])
                        op("act", "activation", [cek], [cek], out=ceb[:], in_=ceb[:], func=AF.Exp, scale=-0.5)
                        op("dve", "scalar_tensor_tensor", [cyk, cek], [sk], out=st[:], in0=cyb[:], scalar=(0.125 if j < 2 else 1.0),
                           in1=ceb[:], op0=ALU.mult, op1=ALU.mult)
                    else:
                        op("dve", "tensor_copy", [cyk], [sk], out=st[:], in_=cyb[:])
                    dma("pool", gdnT[j * 128:(j + 1) * 128, g0:g0 + 512], st[:], [sk], [], "o_" + sk)
                for tbl in range(4):
                    dma("sp", rope_t[tbl][:], c_rope[tbl, :, g0:g0 + 512], [], [f"rope{tbl}"], f"rope{tbl}")
                for isk in range(2):
                    for j in range(4):
                        p, pk = fm(G, WOFF["mk" if isk else "mq"] + j * 128)
                        b = j % 2
                        op("dve", "tensor_tensor", [pk, f"rope{2 * isk}"], [f"r1{b}"], out=r1[b][:], in0=p[:], in1=rope_t[2 * isk][:], op=ALU.mult)
                        p2, pk2 = fm(G, WOFF["mks" if isk else "mqs"] + j * 128)
                        op("dve", "tensor_tensor", [pk2, f"rope{2 * isk + 1}"], [f"r2{b}"], out=r2[b][:], in0=p2[:], in1=rope_t[2 * isk + 1][:], op=ALU.mult)
                        op("pool", "tensor_tensor", [f"r1{b}", f"r2{b}"], [f"r1{b}"], out=r1[b][:], in0=r1[b][:], in1=r2[b][:], op=ALU.add)
                        st, sk = stage_b()
                        op("act", "copy", [f"r1{b}"], [sk], out=st[:], in_=r1[b][:])
                        dst = (mkT if isk else mqT)
                        dma("pool", dst[j * 128:(j + 1) * 128, g0:g0 + 512], st[:], [sk], [], "o_" + sk)
                        if isk:
                            op("dve", "tensor_reduce", [f"r1{b}"], ["ksum"], out=ksum[:, j, 2 * G:2 * G + 2],
                               in_=r1[b][:].rearrange("p (n k) -> p n k", n=2), axis=AX.X, op=ALU.add)
                for tt in range(4):
                    t = G * 4 + tt
                    lhs = lambda c, tt=tt: hT[G % 2][:, c, tt * 128:(tt + 1) * 128]
                    i, p, pk = nextpf()
                    mmg([(p[:], lhs(c), win[:, c, WOFF["z"]:WOFF["z"] + 512]) for c in range(8)], WIN + hk, [pk])
                    st, sk = stage_f()
                    b = tt % 2
                    op("act", "activation", [pk], [f"ce{b}"], out=ce[b][:], in_=p[:], func=AF.Exp, scale=-1.0)
                    op("pool", "tensor_scalar", [f"ce{b}"], [f"ce{b}"], out=ce[b][:], in0=ce[b][:], scalar1=1.0, scalar2=None, op0=ALU.add)
                    op("dve", "reciprocal", [f"ce{b}"], [f"ce{b}"], out=ce[b][:], in_=ce[b][:])
                    op("dve", "tensor_tensor", [f"ce{b}", pk], [sk], out=st[:], in0=p[:], in1=ce[b][:], op=ALU.mult)
                    dma("pool", zs[t * 128:(t + 1) * 128, :], st[:], [sk], [], "o_" + sk)
                    i, p, pk = nextpf()
                    mmg([(p[:], lhs(c), win[:, c, WOFF["mv"]:WOFF["mv"] + 512]) for c in range(8)], WIN + hk, [pk])
                    st, sk = stage_b()
                    op("act", "copy", [pk], [sk], out=st[:], in_=p[:])
                    dma("pool", mvs[t * 128:(t + 1) * 128, :], st[:], [sk], [], "o_" + sk)
            for j in range(4):
                dma("pool", mksum[j * 128:(j + 1) * 128, :], ksum[:, j, :], ["ksum"], [], "o_ksum")
            S_.barrier()

        def head_out_stage(t, O, Ok, col0, zcol0, normw, zt, ztk, tmp, tmpk, ms, msk, outb, outbk, chan):
            dma("sp", zt[:], zs[t * 128:(t + 1) * 128, zcol0:zcol0 + 256], [], [ztk], "zt" + chan)
            Okl = Ok if isinstance(Ok, list) else [Ok]
            op("act", "activation", Okl, [tmpk], out=tmp[:], in_=O[:], func=AF.Square)
            op("dve", "tensor_reduce", [tmpk], [msk], out=ms[:], in_=tmp[:].rearrange("p (h d) -> p h d", h=4), axis=AX.X, op=ALU.add)
            rstd_from_ss(ms[:], ms[:], 64, [msk], [msk])
            op("dve", "tensor_tensor", Okl + [msk], [tmpk], out=tmp[:].rearrange("p (h d) -> p h d", h=4),
               in0=O[:].rearrange("p (h d) -> p h d", h=4), in1=ms[:].unsqueeze(2).to_broadcast([128, 4, 64]), op=ALU.mult)
            op("pool", "tensor_tensor", [ztk, "normw"], [ztk], out=zt[:].rearrange("p (h d) -> p h d", h=4),
               in0=zt[:].rearrange("p (h d) -> p h d", h=4), in1=normw[:].unsqueeze(1).to_broadcast([128, 4, 64]), op=ALU.mult)
            op("dve", "tensor_tensor", [tmpk, ztk], [outbk], out=outb[:], in0=tmp[:], in1=zt[:], op=ALU.mult)
            dma("pool", mix[t * 128:(t + 1) * 128, col0:col0 + 256], outb[:], [outbk], [], "o_" + chan)

        def phase_GLA(l):
            arena[0] = GLOBAL_TOP
            masku = sb("masku", [128, 128], F32)
            normw = sb("normw", [128, 64], F32)
            dma("sp", masku[:], c_masku, [], ["masku"], "c2")
            dma("sp", normw[:], hn_d[l, 0:1, :].broadcast_to([128, 64]), [], ["normw"], "c3")
            inT = [[sb(f"gin{b}_{k}", [128, 2, 128], F32) for k in range(4)] for b in range(2)]
            ones = sb("ones", [128, 128], F32)
            op("dve", "memset", [], ["ones"], ap=ones[:], constant=1.0)
            cum = [sb(f"cum{g}", [128, 128], F32) for g in range(2)]
            nb = [sb(f"nb{g}", [128, 1], F32) for g in range(2)]
            E = [[sb(f"E{g}_{k}", [128, 128], F32) for k in range(3)] for g in range(2)]
            qt = [sb(f"qt{g}", [128, 128], BF16) for g in range(2)]
            kt = [sb(f"kt{g}", [128, 128], BF16) for g in range(2)]
            kh = [sb(f"kh{g}", [128, 128], BF16) for g in range(2)]
            vb = [sb(f"vb{g}", [128, 128], BF16) for g in range(2)]
            khT = [sb(f"khT{g}", [128, 128], BF16) for g in range(2)]
            vT = [sb(f"vT{g}", [128, 128], BF16) for g in range(2)]
            AT = [sb(f"AT{h}", [128, 128], BF16) for h in range(4)]
            St = [sb(f"St{g}", [128, 64], F32) for g in range(2)]
            Sb = [sb(f"Sb{g}", [128, 64], BF16) for g in range(2)]
            O = [sb(f"O{b}", [128, 256], F32) for b in range(2)]
            zt = [sb(f"zt{b}", [128, 256], F32) for b in range(2)]
            tmp = sb("tmp", [128, 256], F32)
            ms = sb("ms", [128, 4], F32)
            outb = [sb(f"outb{b}", [128, 256], BF16) for b in range(2)]
            for g in range(2):
                op("dve", "memset", [], [f"St{g}"], ap=St[g][:], constant=0.0)
                op("pool", "memset", [], [f"Sb{g}"], ap=Sb[g][:], constant=0.0)
            for t in range(NT):
                b = t % 2
                for k in range(4):
                    dma("sp", inT[b][k][:], glaT[k * 256:(k + 1) * 256, t * 128:(t + 1) * 128].rearrange("(g p) t -> p g t", p=128),
                        [], [f"gin{b}_{k}"], f"gin{b}_{k}")
                for g in range(2):
                    q_, k_, v_, sp_ = (inT[b][k][:, g, :] for k in range(4))
                    rk = [f"gin{b}_{k}" for k in range(4)]
                    ck = f"cum{g}"
                    op("dve", "tensor_tensor_scan", [rk[3], "ones"], [ck], out=cum[g][:], data0=ones[:], data1=sp_, initial=0.0,
                       op0=ALU.mult, op1=ALU.add)
                    op("dve", "tensor_scalar", [ck], [f"nb{g}"], out=nb[g][:], in0=cum[g][:, 127:128], scalar1=-1.0 / 16, scalar2=None, op0=ALU.mult)
                    op("act", "activation", [ck], [f"E{g}_0"], out=E[g][0][:], in_=cum[g][:], func=AF.Exp, scale=-1.0 / 16)
                    op("act", "activation", [ck], [f"E{g}_1"], out=E[g][1][:], in_=cum[g][:], func=AF.Exp, scale=1.0 / 16)
                    op("act", "activation", [ck, f"nb{g}"], [f"E{g}_2"], out=E[g][2][:], in_=cum[g][:], func=AF.Exp, scale=1.0 / 16, bias=nb[g][:])
                    op("dve", "scalar_tensor_tensor", [rk[0], f"E{g}_0"], [f"qt{g}"], out=qt[g][:], in0=q_, scalar=0.125, in1=E[g][0][:],
                       op0=ALU.mult, op1=ALU.mult)
                    op("dve", "tensor_tensor", [rk[1], f"E{g}_1"], [f"kt{g}"], out=kt[g][:], in0=k_, in1=E[g][1][:], op=ALU.mult)
                    op("pool", "tensor_tensor", [rk[1], f"E{g}_2"], [f"khT{g}"], out=khT[g][:], in0=k_, in1=E[g][2][:], op=ALU.mult)
                    op("pool", "tensor_copy", [rk[2]], [f"vT{g}"], out=vT[g][:], in_=v_)
                    trg([(PB[0][:, 0:128], khT[g][:], identb[:]), (PB[0][:, 128:256], vT[g][:], identb[:])],
                        [f"khT{g}", f"vT{g}", "identb"], ["pb0"])
                    op("act", "copy", ["pb0"], [f"kh{g}"], out=kh[g][:], in_=PB[0][:, 0:128])
                    op("act", "copy", ["pb0"], [f"vb{g}"], out=vb[g][:], in_=PB[0][:, 128:256])
                    for hh in range(2):
                        h = 2 * g + hh
                        r = slice(hh * 64, hh * 64 + 64)
                        pa, pak = PF[hh], f"pf{hh}"
                        mmg([(pa[:, 0:128], kt[g][r, :], qt[g][r, :])], [f"kt{g}", f"qt{g}"], [pak])
                        op("dve", "tensor_tensor", [pak, "masku"], [f"AT{h}"], out=AT[h][:], in0=pa[:, 0:128], in1=masku[:], op=ALU.mult)
                        mmg([(PF[2][:, h * 64:(h + 1) * 64], qt[g][r, :], Sb[g][r, :]),
                             (PF[2][:, h * 64:(h + 1) * 64], AT[h][:], vb[g][:, r])],
                            [f"qt{g}", f"Sb{g}", f"AT{h}", f"vb{g}"], [f"pf2_{h}"])
                        mmg([(PF[3][r, g * 64:(g + 1) * 64], kh[g][:, r], vb[g][:, r])], [f"kh{g}", f"vb{g}"], [f"pf3_{g}_{hh}"])
                    op("dve", "scalar_tensor_tensor", [f"St{g}", f"E{g}_0", f"pf3_{g}_0", f"pf3_{g}_1"], [f"St{g}"], out=St[g][:], in0=St[g][:],
                       scalar=E[g][0][:, 127:128], in1=PF[3][:, g * 64:(g + 1) * 64], op0=ALU.mult, op1=ALU.add)
                    op("act", "copy", [f"St{g}"], [f"Sb{g}"], out=Sb[g][:], in_=St[g][:])
                op("act", "copy", [f"pf2_{h}" for h in range(4)], [f"O{b}"], out=O[b][:], in_=PF[2][:, 0:256])
                head_out_stage(t, O[b], f"O{b}", 0, 0, normw, zt[b], f"zt{b}", tmp, "tmp", ms, "ms", outb[b], f"outb{b}", f"gla{b}")
            S_.barrier()

        def phase_GDN(l):
            arena[0] = GLOBAL_TOP
            negu = sb("negu", [128, 128], F32)
            posl = sb("posl", [128, 128], F32)
            normw = sb("normw", [128, 64], F32)
            onesr = sb("onesr", [1, 128], F32)
            dma("sp", negu[:], c_negu, [], ["negu"], "c2")
            dma("sp", posl[:], c_posl, [], ["posl"], "c3")
            dma("sp", normw[:], hn_d[l, 1:2, :].broadcast_to([128, 64]), [], ["normw"], "c4")
            op("dve", "memset", [], ["onesr"], ap=onesr[:], constant=1.0)
            egl = [sb(f"egl{g}", [128, NT], F32) for g in range(2)]
            for g in range(2):
                for hh in range(2):
                    h = 2 * g + hh
                    src = gsct.rearrange("(t p) c -> p t c", p=128)[127:128, :, 8 + h]
                    dma("sp", egl[g][hh * 64:(hh + 1) * 64, :], src.broadcast_to([64, NT]), [], [f"egl{g}"], f"egl{g}{hh}", slow=True)
            inT = [sb(f"din{b}", [128, 6, 128], BF16) for b in range(2)]
            SC = [sb(f"SC{b}", [128, 24], F32) for b in range(2)]
            kbe = [sb(f"kbe{h}", [128, 64], BF16) for h in range(4)]
            khat = [sb(f"khat{h}", [128, 64], BF16) for h in range(4)]
            vbt = [sb(f"vbt{h}", [128, 64], BF16) for h in range(4)]
            gcr = [sb(f"gcr{h}", [1, 128], F32) for h in range(4)]
            DT = [sb(f"DT{h}", [128, 128], F32) for h in range(4)]
            DL = [sb(f"DL{h}", [128, 128], F32) for h in range(4)]
            attnT = [sb(f"attnT{h}", [128, 128], BF16) for h in range(4)]
            Am = [[sb(f"Am{h}_{i}", [128, 128], BF16) for i in range(2)] for h in range(4)]
            Cm = [[sb(f"Cm{h}_{i}", [128, 128], BF16) for i in range(2)] for h in range(4)]
            Ym = [[sb(f"Ym{h}_{i}", [128, 128], BF16) for i in range(2)] for h in range(4)]
            u = [sb(f"u{h}", [128, 64], F32) for h in range(4)]
            wT = [sb(f"wT{g}", [128, 128], BF16) for g in range(2)]
            vn = [sb(f"vn{h}", [128, 64], BF16) for h in range(4)]
            o1 = [sb(f"o1{h}", [128, 64], F32) for h in range(4)]
            St = [sb(f"St{g}", [128, 64], F32) for g in range(2)]
            Sb = [sb(f"Sb{g}", [128, 64], BF16) for g in range(2)]
            O = [sb(f"O{b}", [128, 256], F32) for b in range(2)]
            zt = [sb(f"zt{b}", [128, 256], F32) for b in range(2)]
            tmp = sb("tmp", [128, 256], F32)
            ms = sb("ms", [128, 4], F32)
            outb = [sb(f"outb{b}", [128, 256], BF16) for b in range(2)]
            for g in range(2):
                op("dve", "memset", [], [f"St{g}"], ap=St[g][:], constant=0.0)
                op("pool", "memset", [], [f"Sb{g}"], ap=Sb[g][:], constant=0.0)
            pfi = [0]

            def npf():
                pfi[0] = (pfi[0] + 1) % 6
                return PF[pfi[0]], f"pf{pfi[0]}"

            for t in range(NT):
                b = t % 2
                dma("sp", inT[b][:], gdnT[:, t * 128:(t + 1) * 128].rearrange("(g p) t -> p g t", p=128), [], [f"din{b}"], f"din{b}")
                dma("sp", SC[b][:], gsct[t * 128:(t + 1) * 128, :], [], [f"SC{b}"], f"SC{b}")
                ink, sck = f"din{b}", f"SC{b}"
                col = lambda q, h: SC[b][:, 4 * q + h:4 * q + h + 1]

                def qT(h): return inT[b][(h % 2) * 64:(h % 2) * 64 + 64, 0 + h // 2, :]
                def kT(h): return inT[b][(h % 2) * 64:(h % 2) * 64 + 64, 2 + h // 2, :]
                def vT(h): return inT[b][(h % 2) * 64:(h % 2) * 64 + 64, 4 + h // 2, :]

                for h in range(4):
                    hh = h % 2
                    pbk = f"pb{hh}"
                    trg([(PB[hh][:, 0:64], kT(h), identb[hh * 64:hh * 64 + 64, hh * 64:hh * 64 + 64]),
                         (PB[hh][:, 64:128], vT(h), identb[hh * 64:hh * 64 + 64, hh * 64:hh * 64 + 64])], [ink, "identb"], [pbk])
                    op("dve", "tensor_scalar", [pbk, sck], [f"kbe{h}"], out=kbe[h][:], in0=PB[hh][:, 0:64], scalar1=col(3, h), scalar2=None, op0=ALU.mult)
                    op("act", "activation", [pbk, sck], [f"khat{h}"], out=khat[h][:], in_=PB[hh][:, 0:64], func=AF.Copy, scale=col(4, h))
                    op("act", "activation", [pbk, sck], [f"vbt{h}"], out=vbt[h][:], in_=PB[hh][:, 64:128], func=AF.Copy, scale=col(1, h))
                    p, pk = npf()
                    mmg([(p[0:1, 0:128], col(0, h), identf[:])], [sck, "identf"], [pk])
                    op("act", "copy", [pk], [f"gcr{h}"], out=gcr[h][:], in_=p[0:1, 0:128])
                    p, pk = npf()
                    mmg([(p[:, 0:128], onesr[:], gcr[h][:]), (p[:, 0:128], identf[:], negu[:])], ["onesr", f"gcr{h}", "identf", "negu"], [pk])
                    op("act", "activation", [pk, sck], [f"DT{h}"], out=DT[h][:], in_=p[:, 0:128], func=AF.Exp, bias=col(5, h))
                    p, pk = npf()
                    mmg([(p[:, 0:128], onesr[:], gcr[h][:]), (p[:, 0:128], identf[:], posl[:])], ["onesr", f"gcr{h}", "identf", "posl"], [pk])
                    op("act", "activation", [pk, sck], [f"DL{h}"], out=DL[h][:], in_=p[:, 0:128], func=AF.Exp, scale=-1.0, bias=col(0, h))
                    p, pk = npf()
                    mmg([(p[:, 0:128], kT(h), qT(h))], [ink], [pk])
                    op("dve", "tensor_tensor", [pk, f"DT{h}"], [f"attnT{h}"], out=attnT[h][:], in0=p[:, 0:128], in1=DT[h][:], op=ALU.mult)
                    p, pk = npf()
                    mmg([(p[:, 0:128], kT(h), kT(h))], [ink], [pk])
                    op("dve", "scalar_tensor_tensor", [pk, sck, f"DL{h}"], [f"Cm{h}_0"], out=Cm[h][0][:], in0=p[:, 0:128], scalar=col(1, h),
                       in1=DL[h][:], op0=ALU.mult, op1=ALU.mult)
                    trg([(PB[hh][:, 128:256], Cm[h][0][:], identb[:])], [f"Cm{h}_0", "identb"], [pbk + "b"])
                    op("act", "copy", [pbk + "b"], [f"Am{h}_0"], out=Am[h][0][:], in_=PB[hh][:, 128:256])
                    op("pool", "tensor_tensor", [f"Am{h}_0", "identb"], [f"Ym{h}_0"], out=Ym[h][0][:], in0=identb[:], in1=Am[h][0][:], op=ALU.subtract)
                for k in range(1, 7):
                    a0, a1 = (k - 1) % 2, k % 2
                    for h in range(4):
                        p, pk = npf()
                        mmg([(p[:, 0:128], Am[h][a0][:], Cm[h][a0][:])], [f"Am{h}_{a0}", f"Cm{h}_{a0}"], [pk])
                        op("act" if h % 2 == 0 else "dve", "copy" if h % 2 == 0 else "tensor_copy", [pk], [f"Cm{h}_{a1}"], out=Cm[h][a1][:], in_=p[:, 0:128])
                        if k < 6:
                            p2, pk2 = npf()
                            mmg([(p2[:, 0:128], Cm[h][a0][:], Am[h][a0][:])], [f"Am{h}_{a0}", f"Cm{h}_{a0}"], [pk2])
                            op("dve" if h % 2 == 0 else "act", "tensor_copy" if h % 2 == 0 else "copy", [pk2], [f"Am{h}_{a1}"], out=Am[h][a1][:], in_=p2[:, 0:128])
                    for h in range(4):
                        p, pk = npf()
                        mmg([(p[:, 0:128], identb[:], Ym[h][a0][:]), (p[:, 0:128], Cm[h][a1][:], Ym[h][a0][:])],
                            ["identb", f"Ym{h}_{a0}", f"Cm{h}_{a1}"], [pk])
                        op("act" if h % 2 == 0 else "dve", "copy" if h % 2 == 0 else "tensor_copy", [pk], [f"Ym{h}_{a1}"], out=Ym[h][a1][:], in_=p[:, 0:128])
                YF = 0
                for h in range(4):
                    g, hh = h // 2, h % 2
                    r = slice(hh * 64, hh * 64 + 64)
                    p, pk = npf()
                    mmg([(p[:, 0:64], Ym[h][YF][:], vbt[h][:])], [f"Ym{h}_{YF}", f"vbt{h}"], [pk])
                    op("act", "copy", [pk], [f"u{h}"], out=u[h][:], in_=p[:, 0:64])
                    p, pk = npf()
                    mmg([(p[r, 0:128], kbe[h][:], Ym[h][YF][:])], [f"Ym{h}_{YF}", f"kbe{h}"], [pk])
                    op("act", "copy", [pk], [f"wT{g}_{hh}"], out=wT[g][r, :], in_=p[r, 0:128])
                for h in range(4):
                    g, hh = h // 2, h % 2
                    r = slice(hh * 64, hh * 64 + 64)
                    p, pk = npf()
                    mmg([(p[:, 0:64], wT[g][r, :], Sb[g][r, :])], [f"wT{g}_{hh}", f"Sb{g}"], [pk])
                    op("dve", "tensor_tensor", [f"u{h}", pk], [f"vn{h}"], out=vn[h][:], in0=u[h][:], in1=p[:, 0:64], op=ALU.subtract)
                    p, pk = npf()
                    mmg([(p[:, 0:64], qT(h), Sb[g][r, :])], [ink, f"Sb{g}"], [pk])
                    op("act", "activation", [pk, sck], [f"o1{h}"], out=o1[h][:], in_=p[:, 0:64], func=AF.Copy, scale=col(2, h))
                    p, pk = npf()
                    mmg([(p[:, 0:64], attnT[h][:], vn[h][:])], [f"attnT{h}", f"vn{h}"], [pk])
                    op("dve", "tensor_tensor", [f"o1{h}", pk], [f"O{b}_{h}"], out=O[b][:, h * 64:(h + 1) * 64], in0=o1[h][:], in1=p[:, 0:64], op=ALU.add)
                for g in range(2):
                    p, pk = npf()
                    for hh in range(2):
                        h = 2 * g + hh
                        r = slice(hh * 64, hh * 64 + 64)
                        mmg([(p[r, 0:64], khat[h][:], vn[h][:])], [f"khat{h}", f"vn{h}"], [pk])
                    op("dve", "scalar_tensor_tensor", [f"St{g}", f"egl{g}", pk], [f"St{g}"], out=St[g][:], in0=St[g][:],
                       scalar=egl[g][:, t:t + 1], in1=p[:, 0:64], op0=ALU.mult, op1=ALU.add)
                    op("act", "copy", [f"St{g}"], [f"Sb{g}"], out=Sb[g][:], in_=St[g][:])
                head_out_stage(t, O[b], [f"O{b}_{h}" for h in range(4)], 256, 256, normw, zt[b], f"zt{b}", tmp, "tmp", ms, "ms", outb[b], f"outb{b}", f"gdn{b}")
            S_.barrier()


        def phase_MOBA(l):
            arena[0] = GLOBAL_TOP
            qa = [sb(f"qa{i}", [128, S], BF16) for i in range(2)]
            ka = [sb(f"ka{i}", [128, S], BF16) for i in range(2)]
            Va = [sb(f"Va{i}", [128, NT, 65], BF16) for i in range(2)]
            ksf = [sb(f"ksf{i}", [64, 32], F32) for i in range(2)]
            ksb = [sb(f"ksb{i}", [64, 32], BF16) for i in range(2)]
            mb = sb("mb", [128, 32 * 32], F32)
            cm = sb("cm", [128, 4, 512], BF16)
            dma("sp", mb[:], c_mb, [], ["mb"], "c2")
            dma("sp", cm[:], c_cm.rearrange("r p q -> p r q"), [], ["cm"], "c3")
            for i in range(2):
                dma("sp", ka[i][64:128, :], c_onehot, [], [f"ka{i}_oh"], f"c4{i}")
                dma("sp", qa[i][96:128, :], c_onehot[32:64, :], [], [f"qa{i}_z"], f"c5{i}")
                op("pool", "memset", [], [f"Va{i}_1"], ap=Va[i][:, :, 64:65], constant=1.0)
            gm = [sb(f"gm{i}", [128, 32], F32) for i in range(2)]
            top8 = [sb(f"top8{i}", [128, 8], F32) for i in range(2)]
            bia = [sb(f"bia{i}", [128, 32], BF16) for i in range(2)]
            Pt = [sb(f"Pt{i}", [128, 512], BF16) for i in range(3)]
            osb = [sb(f"osb{i}", [128, 512], F32) for i in range(2)]
            for i in range(2):
                op("pool", "memset", [], [f"osb{i}"], ap=osb[i][:], constant=0.0)
            rden = [sb(f"rden{i}", [128, 1], F32) for i in range(2)]
            ob = [sb(f"ob{i}", [128, 64], BF16) for i in range(2)]
            of = [sb(f"of{i}", [128, 65], F32) for i in range(2)]
            pn = [0]
            import os
            MST = int(os.environ.get('MOBA_STAGE', '9'))
            for h in range(int(os.environ.get('MOBA_HEADS', '8'))):
                i = h % 2
                dma("sp", qa[i][0:64, :], mqT[h * 64:(h + 1) * 64, :], [], [f"qa{i}"], f"qa{i}")
                dma("pool", ka[i][0:64, :], mkT[h * 64:(h + 1) * 64, :], [], [f"ka{i}"], f"ka{i}")
                dma("sp", Va[i][:, :, 0:64], mvs[:, h * 64:(h + 1) * 64].rearrange("(t p) d -> p t d", p=128), [], [f"Va{i}"], f"Va{i}")
                dma("sp", ksf[i][:], mksum[h * 64:(h + 1) * 64, :], [], [f"ksf{i}"], f"ksf{i}")
                op("dve", "tensor_copy", [f"ksf{i}"], [f"ksb{i}"], out=ksb[i][:], in_=ksf[i][:])
                for t in range(NT if MST >= 1 else 0):
                    j = t % 2
                    own = t // 2
                    mmg([(PF[4][:, j * 32:(j + 1) * 32], qa[i][0:64, t * 128:(t + 1) * 128], ksb[i][:, :])], [f"qa{i}", f"ksb{i}"], [f"pf4_{j}"])
                    op("dve", "tensor_tensor", [f"pf4_{j}", "mb"], [f"gm{j}"], out=gm[j][:], in0=PF[4][:, j * 32:(j + 1) * 32],
                       in1=mb[:, own * 32:(own + 1) * 32], op=ALU.add)
                    op("dve", "max", [f"gm{j}"], [f"top8{j}"], out=top8[j][:], in_=gm[j][:])
                    op("dve", "tensor_scalar", [f"top8{j}"], [f"top8{j}"], out=top8[j][:, 3:4], in0=top8[j][:, 3:4], scalar1=-BIG / 2, scalar2=None, op0=ALU.max)
                    op("dve", "tensor_scalar", [f"gm{j}", f"top8{j}"], [f"gm{j}"], out=gm[j][:], in0=gm[j][:], scalar1=top8[j][:, 3:4], scalar2=BIG,
                       op0=ALU.is_ge, op1=ALU.mult)
                    op("dve", "tensor_scalar", [f"gm{j}"], [f"bia{j}"], out=bia[j][:], in0=gm[j][:], scalar1=-BIG, scalar2=None, op0=ALU.add)
                    trg([(PB[j][64:96, 0:128], bia[j][:], identb[:])], [f"bia{j}", "identb"], [f"pb{j}"])
                    op("act", "copy", [f"pb{j}"], [f"qa{i}_b{t}"], out=qa[i][64:96, t * 128:(t + 1) * 128], in_=PB[j][64:96, 0:128])
                for G in range(NG if MST >= 2 else 0):
                    qk = [f"qa{i}", f"qa{i}_z"] + [f"qa{i}_b{t}" for t in range(G * 4, G * 4 + 4)]
                    oi = G % 2
                    nk = 4 * G + 4
                    for kt_ in range(nk):
                        pi = pn[0] % 2
                        pn[0] += 1
                        pti = pn[0] % 3
                        mmg([(PF[pi][:], ka[i][:, kt_ * 128:(kt_ + 1) * 128], qa[i][:, G * 512:(G + 1) * 512])],
                            [f"ka{i}", f"ka{i}_oh"] + qk, [f"pf{pi}"])
                        op("act", "activation", [f"pf{pi}"], [f"Pt{pti}"], out=Pt[pti][:], in_=PF[pi][:], func=AF.Exp)
                        if kt_ >= 4 * G:
                            op("pool", "tensor_tensor", [f"Pt{pti}", "cm"], [f"Pt{pti}"], out=Pt[pti][:], in0=Pt[pti][:], in1=cm[:, kt_ - 4 * G, :], op=ALU.mult)

                        def f(e, o=PF[2 + oi][0:65, :], l_=Va[i][:, kt_, :], r_=Pt[pti][:], st=(kt_ == 0), sp=(kt_ == nk - 1)):
                            return e.matmul(out=o, lhsT=l_, rhs=r_, start=st, stop=sp)
                        S_.add("pe", f, reads=[f"Va{i}", f"Va{i}_1", f"Pt{pti}"], writes=[f"pf{2 + oi}"])
                    op("act", "copy", [f"pf{2 + oi}"], [f"osb{oi}"], out=osb[oi][0:65, :], in_=PF[2 + oi][0:65, :])
                    for tt in range(4 if MST >= 3 else 0):
                        t = G * 4 + tt
                        j = tt % 2
                        mmg([(PF[5][:, 0:128], osb[oi][:, tt * 128:(tt + 1) * 128], identf[:])], [f"osb{oi}", "identf"], ["pf5"])
                        SUB = int(os.environ.get('MOBA_SUB', '9'))
                        if SUB >= 1:
                            op("dve", "tensor_copy", ["pf5"], [f"of{j}"], out=of[j][:], in_=PF[5][:, 0:65])
                            op("dve", "reciprocal", [f"of{j}"], [f"rden{j}"], out=rden[j][:], in_=of[j][:, 64:65])
                        if SUB >= 2:
                            op("dve", "tensor_scalar", [f"of{j}", f"rden{j}"], [f"ob{j}"], out=ob[j][:], in0=of[j][:, 0:64],
                               scalar1=rden[j][:], scalar2=None, op0=ALU.mult)
                        if SUB >= 3:
                            dma("pool", mix[t * 128:(t + 1) * 128, 512 + h * 64:512 + (h + 1) * 64], ob[j][:], [f"ob{j}"], [], f"o_ob{j}")
            S_.barrier()

        def phase_C1(l, xsrc):
            arena[0] = GLOBAL_TOP
            wo = sb("wo", [128, 8, D], BF16)
            gpost = sb("gpost", [128, D], F32)
            gpre2 = sb("gpre2", [128, D], F32)
            for c in range(8):
                dma("sp" if c % 2 == 0 else "pool", wo[:, c, :], wo_d[l, c * 128:(c + 1) * 128, :], [], [f"wo{c}"], f"w{c}")
            WO = [f"wo{c}" for c in range(8)]
            dma("sp", gpost[:], gains_d[l, 1:2, :].broadcast_to([128, D]), [], ["gpost"], "c2")
            dma("sp", gpre2[:], gains_d[l, 2:3, :].broadcast_to([128, D]), [], ["gpre2"], "c3")
            mt = [sb(f"mt{i}", [128, D], BF16) for i in range(2)]
            mT = [sb(f"mT{i}", [128, 8, 128], BF16) for i in range(2)]
            xt = [sb(f"xt{i}", [128, D], F32) for i in range(2)]
            yn = [sb(f"yn{i}", [128, D], F32) for i in range(2)]
            sq = sb("sq", [128, D], F32)
            ss = [sb(f"ss{i}", [128, 2], F32) for i in range(2)]
            rs = [sb(f"rs{i}", [128, 1], F32) for i in range(2)]
            hb = [sb(f"hb{i}", [128, D], BF16) for i in range(2)]
            for t in range(NT):
                b = t % 2
                rows = slice(t * 128, (t + 1) * 128)
                dma("sp", mt[b][:], mix[rows, :], [], [f"mt{b}"], f"mt{b}")
                dma("sp", xt[b][:], xsrc[rows, :], [], [f"xt{b}"], f"xt{b}")
                for half in range(2):
                    trg([(PB[half][:, c * 128:(c + 1) * 128], mt[b][:, (half * 4 + c) * 128:(half * 4 + c + 1) * 128], identb[:]) for c in range(4)],
                        [f"mt{b}", "identb"], [f"pb{half}"])
                    src = PB[half][:, 0:512].rearrange("p (c t) -> p c t", c=4)
                    if half == 0:
                        op("act", "copy", [f"pb{half}"], [f"mT{b}_{half}"], out=mT[b][:, 0:4, :], in_=src)
                    else:
                        op("dve", "tensor_copy", [f"pb{half}"], [f"mT{b}_{half}"], out=mT[b][:, 4:8, :], in_=src)
                for n in range(2):
                    pi = (2 * t + n) % 4
                    mmg([(PF[pi][:], mT[b][:, c, :], wo[:, c, n * 512:(n + 1) * 512]) for c in range(8)], WO + [f"mT{b}_0", f"mT{b}_1"], [f"pf{pi}"])
                    op("act", "activation", [f"pf{pi}"], ["sq", f"ss{b}_{n}"], out=sq[:, n * 512:(n + 1) * 512], in_=PF[pi][:], func=AF.Square,
                       accum_out=ss[b][:, n:n + 1])
                op("dve", "tensor_tensor", [f"ss{b}_0", f"ss{b}_1"], [f"rs{b}"], out=rs[b][:], in0=ss[b][:, 0:1], in1=ss[b][:, 1:2], op=ALU.add)
                rstd_from_ss(rs[b][:], rs[b][:], D, [f"rs{b}"], [f"rs{b}"])
                for n in range(2):
                    pi = (2 * t + n) % 4
                    op("dve", "scalar_tensor_tensor", [f"pf{pi}", f"rs{b}", "gpost"], [f"yn{b}_{n}"], out=yn[b][:, n * 512:(n + 1) * 512], in0=PF[pi][:],
                       scalar=rs[b][:], in1=gpost[:, n * 512:(n + 1) * 512], op0=ALU.mult, op1=ALU.mult)
                op("pool", "tensor_tensor", [f"yn{b}_0", f"yn{b}_1", f"xt{b}"], [f"yn{b}"], out=yn[b][:], in0=yn[b][:], in1=xt[b][:], op=ALU.add)
                dma("pool", x1s[rows, :], yn[b][:], [f"yn{b}"], [], f"o_yn{b}")
                op("act", "activation", [f"yn{b}"], ["sq", f"ss{b}_0"], out=sq[:], in_=yn[b][:], func=AF.Square, accum_out=ss[b][:, 0:1])
                rstd_from_ss(ss[b][:, 0:1], rs[b][:], D, [f"ss{b}_0"], [f"rs{b}"])
                op("dve", "scalar_tensor_tensor", [f"yn{b}", f"rs{b}", "gpre2"], [f"hb{b}"], out=hb[b][:], in0=yn[b][:], scalar=rs[b][:], in1=gpre2[:],
                   op0=ALU.mult, op1=ALU.mult)
                dma("pool", h2s[rows, :], hb[b][:], [f"hb{b}"], [], f"o_hb{b}")
            S_.barrier()

        def phase_C2(l, dst):
            arena[0] = GLOBAL_TOP
            wg = sb("wg", [128, 8, DFF], BF16)
            wu = sb("wu", [128, 8, DFF], BF16)
            wd = sb("wd", [128, NFC, D], BF16)
            gpost = sb("gpost", [128, D], F32)
            for c in range(8):
                dma("sp", wg[:, c, :], wg_d[l, c * 128:(c + 1) * 128, :], [], [f"wg{c}"], f"w{c}")
                dma("pool", wu[:, c, :], wu_d[l, c * 128:(c + 1) * 128, :], [], [f"wu{c}"], f"wu{c}")
            for f_ in range(NFC):
                dma("sp" if f_ % 2 else "pool", wd[:, f_, :], wd_d[l, f_ * 128:(f_ + 1) * 128, :], [], [f"wd{f_}"], f"wd{f_ % 4}")
            WG = [f"wg{c}" for c in range(8)]
            WU = [f"wu{c}" for c in range(8)]
            WD = [f"wd{f_}" for f_ in range(NFC)]
            dma("sp", gpost[:], gains_d[l, 3:4, :].broadcast_to([128, D]), [], ["gpost"], "c2")
            ht = [sb(f"ht{i}", [128, D], BF16) for i in range(2)]
            hT = sb("hT", [128, 8, 512], BF16)
            aT = sb("aT", [128, NFC, 512], BF16)
            th = [sb(f"th{i}", [128, 512], F32) for i in range(2)]
            us = [sb(f"us{i}", [128, 512], F32) for i in range(2)]
            xt = [sb(f"xt{i}", [128, D], F32) for i in range(2)]
            yn = [sb(f"yn{i}", [128, D], F32) for i in range(2)]
            sq = sb("sq", [128, 512], F32)
            ss = [sb(f"ss{i}", [128, 2], F32) for i in range(2)]
            rs = [sb(f"rs{i}", [128, 1], F32) for i in range(2)]
            for G in range(NG):
                for tt in range(4):
                    t = G * 4 + tt
                    b = t % 2
                    dma("sp", ht[b][:], h2s[t * 128:(t + 1) * 128, :], [], [f"ht{b}"], f"ht{b}")
                    for half in range(2):
                        trg([(PB[half][:, c * 128:(c + 1) * 128], ht[b][:, (half * 4 + c) * 128:(half * 4 + c + 1) * 128], identb[:]) for c in range(4)],
                            [f"ht{b}", "identb"], [f"pb{half}"])
                        src = PB[half][:, 0:512].rearrange("p (c t) -> p c t", c=4)
                        dst_ = hT[:, half * 4:(half + 1) * 4, tt * 128:(tt + 1) * 128]
                        if half == 0:
                            op("act", "copy", [f"pb{half}"], [f"hT_{tt}"], out=dst_, in_=src)
                        else:
                            op("dve", "tensor_copy", [f"pb{half}"], [f"hT_{tt}"], out=dst_, in_=src)
                hk = [f"hT_{tt}" for tt in range(4)]
                for f_ in range(NFC):
                    b = f_ % 2
                    pg, pu = PF[2 * b], PF[2 * b + 1]
                    mmg([(pg[:], wg[:, c, f_ * 128:(f_ + 1) * 128], hT[:, c, :]) for c in range(8)], WG + hk, [f"pf{2 * b}"])
                    mmg([(pu[:], wu[:, c, f_ * 128:(f_ + 1) * 128], hT[:, c, :]) for c in range(8)], WU + hk, [f"pf{2 * b + 1}"])
                    op("act", "activation", [f"pf{2 * b}"], [f"th{b}"], out=th[b][:], in_=pg[:], func=AF.Tanh, scale=0.5)
                    op("act", "copy", [f"pf{2 * b + 1}"], [f"us{b}"], out=us[b][:], in_=pu[:])
                    op("dve", "scalar_tensor_tensor", [f"th{b}", f"pf{2 * b}"], [f"th{b}"], out=th[b][:], in0=th[b][:], scalar=1.0, in1=pg[:],
                       op0=ALU.add, op1=ALU.mult)
                    op("dve", "scalar_tensor_tensor", [f"th{b}", f"us{b}"], [f"aT_{f_}"], out=aT[:, f_, :], in0=th[b][:], scalar=0.5, in1=us[b][:],
                       op0=ALU.mult, op1=ALU.mult)
                ak = [f"aT_{f_}" for f_ in range(NFC)]
                for tt in range(4):
                    t = G * 4 + tt
                    b = t % 2
                    rows = slice(t * 128, (t + 1) * 128)
                    dma("sp", xt[b][:], x1s[rows, :], [], [f"xt{b}"], f"xt{b}")
                    for n in range(2):
                        pi = 4 + n
                        mmg([(PF[pi][:], aT[:, f_, tt * 128:(tt + 1) * 128], wd[:, f_, n * 512:(n + 1) * 512]) for f_ in range(NFC)], WD + ak, [f"pf{pi}"])
                        op("act", "activation", [f"pf{pi}"], ["sq", f"ss{b}_{n}"], out=sq[:], in_=PF[pi][:], func=AF.Square, accum_out=ss[b][:, n:n + 1])
                    op("dve", "tensor_tensor", [f"ss{b}_0", f"ss{b}_1"], [f"rs{b}"], out=rs[b][:], in0=ss[b][:, 0:1], in1=ss[b][:, 1:2], op=ALU.add)
                    rstd_from_ss(rs[b][:], rs[b][:], D, [f"rs{b}"], [f"rs{b}"])
                    for n in range(2):
                        pi = 4 + n
                        op("dve", "scalar_tensor_tensor", [f"pf{pi}", f"rs{b}", "gpost"], [f"yn{b}_{n}"], out=yn[b][:, n * 512:(n + 1) * 512], in0=PF[pi][:],
                           scalar=rs[b][:], in1=gpost[:, n * 512:(n + 1) * 512], op0=ALU.mult, op1=ALU.mult)
                    op("pool", "tensor_tensor", [f"yn{b}_0", f"yn{b}_1", f"xt{b}"], [f"yn{b}"], out=yn[b][:], in0=yn[b][:], in1=xt[b][:], op=ALU.add)
                    dma("pool", dst[rows, :], yn[b][:], [f"yn{b}"], [], f"o_yn{b}")
            S_.barrier()

        for l in range(depth):
            xsrc = x_in if l == 0 else xs
            import os
            PH = os.environ.get("PHASES", "A,GLA,GDN,MOBA,C1,C2").split(",")
            if "A" in PH: phase_A(l, xsrc)
            if "GLA" in PH: phase_GLA(l)
            if "GDN" in PH: phase_GDN(l)
            if "MOBA" in PH: phase_MOBA(l)
            if "C1" in PH: phase_C1(l, xsrc)
            if "C2" in PH: phase_C2(l, y_out if l == depth - 1 else xs)

        semkeys = S_.emit()
        sems = [es.enter_context(nc.semaphore(f"s{i}")) for i in range(len(semkeys))]
        block = es.enter_context(nc.Block())
        S_.run(semkeys, sems, block)
    return nc


def _consts(S):
    j = np.arange(128)[:, None]
    i = np.arange(128)[None, :]
    c = {}
    c["c_identb"] = np.eye(128, dtype=np.float32).astype(BF)
    c["c_identf"] = np.eye(128, dtype=np.float32)
    c["c_masku"] = (j <= i).astype(np.float32)
    c["c_negu"] = np.where(j <= i, 0.0, -BIG).astype(np.float32)
    c["c_posl"] = np.where(i < j, 0.0, BIG).astype(np.float32)
    c["c_triu"] = (j <= i).astype(np.float32)
    c["c_trisu"] = (j > i).astype(np.float32)
    c["c_bones"] = ((j // 64) == (i // 64)).astype(np.float32)
    half = 32
    inv = (np.float32(10000.0) ** (-np.arange(half, dtype=np.float32) / np.float32(half))).astype(np.float32)
    pos = np.arange(S, dtype=np.float32)
    ang = (pos[:, None] * inv[None, :]).astype(np.float32)
    cos = np.cos(ang).astype(np.float32).T
    sin = np.sin(ang).astype(np.float32).T
    p = np.arange(128)
    cosP = cos[p % 32]
    sinP = sin[p % 32] * np.where((p % 64) < 32, -1.0, 1.0)[:, None]
    c["c_rope"] = np.stack([cosP * 0.125, sinP * 0.125, cosP, sinP]).astype(np.float32)
    c["c_onehot"] = (np.arange(64)[:, None] == (np.arange(S)[None, :] // 256)).astype(np.float32).astype(BF)
    own = np.arange(32)[:, None]
    jb = np.arange(32)[None, :]
    mbt = np.where(jb < own, 0.0, np.where(jb == own, BIG, -BIG)).astype(np.float32)
    c["c_mb"] = np.broadcast_to(mbt.reshape(1, 32 * 32), (128, 32 * 32)).copy()
    cm = np.zeros((4, 128, 512), np.float32)
    key = np.arange(128)[:, None]
    q = np.arange(512)[None, :]
    for r in range(4):
        cm[r] = ((r * 128 + key) <= q).astype(np.float32)
    c["c_cm"] = cm.astype(BF)
    return c


def _prep_weights(inp, depth):
    w = {}
    w["win"] = np.ascontiguousarray(inp["w_in"][:depth][:, :, WIN_IDX]).astype(BF)
    wga = np.zeros((depth, 33, 256), np.float32)
    wga[:, 0:16, :] = inp["gla_w_gate"][:depth]
    wga[:, 32, :] = inp["gla_b_gate"][:depth]
    w["wga"] = wga.astype(BF)
    w["wo"] = np.asarray(inp["w_o"][:depth]).astype(BF)
    w["wg"] = np.asarray(inp["ffn_w_gate"][:depth]).astype(BF)
    w["wu"] = np.asarray(inp["ffn_w_up"][:depth]).astype(BF)
    w["wd"] = np.asarray(inp["ffn_w_down"][:depth]).astype(BF)
    w["gains"] = np.stack([inp["norm_mix_pre"][:depth], inp["norm_mix_post"][:depth], inp["norm_ffn_pre"][:depth],
                           inp["norm_ffn_post"][:depth]], axis=1).astype(np.float32)
    w["hn"] = np.stack([inp["gla_norm"][:depth], inp["gdn_norm"][:depth]], axis=1).astype(np.float32)
    w["conv"] = np.ascontiguousarray(np.transpose(inp["gdn_conv"][:depth], (0, 2, 1))).astype(np.float32)
    w["gsc"] = np.stack([inp["gdn_a_log"][:depth], inp["gdn_dt_bias"][:depth]], axis=1).astype(np.float32)
    return w


def run(inputs, n_cores=8, dbg=False, depth=None):
    inp = {k: np.asarray(v) for k, v in inputs.items()}
    x = inp["x"]
    B, S, _ = x.shape
    depth = depth or inp["w_in"].shape[0]
    nc = build(S, depth, dbg=dbg)
    shared = {}
    shared.update(_consts(S))
    shared.update(_prep_weights(inp, depth))
    in_maps = []
    for c in range(n_cores):
        m = dict(shared)
        m["x"] = np.ascontiguousarray(x[c % B]).astype(np.float32)
        in_maps.append(m)
    res = run_bass_kernel_spmd(nc, in_maps, core_ids=list(range(n_cores)))
    return res


def kernel(**inputs):
    res = run(inputs)
    B = np.asarray(inputs["x"]).shape[0]
    out = np.stack([res.results[b]["y"] for b in range(B)], axis=0)
    return out.astype(np.float32)
```

```python
import numpy as np
import ml_dtypes
import concourse.bass as bass
import concourse.mybir as mybir
from concourse.bass_utils import run_bass_kernel_spmd
from contextlib import ExitStack

F32 = mybir.dt.float32
BF16 = mybir.dt.bfloat16
AF = mybir.ActivationFunctionType
ALU = mybir.AluOpType
AX = mybir.AxisListType
BF = ml_dtypes.bfloat16

D = 1024
DFF = 2816
NFC = DFF // 128
BIG = 30000.0
ENGS = ("pe", "act", "dve", "pool", "sp")


class _Op:
    __slots__ = ("eng", "fn", "deps", "dma", "chan", "idx", "sig", "waits", "need")

    def __init__(self, eng, fn, dma, chan, idx):
        self.eng, self.fn, self.dma, self.chan, self.idx = eng, fn, dma, chan, idx
        self.deps = ()
        self.sig = None
        self.need = False
        self.waits = ()


class Sched:
    def __init__(self):
        self.ops = []
        self.lastw = {}
        self.readers = {}
        self.last_on_eng = {}
        self.last_on_chan = {}

    @staticmethod
    def _norm(keys):
        return [k[:3] if (k[:2] in ("pf", "pb") and len(k) > 2 and k[2].isdigit()) else k for k in keys]

    def add(self, eng, fn, reads=(), writes=(), dma=False, chan=None):
        reads = self._norm(reads)
        writes = self._norm(writes)
        op = _Op(eng, fn, dma, chan if dma else None, len(self.ops))
        deps = set()
        for k in reads:
            w = self.lastw.get(k)
            if w is not None:
                deps.add(w)
        for k in writes:
            w = self.lastw.get(k)
            if w is not None:
                deps.add(w)
            for r in self.readers.get(k, ()):
                deps.add(r)
        op.deps = deps
        for k in reads:
            self.readers.setdefault(k, []).append(op.idx)
        for k in writes:
            self.lastw[k] = op.idx
            self.readers[k] = []
        self.ops.append(op)
        if dma:
            self.last_on_chan[chan] = op.idx
        else:
            self.last_on_eng[eng] = op.idx
        return op

    def barrier(self):
        allprev = set(self.last_on_eng.values()) | set(self.last_on_chan.values())
        for e in ENGS:
            op = _Op(e, None, False, None, len(self.ops))
            op.deps = set(allprev)
            self.ops.append(op)
            self.last_on_eng[e] = op.idx
        self.lastw.clear()
        self.readers.clear()

    def emit(self):
        ops = self.ops
        for op in ops:
            for d in op.deps:
                ops[d].need = True
        cnt = {}
        for op in ops:
            if op.fn is None:
                continue
            if op.dma:
                key = ("chan", op.chan)
                cnt[key] = cnt.get(key, 0) + 16
                op.sig = (key, cnt[key])
            elif op.need:
                key = ("eng", op.eng)
                cnt[key] = cnt.get(key, 0) + 1
                op.sig = (key, cnt[key])
        water = {e: {} for e in ENGS}
        for op in ops:
            need = {}
            for d in op.deps:
                dop = ops[d]
                if dop.sig is None:
                    continue
                k, v = dop.sig
                if need.get(k, 0) < v:
                    need[k] = v
            wm = water[op.eng]
            waits = []
            for k, v in need.items():
                if wm.get(k, 0) < v:
                    wm[k] = v
                    waits.append((k, v))
            op.waits = waits
        return sorted(cnt.keys())

    def run(self, semkeys, sems, block):
        ops = self.ops
        semmap = dict(zip(semkeys, sems))

        def stream(engname):
            def body(e):
                for op in ops:
                    if op.eng != engname:
                        continue
                    for k, v in op.waits:
                        e.wait_ge(semmap[k], v)
                    if op.fn is None:
                        continue
                    ins = op.fn(e)
                    if op.sig is not None:
                        ins.then_inc(semmap[op.sig[0]], 16 if op.dma else 1)
            return body

        block.tensor(stream("pe"))
        block.scalar(stream("act"))
        block.vector(stream("dve"))
        block.gpsimd(stream("pool"))
        block.sync(stream("sp"))


def _win_cols():
    o = {}
    idx = []
    base = {"gq": 0, "gk": 256, "gv": 512, "gz": 768, "ga": 1024, "dq": 1040, "dk": 1296, "dv": 1552,
            "dz": 1808, "db": 2064, "da": 2068, "mq": 2072, "mk": 2584, "mv": 3096}

    def put(name, cols):
        o[name] = len(idx)
        idx.extend(cols)

    put("gq", range(base["gq"], base["gq"] + 256))
    put("gk", range(base["gk"], base["gk"] + 256))
    put("gv", range(base["gv"], base["gv"] + 256))
    put("ga", range(base["ga"], base["ga"] + 16))
    put("dq", range(base["dq"], base["dq"] + 256))
    put("dk", range(base["dk"], base["dk"] + 256))
    put("dv", range(base["dv"], base["dv"] + 256))
    put("dba", range(base["db"], base["db"] + 8))
    swap = lambda b: [b + h * 64 + (d + 32) % 64 for h in range(8) for d in range(64)]
    put("mq", range(base["mq"], base["mq"] + 512))
    put("mqs", swap(base["mq"]))
    put("mk", range(base["mk"], base["mk"] + 512))
    put("mks", swap(base["mk"]))
    put("z", list(range(base["gz"], base["gz"] + 256)) + list(range(base["dz"], base["dz"] + 256)))
    put("mv", range(base["mv"], base["mv"] + 512))
    return np.array(idx, dtype=np.int64), o


WIN_IDX, WOFF = _win_cols()
NCOL = len(WIN_IDX)


def build(S, depth, dbg=False):
    NT = S // 128
    NG = S // 512
    NB = S // 256
    nc = bass.Bass("TRN2", target_bir_lowering=False)
    sc_kind = "ExternalOutput" if dbg else "Internal"

    def din(name, shape, dt):
        return nc.dram_tensor(name, shape, dt, kind="ExternalInput").ap()

    def dscr(name, shape, dt):
        return nc.dram_tensor(name, shape, dt, kind=sc_kind).ap()

    x_in = din("x", [S, D], F32)
    win_d = din("win", [depth, D, NCOL], BF16)
    wga_d = din("wga", [depth, 33, 256], BF16)
    wo_d = din("wo", [depth, D, D], BF16)
    wg_d = din("wg", [depth, D, DFF], BF16)
    wu_d = din("wu", [depth, D, DFF], BF16)
    wd_d = din("wd", [depth, DFF, D], BF16)
    gains_d = din("gains", [depth, 4, D], F32)
    hn_d = din("hn", [depth, 2, 64], F32)
    conv_d = din("conv", [depth, 768, 4], F32)
    gsc_d = din("gsc", [depth, 2, 4], F32)
    c_identb = din("c_identb", [128, 128], BF16)
    c_identf = din("c_identf", [128, 128], F32)
    c_masku = din("c_masku", [128, 128], F32)
    c_negu = din("c_negu", [128, 128], F32)
    c_posl = din("c_posl", [128, 128], F32)
    c_triu = din("c_triu", [128, 128], F32)
    c_trisu = din("c_trisu", [128, 128], F32)
    c_bones = din("c_bones", [128, 128], F32)
    c_rope = din("c_rope", [4, 128, S], F32)
    c_onehot = din("c_onehot", [64, S], BF16)
    c_mb = din("c_mb", [128, 32 * 32], F32)
    c_cm = din("c_cm", [4, 128, 512], BF16)
    y_out = nc.dram_tensor("y", [S, D], F32, kind="ExternalOutput").ap()

    xs = dscr("xs", [S, D], F32)
    x1s = dscr("x1s", [S, D], F32)
    h2s = dscr("h2s", [S, D], BF16)
    glaT = dscr("glaT", [1024, S], F32)
    gdnT = dscr("gdnT", [768, S], BF16)
    gsct = dscr("gsct", [S, 24], F32)
    mqT = dscr("mqT", [512, S], BF16)
    mkT = dscr("mkT", [512, S], BF16)
    mksum = dscr("mksum", [512, 32], F32)
    mvs = dscr("mvs", [S, 512], BF16)
    zs = dscr("zs", [S, 512], F32)
    mix = dscr("mix", [S, D], BF16)

    S_ = Sched()
    uid = [0]
    ARENA_BASE = 16640
    ARENA_CAP = 226000
    arena = [ARENA_BASE]

    def sb(name, shape, dt):
        nbytes = int(np.prod(shape[1:])) * (4 if dt == F32 else 2)
        nbytes = (nbytes + 63) // 64 * 64
        off = arena[0]
        arena[0] += nbytes
        assert arena[0] <= ARENA_CAP, (name, arena[0])
        uid[0] += 1
        return nc.alloc_sbuf_tensor_at(f"{name}_{uid[0]}", shape, dt, offset=off)

    es = ExitStack()
    with es:
        PF = [es.enter_context(nc.psum_tensor(f"pf{i}", [128, 512], F32)) for i in range(6)]
        PB = [es.enter_context(nc.psum_tensor(f"pb{i}", [128, 1024], BF16)) for i in range(2)]

        def op(eng, meth, reads, writes, **kw):
            S_.add(eng, lambda e, m=meth, k=kw: getattr(e, m)(**k), reads=reads, writes=writes)

        def mmg(items, reads, writes):
            def f(e, items=items):
                n = len(items)
                for i, (o, l, r) in enumerate(items):
                    ins = e.matmul(out=o, lhsT=l, rhs=r, start=(i == 0), stop=(i == n - 1))
                return ins
            S_.add("pe", f, reads=reads, writes=writes)

        def trg(items, reads, writes):
            def f(e, items=items):
                for (o, i_, idn) in items:
                    ins = e.transpose(out=o, in_=i_, identity=idn)
                return ins
            S_.add("pe", f, reads=reads, writes=writes)

        def dma(q, out, in_, reads, writes, chan, slow=False):
            if slow:
                S_.add(q, lambda e, o=out, i=in_: e.dma_start(out=o, in_=i, allow_slow_non_contiguous=True),
                       reads=reads, writes=writes, dma=True, chan=chan)
            else:
                S_.add(q, lambda e, o=out, i=in_: e.dma_start(out=o, in_=i), reads=reads, writes=writes, dma=True, chan=chan)

        identb = sb("identb", [128, 128], BF16)
        identf = sb("identf", [128, 128], F32)
        epsc = sb("epsc", [128, 1], F32)
        onec = sb("onec", [128, 1], F32)
        dma("sp", identb[:], c_identb, [], ["identb"], "c0")
        dma("sp", identf[:], c_identf, [], ["identf"], "c1")
        op("dve", "memset", [], ["epsc"], ap=epsc[:], constant=1e-6)
        op("dve", "memset", [], ["onec"], ap=onec[:], constant=1.0)
        GLOBAL_TOP = arena[0]

        def rstd_from_ss(ss_ap, out_ap, n, rk, wk):
            op("act", "activation", rk + ["epsc"], wk, out=out_ap, in_=ss_ap, func=AF.Ln, scale=1.0 / n, bias=epsc[:ss_ap.shape[0], :])
            op("act", "activation", wk, wk, out=out_ap, in_=out_ap, func=AF.Exp, scale=-0.5)

        def phase_A(l, xsrc):
            arena[0] = GLOBAL_TOP
            win = sb("win", [128, 8, NCOL], BF16)
            wga = sb("wga", [33, 256], BF16)
            gpre = sb("gpre", [128, D], F32)
            convw = sb("convw", [128, 6, 4], F32)
            dtb = sb("dtb", [128, 4], F32)
            nA = sb("nA", [128, 4], F32)
            triu = sb("triu", [128, 128], F32)
            trisu = sb("trisu", [128, 128], F32)
            bones = sb("bones", [128, 128], F32)
            for c in range(8):
                dma("sp" if c % 2 == 0 else "pool", win[:, c, :], win_d[l, c * 128:(c + 1) * 128, :], [], [f"win{c}"], f"w{c}")
            WIN = [f"win{c}" for c in range(8)]
            dma("sp", wga[:], wga_d[l], [], ["wga"], "c2")
            dma("sp", gpre[:], gains_d[l, 0:1, :].broadcast_to([128, D]), [], ["gpre"], "c3")
            dma("sp", convw[:], conv_d[l].rearrange("(g p) k -> p g k", p=128), [], ["convw"], "c4")
            dma("sp", dtb[:], gsc_d[l, 1:2, :].broadcast_to([128, 4]), [], ["dtb"], "c5")
            dma("sp", nA[:], gsc_d[l, 0:1, :].broadcast_to([128, 4]), [], ["nA"], "c6")
            dma("sp", triu[:], c_triu, [], ["triu"], "c7")
            dma("sp", trisu[:], c_trisu, [], ["trisu"], "c8")
            dma("sp", bones[:], c_bones, [], ["bones"], "c9")
            op("act", "activation", ["nA"], ["nA"], out=nA[:], in_=nA[:], func=AF.Exp)
            op("dve", "tensor_scalar", ["nA"], ["nA"], out=nA[:], in0=nA[:], scalar1=-1.0, scalar2=None, op0=ALU.mult)

            xt = [sb(f"xt{i}", [128, D], F32) for i in range(2)]
            sq = sb("sq", [128, D], F32)
            hb = [sb(f"hb{i}", [128, D], BF16) for i in range(2)]
            ss = [sb(f"ss{i}", [128, 1], F32) for i in range(2)]
            rs = [sb(f"rs{i}", [128, 1], F32) for i in range(2)]
            hT = [sb(f"hT{i}", [128, 8, 512], BF16) for i in range(2)]
            gaT = sb("gaT", [33, 512], BF16)
            op("dve", "memset", [], ["gaT"], ap=gaT[:], constant=0.0)
            op("dve", "memset", ["gaT"], ["gaT"], ap=gaT[32:33, :], constant=1.0)
            stf = [sb(f"stf{i}", [128, 512], F32) for i in range(3)]
            stb = [sb(f"stb{i}", [128, 512], BF16) for i in range(3)]
            xc = [sb(f"xc{i}", [128, 3 + 512], F32) for i in range(6)]
            cy = [sb(f"cy{i}", [128, 512], F32) for i in range(2)]
            ce = [sb(f"ce{i}", [128, 512], F32) for i in range(2)]
            rope_t = [sb(f"rope{i}", [128, 512], F32) for i in range(4)]
            r1 = [sb(f"r1{i}", [128, 512], F32) for i in range(2)]
            r2 = [sb(f"r2{i}", [128, 512], F32) for i in range(2)]
            ksum = sb("ksum", [128, 4, 32], F32)
            sc = [sb(f"sc{i}", [128, 24], F32) for i in range(2)]
            sct = [sb(f"sct{i}", [128, 8], F32) for i in range(2)]
            op("pool", "memset", [], ["ksum"], ap=ksum[:], constant=0.0)
            for i in range(6):
                op("pool", "memset", [], [f"xc{i}"], ap=xc[i][:, 0:3], constant=0.0)
            nst = [0, 0]
            pfi = [0]

            def nextpf():
                pfi[0] = (pfi[0] + 1) % 4
                return pfi[0], PF[pfi[0]], f"pf{pfi[0]}"

            def load_norm(G):
                hTb = hT[G % 2]
                for tt in range(4):
                    t = G * 4 + tt
                    b = t % 2
                    dma("sp", xt[b][:], xsrc[t * 128:(t + 1) * 128, :], [], [f"xt{b}"], f"xt{b}")
                    op("act", "activation", [f"xt{b}"], ["sq", f"ss{b}"], out=sq[:], in_=xt[b][:], func=AF.Square, accum_out=ss[b][:])
                    rstd_from_ss(ss[b][:], rs[b][:], D, [f"ss{b}"], [f"rs{b}"])
                    op("dve", "scalar_tensor_tensor", [f"xt{b}", f"rs{b}", "gpre"], [f"hb{b}"], out=hb[b][:], in0=xt[b][:],
                       scalar=rs[b][:], in1=gpre[:], op0=ALU.mult, op1=ALU.mult)
                    for half in range(2):
                        trg([(PB[half][:, c * 128:(c + 1) * 128], hb[b][:, (half * 4 + c) * 128:(half * 4 + c + 1) * 128], identb[:])
                             for c in range(4)], [f"hb{b}", "identb"], [f"pb{half}"])
                        dst = hTb[:, half * 4:(half + 1) * 4, tt * 128:(tt + 1) * 128]
                        src = PB[half][:, 0:512].rearrange("p (c t) -> p c t", c=4)
                        if half == 0:
                            op("act", "copy", [f"pb{half}"], [f"hT{G % 2}_{tt}"], out=dst, in_=src)
                        else:
                            op("dve", "tensor_copy", [f"pb{half}"], [f"hT{G % 2}_{tt}"], out=dst, in_=src)

            def fm(G, coff, ncols=128):
                i, p, pk = nextpf()
                hTb = hT[G % 2]
                mmg([(p[0:ncols, :], win[:, c, coff:coff + ncols], hTb[:, c, :]) for c in range(8)],
                    WIN + [f"hT{G % 2}_{tt}" for tt in range(4)], [pk])
                return p, pk

            def stage_f():
                nst[0] = (nst[0] + 1) % 3
                return stf[nst[0]], f"stf{nst[0]}"

            def stage_b():
                nst[1] = (nst[1] + 1) % 3
                return stb[nst[1]], f"stb{nst[1]}"

            for G in range(NG):
                g0 = G * 512
                load_norm(G)
                hk = [f"hT{G % 2}_{tt}" for tt in range(4)]
                for j in range(6):
                    p, pk = fm(G, WOFF["gq"] + j * 128)
                    st, sk = stage_f()
                    op("act", "copy", [pk], [sk], out=st[:], in_=p[:])
                    dma("pool", glaT[j * 128:(j + 1) * 128, g0:g0 + 512], st[:], [sk], [], "o_" + sk)
                p, pk = fm(G, WOFF["ga"], 16)
                op("act", "copy", [pk], ["gaT"], out=gaT[0:16, :], in_=p[0:16, :])
                for j in range(2):
                    i, p2, pk2 = nextpf()
                    mmg([(p2[:], wga[:, j * 128:(j + 1) * 128], gaT[:, :])], ["wga", "gaT"], [pk2])
                    st, sk = stage_f()
                    op("act", "activation", [pk2], [sk], out=st[:], in_=p2[:], func=AF.Exp, scale=-1.0)
                    op("act", "activation", [sk, "onec"], [sk], out=st[:], in_=st[:], func=AF.Ln, bias=onec[:])
                    dma("pool", glaT[768 + j * 128:768 + (j + 1) * 128, g0:g0 + 512], st[:], [sk], [], "o_" + sk)
                for tt in range(4):
                    t = G * 4 + tt
                    b = t % 2
                    i, p, pk = nextpf()
                    mmg([(p[:, 0:8], hT[G % 2][:, c, tt * 128:(tt + 1) * 128], win[:, c, WOFF["dba"]:WOFF["dba"] + 8]) for c in range(8)],
                        WIN + hk, [pk])
                    scb, sck, stt_, stk = sc[b], f"sc{b}", sct[b], f"sct{b}"
                    op("act", "activation", [pk], [stk], out=stt_[:, 0:4], in_=p[:, 0:4], func=AF.Exp, scale=-1.0)
                    op("dve", "tensor_scalar", [stk], [stk], out=stt_[:, 0:4], in0=stt_[:, 0:4], scalar1=1.0, scalar2=None, op0=ALU.add)
                    op("dve", "reciprocal", [stk], [sck], out=scb[:, 4:8], in_=stt_[:, 0:4])
                    op("dve", "tensor_tensor", [pk, "dtb"], [stk], out=stt_[:, 4:8], in0=p[:, 4:8], in1=dtb[:], op=ALU.add)
                    op("act", "activation", [stk], [stk], out=stt_[:, 4:8], in_=stt_[:, 4:8], func=AF.Exp)
                    op("act", "activation", [stk, "onec"], [stk], out=stt_[:, 4:8], in_=stt_[:, 4:8], func=AF.Ln, bias=onec[:])
                    op("dve", "tensor_tensor", [stk, "nA"], [stk], out=stt_[:, 4:8], in0=stt_[:, 4:8], in1=nA[:], op=ALU.mult)
                    i2, pc, pck = nextpf()
                    mmg([(pc[:, 0:4], triu[:], stt_[:, 4:8])], ["triu", stk], [pck])
                    op("act", "copy", [pck], [sck], out=scb[:, 0:4], in_=pc[:, 0:4])
                    op("act", "activation", [pck], [sck], out=scb[:, 8:12], in_=pc[:, 0:4], func=AF.Exp)
                    op("dve", "tensor_scalar", [pck], [sck], out=scb[:, 20:24], in0=pc[:, 0:4], scalar1=-1.0, scalar2=None, op0=ALU.mult)
                    i3, pr, prk = nextpf()
                    mmg([(pr[:, 0:4], trisu[:], stt_[:, 4:8])], ["trisu", stk], [prk])
                    op("act", "activation", [prk], [sck], out=scb[:, 16:20], in_=pr[:, 0:4], func=AF.Exp)
                    op("dve", "tensor_tensor", [sck], [sck], out=scb[:, 12:16], in0=scb[:, 4:8], in1=scb[:, 8:12], op=ALU.mult)
                    dma("pool", gsct[t * 128:(t + 1) * 128, :], scb[:], [sck], [], "o_" + sck)
                for j in range(6):
                    p, pk = fm(G, WOFF["dq"] + j * 128)
                    xk = f"xc{j}"
                    op("act", "copy", [pk], [xk], out=xc[j][:, 3:515], in_=p[:])
                    b = j % 2
                    cyb, cyk, ceb, cek = cy[b], f"cy{b}", ce[b], f"ce{b}"
                    op("dve", "tensor_scalar", [xk, "convw"], [cyk], out=cyb[:], in0=xc[j][:, 3:515], scalar1=convw[:, j, 3:4], scalar2=None, op0=ALU.mult)
                    for i in range(3):
                        op("dve", "scalar_tensor_tensor", [xk, "convw", cyk], [cyk], out=cyb[:], in0=xc[j][:, i:i + 512],
                           scalar=convw[:, j, i:i + 1], in1=cyb[:], op0=ALU.mult, op1=ALU.add)
                    op("pool", "tensor_copy", [xk], [xk], out=xc[j][:, 0:3], in_=xc[j][:, 512:515])
                    op("act", "activation", [cyk], [cek], out=ceb[:], in_=cyb[:], func=AF.Exp, scale=-1.0)
                    op("pool", "tensor_scalar", [cek], [cek], out=ceb[:], in0=ceb[:], scalar1=1.0, scalar2=None, op0=ALU.add)
                    op("dve", "reciprocal", [cek], [cek], out=ceb[:], in_=ceb[:])
                    op("dve", "tensor_tensor", [cek, cyk], [cyk], out=cyb[:], in0=cyb[:], in1=ceb[:], op=ALU.mult)
                    st, sk = stage_b()
                    if j < 4:
                        op("pool", "tensor_tensor", [cyk], [cek], out=ceb[:], in0=cyb[:], in1=cyb[:], op=ALU.mult)
                        i, pn, pnk = nextpf()
                        mmg([(pn[:], bones[:], ceb[:])], ["bones", cek], [pnk])
                        op("act", "activation", [pnk, "epsc"], [cek], out=ceb[:], in_=pn[:], func=AF.Ln, bias=epsc[:])
                        op("act", "activation", [cek], [cek], out=ceb[:], in_=ceb[:], func=AF.Exp, scale=-0.5)
                        op("dve", "scalar_tensor_tensor", [cyk, cek], [sk], out=st[:], in0=cyb[:], scalar=(0.125 if j < 2 else 1.0),
                           in1=ceb[:], op0=ALU.mult, op1=ALU.mult)
                    else:
                        op("dve", "tensor_copy", [cyk], [sk], out=st[:], in_=cyb[:])
                    dma("pool", gdnT[j * 128:(j + 1) * 128, g0:g0 + 512], st[:], [sk], [], "o_" + sk)
                for tbl in range(4):
                    dma("sp", rope_t[tbl][:], c_rope[tbl, :, g0:g0 + 512], [], [f"rope{tbl}"], f"rope{tbl}")
                for isk in range(2):
                    for j in range(4):
                        p, pk = fm(G, WOFF["mk" if isk else "mq"] + j * 128)
                        b = j % 2
                        op("dve", "tensor_tensor", [pk, f"rope{2 * isk}"], [f"r1{b}"], out=r1[b][:], in0=p[:], in1=rope_t[2 * isk][:], op=ALU.mult)
                        p2, pk2 = fm(G, WOFF["mks" if isk else "mqs"] + j * 128)
                        op("dve", "tensor_tensor", [pk2, f"rope{2 * isk + 1}"], [f"r2{b}"], out=r2[b][:], in0=p2[:], in1=rope_t[2 * isk + 1][:], op=ALU.mult)
                        op("pool", "tensor_tensor", [f"r1{b}", f"r2{b}"], [f"r1{b}"], out=r1[b][:], in0=r1[b][:], in1=r2[b][:], op=ALU.add)
                        st, sk = stage_b()
                        op("act", "copy", [f"r1{b}"], [sk], out=st[:], in_=r1[b][:])
                        dst = (mkT if isk else mqT)
                        dma("pool", dst[j * 128:(j + 1) * 128, g0:g0 + 512], st[:], [sk], [], "o_" + sk)
                        if isk:
                            op("dve", "tensor_reduce", [f"r1{b}"], ["ksum"], out=ksum[:, j, 2 * G:2 * G + 2],
                               in_=r1[b][:].rearrange("p (n k) -> p n k", n=2), axis=AX.X, op=ALU.add)
                for tt in range(4):
                    t = G * 4 + tt
                    lhs = lambda c, tt=tt: hT[G % 2][:, c, tt * 128:(tt + 1) * 128]
                    i, p, pk = nextpf()
                    mmg([(p[:], lhs(c), win[:, c, WOFF["z"]:WOFF["z"] + 512]) for c in range(8)], WIN + hk, [pk])
                    st, sk = stage_f()
                    b = tt % 2
                    op("act", "activation", [pk], [f"ce{b}"], out=ce[b][:], in_=p[:], func=AF.Exp, scale=-1.0)
                    op("pool", "tensor_scalar", [f"ce{b}"], [f"ce{b}"], out=ce[b][:], in0=ce[b][:], scalar1=1.0, scalar2=None, op0=ALU.add)
                    op("dve", "reciprocal", [f"ce{b}"], [f"ce{b}"], out=ce[b][:], in_=ce[b][:])
                    op("dve", "tensor_tensor", [f"ce{b}", pk], [sk], out=st[:], in0=p[:], in1=ce[b][:], op=ALU.mult)
                    dma("pool", zs[t * 128:(t + 1) * 128, :], st[:], [sk], [], "o_" + sk)
                    i, p, pk = nextpf()
                    mmg([(p[:], lhs(c), win[:, c, WOFF["mv"]:WOFF["mv"] + 512]) for c in range(8)], WIN + hk, [pk])
                    st, sk = stage_b()
                    op("act", "copy", [pk], [sk], out=st[:], in_=p[:])
                    dma("pool", mvs[t * 128:(t + 1) * 128, :], st[:], [sk], [], "o_" + sk)
            for j in range(4):
                dma("pool", mksum[j * 128:(j + 1) * 128, :], ksum[:, j, :], ["ksum"], [], "o_ksum")
            S_.barrier()

        def head_out_stage(t, O, Ok, col0, zcol0, normw, zt, ztk, tmp, tmpk, ms, msk, outb, outbk, chan):
            dma("sp", zt[:], zs[t * 128:(t + 1) * 128, zcol0:zcol0 + 256], [], [ztk], "zt" + chan)
            Okl = Ok if isinstance(Ok, list) else [Ok]
            op("act", "activation", Okl, [tmpk], out=tmp[:], in_=O[:], func=AF.Square)
            op("dve", "tensor_reduce", [tmpk], [msk], out=ms[:], in_=tmp[:].rearrange("p (h d) -> p h d", h=4), axis=AX.X, op=ALU.add)
            rstd_from_ss(ms[:], ms[:], 64, [msk], [msk])
            op("dve", "tensor_tensor", Okl + [msk], [tmpk], out=tmp[:].rearrange("p (h d) -> p h d", h=4),
               in0=O[:].rearrange("p (h d) -> p h d", h=4), in1=ms[:].unsqueeze(2).to_broadcast([128, 4, 64]), op=ALU.mult)
            op("pool", "tensor_tensor", [ztk, "normw"], [ztk], out=zt[:].rearrange("p (h d) -> p h d", h=4),
               in0=zt[:].rearrange("p (h d) -> p h d", h=4), in1=normw[:].unsqueeze(1).to_broadcast([128, 4, 64]), op=ALU.mult)
            op("dve", "tensor_tensor", [tmpk, ztk], [outbk], out=outb[:], in0=tmp[:], in1=zt[:], op=ALU.mult)
            dma("pool", mix[t * 128:(t + 1) * 128, col0:col0 + 256], outb[:], [outbk], [], "o_" + chan)

        def phase_GLA(l):
            arena[0] = GLOBAL_TOP
            masku = sb("masku", [128, 128], F32)
            normw = sb("normw", [128, 64], F32)
            dma("sp", masku[:], c_masku, [], ["masku"], "c2")
            dma("sp", normw[:], hn_d[l, 0:1, :].broadcast_to([128, 64]), [], ["normw"], "c3")
            inT = [[sb(f"gin{b}_{k}", [128, 2, 128], F32) for k in range(4)] for b in range(2)]
            ones = sb("ones", [128, 128], F32)
            op("dve", "memset", [], ["ones"], ap=ones[:], constant=1.0)
            cum = [sb(f"cum{g}", [128, 128], F32) for g in range(2)]
            nb = [sb(f"nb{g}", [128, 1], F32) for g in range(2)]
            E = [[sb(f"E{g}_{k}", [128, 128], F32) for k in range(3)] for g in range(2)]
            qt = [sb(f"qt{g}", [128, 128], BF16) for g in range(2)]
            kt = [sb(f"kt{g}", [128, 128], BF16) for g in range(2)]
            kh = [sb(f"kh{g}", [128, 128], BF16) for g in range(2)]
            vb = [sb(f"vb{g}", [128, 128], BF16) for g in range(2)]
            khT = [sb(f"khT{g}", [128, 128], BF16) for g in range(2)]
            vT = [sb(f"vT{g}", [128, 128], BF16) for g in range(2)]
            AT = [sb(f"AT{h}", [128, 128], BF16) for h in range(4)]
            St = [sb(f"St{g}", [128, 64], F32) for g in range(2)]
            Sb = [sb(f"Sb{g}", [128, 64], BF16) for g in range(2)]
            O = [sb(f"O{b}", [128, 256], F32) for b in range(2)]
            zt = [sb(f"zt{b}", [128, 256], F32) for b in range(2)]
            tmp = sb("tmp", [128, 256], F32)
            ms = sb("ms", [128, 4], F32)
            outb = [sb(f"outb{b}", [128, 256], BF16) for b in range(2)]
            for g in range(2):
                op("dve", "memset", [], [f"St{g}"], ap=St[g][:], constant=0.0)
                op("pool", "memset", [], [f"Sb{g}"], ap=Sb[g][:], constant=0.0)
            for t in range(NT):
                b = t % 2
                for k in range(4):
                    dma("sp", inT[b][k][:], glaT[k * 256:(k + 1) * 256, t * 128:(t + 1) * 128].rearrange("(g p) t -> p g t", p=128),
                        [], [f"gin{b}_{k}"], f"gin{b}_{k}")
                for g in range(2):
                    q_, k_, v_, sp_ = (inT[b][k][:, g, :] for k in range(4))
                    rk = [f"gin{b}_{k}" for k in range(4)]
                    ck = f"cum{g}"
                    op("dve", "tensor_tensor_scan", [rk[3], "ones"], [ck], out=cum[g][:], data0=ones[:], data1=sp_, initial=0.0,
                       op0=ALU.mult, op1=ALU.add)
                    op("dve", "tensor_scalar", [ck], [f"nb{g}"], out=nb[g][:], in0=cum[g][:, 127:128], scalar1=-1.0 / 16, scalar2=None, op0=ALU.mult)
                    op("act", "activation", [ck], [f"E{g}_0"], out=E[g][0][:], in_=cum[g][:], func=AF.Exp, scale=-1.0 / 16)
                    op("act", "activation", [ck], [f"E{g}_1"], out=E[g][1][:], in_=cum[g][:], func=AF.Exp, scale=1.0 / 16)
                    op("act", "activation", [ck, f"nb{g}"], [f"E{g}_2"], out=E[g][2][:], in_=cum[g][:], func=AF.Exp, scale=1.0 / 16, bias=nb[g][:])
                    op("dve", "scalar_tensor_tensor", [rk[0], f"E{g}_0"], [f"qt{g}"], out=qt[g][:], in0=q_, scalar=0.125, in1=E[g][0][:],
                       op0=ALU.mult, op1=ALU.mult)
                    op("dve", "tensor_tensor", [rk[1], f"E{g}_1"], [f"kt{g}"], out=kt[g][:], in0=k_, in1=E[g][1][:], op=ALU.mult)
                    op("pool", "tensor_tensor", [rk[1], f"E{g}_2"], [f"khT{g}"], out=khT[g][:], in0=k_, in1=E[g][2][:], op=ALU.mult)
                    op("pool", "tensor_copy", [rk[2]], [f"vT{g}"], out=vT[g][:], in_=v_)
                    trg([(PB[0][:, 0:128], khT[g][:], identb[:]), (PB[0][:, 128:256], vT[g][:], identb[:])],
                        [f"khT{g}", f"vT{g}", "identb"], ["pb0"])
                    op("act", "copy", ["pb0"], [f"kh{g}"], out=kh[g][:], in_=PB[0][:, 0:128])
                    op("act", "copy", ["pb0"], [f"vb{g}"], out=vb[g][:], in_=PB[0][:, 128:256])
                    for hh in range(2):
                        h = 2 * g + hh
                        r = slice(hh * 64, hh * 64 + 64)
                        pa, pak = PF[hh], f"pf{hh}"
                        mmg([(pa[:, 0:128], kt[g][r, :], qt[g][r, :])], [f"kt{g}", f"qt{g}"], [pak])
                        op("dve", "tensor_tensor", [pak, "masku"], [f"AT{h}"], out=AT[h][:], in0=pa[:, 0:128], in1=masku[:], op=ALU.mult)
                        mmg([(PF[2][:, h * 64:(h + 1) * 64], qt[g][r, :], Sb[g][r, :]),
                             (PF[2][:, h * 64:(h + 1) * 64], AT[h][:], vb[g][:, r])],
                            [f"qt{g}", f"Sb{g}", f"AT{h}", f"vb{g}"], [f"pf2_{h}"])
                        mmg([(PF[3][r, g * 64:(g + 1) * 64], kh[g][:, r], vb[g][:, r])], [f"kh{g}", f"vb{g}"], [f"pf3_{g}_{hh}"])
                    op("dve", "scalar_tensor_tensor", [f"St{g}", f"E{g}_0", f"pf3_{g}_0", f"pf3_{g}_1"], [f"St{g}"], out=St[g][:], in0=St[g][:],
                       scalar=E[g][0][:, 127:128], in1=PF[3][:, g * 64:(g + 1) * 64], op0=ALU.mult, op1=ALU.add)
                    op("act", "copy", [f"St{g}"], [f"Sb{g}"], out=Sb[g][:], in_=St[g][:])
                op("act", "copy", [f"pf2_{h}" for h in range(4)], [f"O{b}"], out=O[b][:], in_=PF[2][:, 0:256])
                head_out_stage(t, O[b], f"O{b}", 0, 0, normw, zt[b], f"zt{b}", tmp, "tmp", ms, "ms", outb[b], f"outb{b}", f"gla{b}")
            S_.barrier()

        def phase_GDN(l):
            arena[0] = GLOBAL_TOP
            negu = sb("negu", [128, 128], F32)
            posl = sb("posl", [128, 128], F32)
            normw = sb("normw", [128, 64], F32)
            onesr = sb("onesr", [1, 128], F32)
            dma("sp", negu[:], c_negu, [], ["negu"], "c2")
            dma("sp", posl[:], c_posl, [], ["posl"], "c3")
            dma("sp", normw[:], hn_d[l, 1:2, :].broadcast_to([128, 64]), [], ["normw"], "c4")
            op("dve", "memset", [], ["onesr"], ap=onesr[:], constant=1.0)
            egl = [sb(f"egl{g}", [128, NT], F32) for g in range(2)]
            for g in range(2):
                for hh in range(2):
                    h = 2 * g + hh
                    src = gsct.rearrange("(t p) c -> p t c", p=128)[127:128, :, 8 + h]
                    dma("sp", egl[g][hh * 64:(hh + 1) * 64, :], src.broadcast_to([64, NT]), [], [f"egl{g}"], f"egl{g}{hh}", slow=True)
            inT = [sb(f"din{b}", [128, 6, 128], BF16) for b in range(2)]
            SC = [sb(f"SC{b}", [128, 24], F32) for b in range(2)]
            kbe = [sb(f"kbe{h}", [128, 64], BF16) for h in range(4)]
            khat = [sb(f"khat{h}", [128, 64], BF16) for h in range(4)]
            vbt = [sb(f"vbt{h}", [128, 64], BF16) for h in range(4)]
            gcr = [sb(f"gcr{h}", [1, 128], F32) for h in range(4)]
            DT = [sb(f"DT{h}", [128, 128], F32) for h in range(4)]
            DL = [sb(f"DL{h}", [128, 128], F32) for h in range(4)]
            attnT = [sb(f"attnT{h}", [128, 128], BF16) for h in range(4)]
            Am = [[sb(f"Am{h}_{i}", [128, 128], BF16) for i in range(2)] for h in range(4)]
            Cm = [[sb(f"Cm{h}_{i}", [128, 128], BF16) for i in range(2)] for h in range(4)]
            Ym = [[sb(f"Ym{h}_{i}", [128, 128], BF16) for i in range(2)] for h in range(4)]
            u = [sb(f"u{h}", [128, 64], F32) for h in range(4)]
            wT = [sb(f"wT{g}", [128, 128], BF16) for g in range(2)]
            vn = [sb(f"vn{h}", [128, 64], BF16) for h in range(4)]
            o1 = [sb(f"o1{h}", [128, 64], F32) for h in range(4)]
            St = [sb(f"St{g}", [128, 64], F32) for g in range(2)]
            Sb = [sb(f"Sb{g}", [128, 64], BF16) for g in range(2)]
            O = [sb(f"O{b}", [128, 256], F32) for b in range(2)]
            zt = [sb(f"zt{b}", [128, 256], F32) for b in range(2)]
            tmp = sb("tmp", [128, 256], F32)
            ms = sb("ms", [128, 4], F32)
            outb = [sb(f"outb{b}", [128, 256], BF16) for b in range(2)]
            for g in range(2):
                op("dve", "memset", [], [f"St{g}"], ap=St[g][:], constant=0.0)
                op("pool", "memset", [], [f"Sb{g}"], ap=Sb[g][:], constant=0.0)
            pfi = [0]

            def npf():
                pfi[0] = (pfi[0] + 1) % 6
                return PF[pfi[0]], f"pf{pfi[0]}"

            for t in range(NT):
                b = t % 2
                dma("sp", inT[b][:], gdnT[:, t * 128:(t + 1) * 128].rearrange("(g p) t -> p g t", p=128), [], [f"din{b}"], f"din{b}")
                dma("sp", SC[b][:], gsct[t * 128:(t + 1) * 128, :], [], [f"SC{b}"], f"SC{b}")
                ink, sck = f"din{b}", f"SC{b}"
                col = lambda q, h: SC[b][:, 4 * q + h:4 * q + h + 1]

                def qT(h): return inT[b][(h % 2) * 64:(h % 2) * 64 + 64, 0 + h // 2, :]
                def kT(h): return inT[b][(h % 2) * 64:(h % 2) * 64 + 64, 2 + h // 2, :]
                def vT(h): return inT[b][(h % 2) * 64:(h % 2) * 64 + 64, 4 + h // 2, :]

                for h in range(4):
                    hh = h % 2
                    pbk = f"pb{hh}"
                    trg([(PB[hh][:, 0:64], kT(h), identb[hh * 64:hh * 64 + 64, hh * 64:hh * 64 + 64]),
                         (PB[hh][:, 64:128], vT(h), identb[hh * 64:hh * 64 + 64, hh * 64:hh * 64 + 64])], [ink, "identb"], [pbk])
                    op("dve", "tensor_scalar", [pbk, sck], [f"kbe{h}"], out=kbe[h][:], in0=PB[hh][:, 0:64], scalar1=col(3, h), scalar2=None, op0=ALU.mult)
                    op("act", "activation", [pbk, sck], [f"khat{h}"], out=khat[h][:], in_=PB[hh][:, 0:64], func=AF.Copy, scale=col(4, h))
                    op("act", "activation", [pbk, sck], [f"vbt{h}"], out=vbt[h][:], in_=PB[hh][:, 64:128], func=AF.Copy, scale=col(1, h))
                    p, pk = npf()
                    mmg([(p[0:1, 0:128], col(0, h), identf[:])], [sck, "identf"], [pk])
                    op("act", "copy", [pk], [f"gcr{h}"], out=gcr[h][:], in_=p[0:1, 0:128])
                    p, pk = npf()
                    mmg([(p[:, 0:128], onesr[:], gcr[h][:]), (p[:, 0:128], identf[:], negu[:])], ["onesr", f"gcr{h}", "identf", "negu"], [pk])
                    op("act", "activation", [pk, sck], [f"DT{h}"], out=DT[h][:], in_=p[:, 0:128], func=AF.Exp, bias=col(5, h))
                    p, pk = npf()
                    mmg([(p[:, 0:128], onesr[:], gcr[h][:]), (p[:, 0:128], identf[:], posl[:])], ["onesr", f"gcr{h}", "identf", "posl"], [pk])
                    op("act", "activation", [pk, sck], [f"DL{h}"], out=DL[h][:], in_=p[:, 0:128], func=AF.Exp, scale=-1.0, bias=col(0, h))
                    p, pk = npf()
                    mmg([(p[:, 0:128], kT(h), qT(h))], [ink], [pk])
                    op("dve", "tensor_tensor", [pk, f"DT{h}"], [f"attnT{h}"], out=attnT[h][:], in0=p[:, 0:128], in1=DT[h][:], op=ALU.mult)
                    p, pk = npf()
                    mmg([(p[:, 0:128], kT(h), kT(h))], [ink], [pk])
                    op("dve", "scalar_tensor_tensor", [pk, sck, f"DL{h}"], [f"Cm{h}_0"], out=Cm[h][0][:], in0=p[:, 0:128], scalar=col(1, h),
                       in1=DL[h][:], op0=ALU.mult, op1=ALU.mult)
                    trg([(PB[hh][:, 128:256], Cm[h][0][:], identb[:])], [f"Cm{h}_0", "identb"], [pbk + "b"])
                    op("act", "copy", [pbk + "b"], [f"Am{h}_0"], out=Am[h][0][:], in_=PB[hh][:, 128:256])
                    op("pool", "tensor_tensor", [f"Am{h}_0", "identb"], [f"Ym{h}_0"], out=Ym[h][0][:], in0=identb[:], in1=Am[h][0][:], op=ALU.subtract)
                for k in range(1, 7):
                    a0, a1 = (k - 1) % 2, k % 2
                    for h in range(4):
                        p, pk = npf()
                        mmg([(p[:, 0:128], Am[h][a0][:], Cm[h][a0][:])], [f"Am{h}_{a0}", f"Cm{h}_{a0}"], [pk])
                        op("act" if h % 2 == 0 else "dve", "copy" if h % 2 == 0 else "tensor_copy", [pk], [f"Cm{h}_{a1}"], out=Cm[h][a1][:], in_=p[:, 0:128])
                        if k < 6:
                            p2, pk2 = npf()
                            mmg([(p2[:, 0:128], Cm[h][a0][:], Am[h][a0][:])], [f"Am{h}_{a0}", f"Cm{h}_{a0}"], [pk2])
                            op("dve" if h % 2 == 0 else "act", "tensor_copy" if h % 2 == 0 else "copy", [pk2], [f"Am{h}_{a1}"], out=Am[h][a1][:], in_=p2[:, 0:128])
                    for h in range(4):
                        p, pk = npf()
                        mmg([(p[:, 0:128], identb[:], Ym[h][a0][:]), (p[:, 0:128], Cm[h][a1][:], Ym[h][a0][:])],
                            ["identb", f"Ym{h}_{a0}", f"Cm{h}_{a1}"], [pk])
                        op("act" if h % 2 == 0 else "dve", "copy" if h % 2 == 0 else "tensor_copy", [pk], [f"Ym{h}_{a1}"], out=Ym[h][a1][:], in_=p[:, 0:128])
                YF = 0
                for h in range(4):
                    g, hh = h // 2, h % 2
                    r = slice(hh * 64, hh * 64 + 64)
                    p, pk = npf()
                    mmg([(p[:, 0:64], Ym[h][YF][:], vbt[h][:])], [f"Ym{h}_{YF}", f"vbt{h}"], [pk])
                    op("act", "copy", [pk], [f"u{h}"], out=u[h][:], in_=p[:, 0:64])
                    p, pk = npf()
                    mmg([(p[r, 0:128], kbe[h][:], Ym[h][YF][:])], [f"Ym{h}_{YF}", f"kbe{h}"], [pk])
                    op("act", "copy", [pk], [f"wT{g}_{hh}"], out=wT[g][r, :], in_=p[r, 0:128])
                for h in range(4):
                    g, hh = h // 2, h % 2
                    r = slice(hh * 64, hh * 64 + 64)
                    p, pk = npf()
                    mmg([(p[:, 0:64], wT[g][r, :], Sb[g][r, :])], [f"wT{g}_{hh}", f"Sb{g}"], [pk])
                    op("dve", "tensor_tensor", [f"u{h}", pk], [f"vn{h}"], out=vn[h][:], in0=u[h][:], in1=p[:, 0:64], op=ALU.subtract)
                    p, pk = npf()
                    mmg([(p[:, 0:64], qT(h), Sb[g][r, :])], [ink, f"Sb{g}"], [pk])
                    op("act", "activation", [pk, sck], [f"o1{h}"], out=o1[h][:], in_=p[:, 0:64], func=AF.Copy, scale=col(2, h))
                    p, pk = npf()
                    mmg([(p[:, 0:64], attnT[h][:], vn[h][:])], [f"attnT{h}", f"vn{h}"], [pk])
                    op("dve", "tensor_tensor", [f"o1{h}", pk], [f"O{b}_{h}"], out=O[b][:, h * 64:(h + 1) * 64], in0=o1[h][:], in1=p[:, 0:64], op=ALU.add)
                for g in range(2):
                    p, pk = npf()
                    for hh in range(2):
                        h = 2 * g + hh
                        r = slice(hh * 64, hh * 64 + 64)
                        mmg([(p[r, 0:64], khat[h][:], vn[h][:])], [f"khat{h}", f"vn{h}"], [pk])
                    op("dve", "scalar_tensor_tensor", [f"St{g}", f"egl{g}", pk], [f"St{g}"], out=St[g][:], in0=St[g][:],
                       scalar=egl[g][:, t:t + 1], in1=p[:, 0:64], op0=ALU.mult, op1=ALU.add)
                    op("act", "copy", [f"St{g}"], [f"Sb{g}"], out=Sb[g][:], in_=St[g][:])
                head_out_stage(t, O[b], [f"O{b}_{h}" for h in range(4)], 256, 256, normw, zt[b], f"zt{b}", tmp, "tmp", ms, "ms", outb[b], f"outb{b}", f"gdn{b}")
            S_.barrier()


        def phase_MOBA(l):
            arena[0] = GLOBAL_TOP
            qa = [sb(f"qa{i}", [128, S], BF16) for i in range(2)]
            ka = [sb(f"ka{i}", [128, S], BF16) for i in range(2)]
            Va = [sb(f"Va{i}", [128, NT, 65], BF16) for i in range(2)]
            ksf = [sb(f"ksf{i}", [64, 32], F32) for i in range(2)]
            ksb = [sb(f"ksb{i}", [64, 32], BF16) for i in range(2)]
            mb = sb("mb", [128, 32 * 32], F32)
            cm = sb("cm", [128, 4, 512], BF16)
            dma("sp", mb[:], c_mb, [], ["mb"], "c2")
            dma("sp", cm[:], c_cm.rearrange("r p q -> p r q"), [], ["cm"], "c3")
            for i in range(2):
                dma("sp", ka[i][64:128, :], c_onehot, [], [f"ka{i}_oh"], f"c4{i}")
                dma("sp", qa[i][96:128, :], c_onehot[32:64, :], [], [f"qa{i}_z"], f"c5{i}")
                op("pool", "memset", [], [f"Va{i}_1"], ap=Va[i][:, :, 64:65], constant=1.0)
            gm = [sb(f"gm{i}", [128, 32], F32) for i in range(2)]
            top8 = [sb(f"top8{i}", [128, 8], F32) for i in range(2)]
            bia = [sb(f"bia{i}", [128, 32], BF16) for i in range(2)]
            Pt = [sb(f"Pt{i}", [128, 512], BF16) for i in range(3)]
            osb = [sb(f"osb{i}", [128, 512], F32) for i in range(2)]
            for i in range(2):
                op("pool", "memset", [], [f"osb{i}"], ap=osb[i][:], constant=0.0)
            rden = [sb(f"rden{i}", [128, 1], F32) for i in range(2)]
            ob = [sb(f"ob{i}", [128, 64], BF16) for i in range(2)]
            of = [sb(f"of{i}", [128, 65], F32) for i in range(2)]
            def load_head(h):
                i = h % 2
                dma("sp", qa[i][0:64, :], mqT[h * 64:(h + 1) * 64, :], [], [f"qa{i}"], f"qa{i}")
                dma("sp", ka[i][0:64, :], mkT[h * 64:(h + 1) * 64, :], [], [f"ka{i}"], f"ka{i}")
                dma("sp", Va[i][:, :, 0:64], mvs[:, h * 64:(h + 1) * 64].rearrange("(t p) d -> p t d", p=128), [], [f"Va{i}"], f"Va{i}")
                dma("sp", ksf[i][:], mksum[h * 64:(h + 1) * 64, :], [], [f"ksf{i}"], f"ksf{i}")
                op("dve", "tensor_copy", [f"ksf{i}"], [f"ksb{i}"], out=ksb[i][:], in_=ksf[i][:])

            def gate_tile(h, t):
                i = h % 2
                j = t % 2
                own = t // 2
                mmg([(PF[4][:, j * 32:(j + 1) * 32], qa[i][0:64, t * 128:(t + 1) * 128], ksb[i][:, :])], [f"qa{i}", f"ksb{i}"], [f"pf4_{j}"])
                op("dve", "tensor_tensor", [f"pf4_{j}", "mb"], [f"gm{j}"], out=gm[j][:], in0=PF[4][:, j * 32:(j + 1) * 32],
                   in1=mb[:, own * 32:(own + 1) * 32], op=ALU.add)
                op("dve", "max", [f"gm{j}"], [f"top8{j}"], out=top8[j][:], in_=gm[j][:])
                op("dve", "tensor_scalar", [f"top8{j}"], [f"top8{j}"], out=top8[j][:, 3:4], in0=top8[j][:, 3:4], scalar1=-BIG / 2, scalar2=None, op0=ALU.max)
                op("dve", "tensor_scalar", [f"gm{j}", f"top8{j}"], [f"gm{j}"], out=gm[j][:], in0=gm[j][:], scalar1=top8[j][:, 3:4], scalar2=BIG,
                   op0=ALU.is_ge, op1=ALU.mult)
                op("dve", "tensor_scalar", [f"gm{j}"], [f"bia{j}"], out=bia[j][:], in0=gm[j][:], scalar1=-BIG, scalar2=None, op0=ALU.add)
                trg([(PB[j][64:96, 0:128], bia[j][:], identb[:])], [f"bia{j}", "identb"], [f"pb{j}"])
                op("act", "copy", [f"pb{j}"], [f"qa{i}_b{t}"], out=qa[i][64:96, t * 128:(t + 1) * 128], in_=PB[j][64:96, 0:128])

            def out_stage(h, G):
                oi = G % 2
                op("act", "copy", [f"pf{2 + oi}"], [f"osb{oi}"], out=osb[oi][0:65, :], in_=PF[2 + oi][0:65, :])
                for tt in range(4):
                    t = G * 4 + tt
                    j = tt % 2
                    mmg([(PF[5][:, 0:128], osb[oi][:, tt * 128:(tt + 1) * 128], identf[:])], [f"osb{oi}", "identf"], ["pf5"])
                    op("dve", "tensor_copy", ["pf5"], [f"of{j}"], out=of[j][:], in_=PF[5][:, 0:65])
                    op("dve", "reciprocal", [f"of{j}"], [f"rden{j}"], out=rden[j][:], in_=of[j][:, 64:65])
                    op("dve", "tensor_scalar", [f"of{j}", f"rden{j}"], [f"ob{j}"], out=ob[j][:], in0=of[j][:, 0:64],
                       scalar1=rden[j][:], scalar2=None, op0=ALU.mult)
                    dma("sp", mix[t * 128:(t + 1) * 128, 512 + h * 64:512 + (h + 1) * 64], ob[j][:], [f"ob{j}"], [], f"o_ob{j}")

            steps = [(G, kt_) for G in range(NG) for kt_ in range(4 * G + 4)]
            NH = 8
            load_head(0)
            for t in range(NT):
                gate_tile(0, t)
            for h in range(NH):
                i = h % 2
                pend_gate = list(range(NT)) if h + 1 < NH else []
                if h + 1 < NH:
                    load_head(h + 1)

                def emit_st(sidx, i=i):
                    G, kt_ = steps[sidx]
                    pi = sidx % 2
                    qk = [f"qa{i}", f"qa{i}_z"] + [f"qa{i}_b{t}" for t in range(G * 4, G * 4 + 4)]
                    mmg([(PF[pi][:], ka[i][:, kt_ * 128:(kt_ + 1) * 128], qa[i][:, G * 512:(G + 1) * 512])],
                        [f"ka{i}", f"ka{i}_oh"] + qk, [f"pf{pi}"])

                emit_st(0)
                pend_out = []
                for sidx, (G, kt_) in enumerate(steps):
                    if sidx + 1 < len(steps):
                        emit_st(sidx + 1)
                    pi = sidx % 2
                    pti = sidx % 3
                    oi = G % 2
                    nk = 4 * G + 4
                    op("act", "activation", [f"pf{pi}"], [f"Pt{pti}"], out=Pt[pti][:], in_=PF[pi][:], func=AF.Exp)
                    if kt_ >= 4 * G:
                        op("pool", "tensor_tensor", [f"Pt{pti}", "cm"], [f"Pt{pti}"], out=Pt[pti][:], in0=Pt[pti][:], in1=cm[:, kt_ - 4 * G, :], op=ALU.mult)

                    def f(e, o=PF[2 + oi][0:65, :], l_=Va[i][:, kt_, :], r_=Pt[pti][:], st=(kt_ == 0), sp=(kt_ == nk - 1)):
                        return e.matmul(out=o, lhsT=l_, rhs=r_, start=st, stop=sp)
                    S_.add("pe", f, reads=[f"Va{i}", f"Va{i}_1", f"Pt{pti}"], writes=[f"pf{2 + oi}"])
                    for po in list(pend_out):
                        if sidx >= po[1]:
                            out_stage(h, po[0])
                            pend_out.remove(po)
                    if kt_ == nk - 1:
                        pend_out.append((G, sidx + 2))
                    if sidx % 8 == 4 and pend_gate:
                        gate_tile(h + 1, pend_gate.pop(0))
                for po in pend_out:
                    out_stage(h, po[0])
                while pend_gate:
                    gate_tile(h + 1, pend_gate.pop(0))
            S_.barrier()

        def phase_C1(l, xsrc):
            arena[0] = GLOBAL_TOP
            wo = sb("wo", [128, 8, D], BF16)
            gpost = sb("gpost", [128, D], F32)
            gpre2 = sb("gpre2", [128, D], F32)
            for c in range(8):
                dma("sp" if c % 2 == 0 else "pool", wo[:, c, :], wo_d[l, c * 128:(c + 1) * 128, :], [], [f"wo{c}"], f"w{c}")
            WO = [f"wo{c}" for c in range(8)]
            dma("sp", gpost[:], gains_d[l, 1:2, :].broadcast_to([128, D]), [], ["gpost"], "c2")
            dma("sp", gpre2[:], gains_d[l, 2:3, :].broadcast_to([128, D]), [], ["gpre2"], "c3")
            mt = [sb(f"mt{i}", [128, D], BF16) for i in range(2)]
            mT = [sb(f"mT{i}", [128, 8, 128], BF16) for i in range(2)]
            xt = [sb(f"xt{i}", [128, D], F32) for i in range(2)]
            yn = [sb(f"yn{i}", [128, D], F32) for i in range(2)]
            sq = sb("sq", [128, D], F32)
            ss = [sb(f"ss{i}", [128, 2], F32) for i in range(2)]
            rs = [sb(f"rs{i}", [128, 1], F32) for i in range(2)]
            hb = [sb(f"hb{i}", [128, D], BF16) for i in range(2)]
            for t in range(NT):
                b = t % 2
                rows = slice(t * 128, (t + 1) * 128)
                dma("sp", mt[b][:], mix[rows, :], [], [f"mt{b}"], f"mt{b}")
                dma("sp", xt[b][:], xsrc[rows, :], [], [f"xt{b}"], f"xt{b}")
                for half in range(2):
                    trg([(PB[half][:, c * 128:(c + 1) * 128], mt[b][:, (half * 4 + c) * 128:(half * 4 + c + 1) * 128], identb[:]) for c in range(4)],
                        [f"mt{b}", "identb"], [f"pb{half}"])
                    src = PB[half][:, 0:512].rearrange("p (c t) -> p c t", c=4)
                    if half == 0:
                        op("act", "copy", [f"pb{half}"], [f"mT{b}_{half}"], out=mT[b][:, 0:4, :], in_=src)
                    else:
                        op("dve", "tensor_copy", [f"pb{half}"], [f"mT{b}_{half}"], out=mT[b][:, 4:8, :], in_=src)
                for n in range(2):
                    pi = (2 * t + n) % 4
                    mmg([(PF[pi][:], mT[b][:, c, :], wo[:, c, n * 512:(n + 1) * 512]) for c in range(8)], WO + [f"mT{b}_0", f"mT{b}_1"], [f"pf{pi}"])
                    op("act", "activation", [f"pf{pi}"], ["sq", f"ss{b}_{n}"], out=sq[:, n * 512:(n + 1) * 512], in_=PF[pi][:], func=AF.Square,
                       accum_out=ss[b][:, n:n + 1])
                op("dve", "tensor_tensor", [f"ss{b}_0", f"ss{b}_1"], [f"rs{b}"], out=rs[b][:], in0=ss[b][:, 0:1], in1=ss[b][:, 1:2], op=ALU.add)
                rstd_from_ss(rs[b][:], rs[b][:], D, [f"rs{b}"], [f"rs{b}"])
                for n in range(2):
                    pi = (2 * t + n) % 4
                    op("dve", "scalar_tensor_tensor", [f"pf{pi}", f"rs{b}", "gpost"], [f"yn{b}_{n}"], out=yn[b][:, n * 512:(n + 1) * 512], in0=PF[pi][:],
                       scalar=rs[b][:], in1=gpost[:, n * 512:(n + 1) * 512], op0=ALU.mult, op1=ALU.mult)
                op("pool", "tensor_tensor", [f"yn{b}_0", f"yn{b}_1", f"xt{b}"], [f"yn{b}"], out=yn[b][:], in0=yn[b][:], in1=xt[b][:], op=ALU.add)
                dma("pool", x1s[rows, :], yn[b][:], [f"yn{b}"], [], f"o_yn{b}")
                op("act", "activation", [f"yn{b}"], ["sq", f"ss{b}_0"], out=sq[:], in_=yn[b][:], func=AF.Square, accum_out=ss[b][:, 0:1])
                rstd_from_ss(ss[b][:, 0:1], rs[b][:], D, [f"ss{b}_0"], [f"rs{b}"])
                op("dve", "scalar_tensor_tensor", [f"yn{b}", f"rs{b}", "gpre2"], [f"hb{b}"], out=hb[b][:], in0=yn[b][:], scalar=rs[b][:], in1=gpre2[:],
                   op0=ALU.mult, op1=ALU.mult)
                dma("pool", h2s[rows, :], hb[b][:], [f"hb{b}"], [], f"o_hb{b}")
            S_.barrier()

        def phase_C2(l, dst):
            arena[0] = GLOBAL_TOP
            wg = sb("wg", [128, 8, DFF], BF16)
            wu = sb("wu", [128, 8, DFF], BF16)
            wd = sb("wd", [128, NFC, D], BF16)
            gpost = sb("gpost", [128, D], F32)
            for c in range(8):
                dma("sp", wg[:, c, :], wg_d[l, c * 128:(c + 1) * 128, :], [], [f"wg{c}"], f"w{c}")
                dma("pool", wu[:, c, :], wu_d[l, c * 128:(c + 1) * 128, :], [], [f"wu{c}"], f"wu{c}")
            for f_ in range(NFC):
                dma("sp" if f_ % 2 else "pool", wd[:, f_, :], wd_d[l, f_ * 128:(f_ + 1) * 128, :], [], [f"wd{f_}"], f"wd{f_ % 4}")
            WG = [f"wg{c}" for c in range(8)]
            WU = [f"wu{c}" for c in range(8)]
            WD = [f"wd{f_}" for f_ in range(NFC)]
            dma("sp", gpost[:], gains_d[l, 3:4, :].broadcast_to([128, D]), [], ["gpost"], "c2")
            ht = [sb(f"ht{i}", [128, D], BF16) for i in range(2)]
            hT = sb("hT", [128, 8, 512], BF16)
            aT = sb("aT", [128, NFC, 512], BF16)
            th = [sb(f"th{i}", [128, 512], F32) for i in range(2)]
            us = [sb(f"us{i}", [128, 512], F32) for i in range(2)]
            xt = [sb(f"xt{i}", [128, D], F32) for i in range(2)]
            yn = [sb(f"yn{i}", [128, D], F32) for i in range(2)]
            sq = sb("sq", [128, 512], F32)
            ss = [sb(f"ss{i}", [128, 2], F32) for i in range(2)]
            rs = [sb(f"rs{i}", [128, 1], F32) for i in range(2)]
            for G in range(NG):
                for tt in range(4):
                    t = G * 4 + tt
                    b = t % 2
                    dma("sp", ht[b][:], h2s[t * 128:(t + 1) * 128, :], [], [f"ht{b}"], f"ht{b}")
                    for half in range(2):
                        trg([(PB[half][:, c * 128:(c + 1) * 128], ht[b][:, (half * 4 + c) * 128:(half * 4 + c + 1) * 128], identb[:]) for c in range(4)],
                            [f"ht{b}", "identb"], [f"pb{half}"])
                        src = PB[half][:, 0:512].rearrange("p (c t) -> p c t", c=4)
                        dst_ = hT[:, half * 4:(half + 1) * 4, tt * 128:(tt + 1) * 128]
                        if half == 0:
                            op("act", "copy", [f"pb{half}"], [f"hT_{tt}"], out=dst_, in_=src)
                        else:
                            op("dve", "tensor_copy", [f"pb{half}"], [f"hT_{tt}"], out=dst_, in_=src)
                hk = [f"hT_{tt}" for tt in range(4)]
                for f_ in range(NFC):
                    b = f_ % 2
                    pg, pu = PF[2 * b], PF[2 * b + 1]
                    mmg([(pg[:], wg[:, c, f_ * 128:(f_ + 1) * 128], hT[:, c, :]) for c in range(8)], WG + hk, [f"pf{2 * b}"])
                    mmg([(pu[:], wu[:, c, f_ * 128:(f_ + 1) * 128], hT[:, c, :]) for c in range(8)], WU + hk, [f"pf{2 * b + 1}"])
                    op("act", "activation", [f"pf{2 * b}"], [f"th{b}"], out=th[b][:], in_=pg[:], func=AF.Tanh, scale=0.5)
                    op("act", "copy", [f"pf{2 * b + 1}"], [f"us{b}"], out=us[b][:], in_=pu[:])
                    op("dve", "scalar_tensor_tensor", [f"th{b}", f"pf{2 * b}"], [f"th{b}"], out=th[b][:], in0=th[b][:], scalar=1.0, in1=pg[:],
                       op0=ALU.add, op1=ALU.mult)
                    op("dve", "scalar_tensor_tensor", [f"th{b}", f"us{b}"], [f"aT_{f_}"], out=aT[:, f_, :], in0=th[b][:], scalar=0.5, in1=us[b][:],
                       op0=ALU.mult, op1=ALU.mult)
                ak = [f"aT_{f_}" for f_ in range(NFC)]
                for tt in range(4):
                    t = G * 4 + tt
                    b = t % 2
                    rows = slice(t * 128, (t + 1) * 128)
                    dma("sp", xt[b][:], x1s[rows, :], [], [f"xt{b}"], f"xt{b}")
                    for n in range(2):
                        pi = 4 + n
                        mmg([(PF[pi][:], aT[:, f_, tt * 128:(tt + 1) * 128], wd[:, f_, n * 512:(n + 1) * 512]) for f_ in range(NFC)], WD + ak, [f"pf{pi}"])
                        op("act", "activation", [f"pf{pi}"], ["sq", f"ss{b}_{n}"], out=sq[:], in_=PF[pi][:], func=AF.Square, accum_out=ss[b][:, n:n + 1])
                    op("dve", "tensor_tensor", [f"ss{b}_0", f"ss{b}_1"], [f"rs{b}"], out=rs[b][:], in0=ss[b][:, 0:1], in1=ss[b][:, 1:2], op=ALU.add)
                    rstd_from_ss(rs[b][:], rs[b][:], D, [f"rs{b}"], [f"rs{b}"])
                    for n in range(2):
                        pi = 4 + n
                        op("dve", "scalar_tensor_tensor", [f"pf{pi}", f"rs{b}", "gpost"], [f"yn{b}_{n}"], out=yn[b][:, n * 512:(n + 1) * 512], in0=PF[pi][:],
                           scalar=rs[b][:], in1=gpost[:, n * 512:(n + 1) * 512], op0=ALU.mult, op1=ALU.mult)
                    op("pool", "tensor_tensor", [f"yn{b}_0", f"yn{b}_1", f"xt{b}"], [f"yn{b}"], out=yn[b][:], in0=yn[b][:], in1=xt[b][:], op=ALU.add)
                    dma("pool", dst[rows, :], yn[b][:], [f"yn{b}"], [], f"o_yn{b}")
            S_.barrier()

        for l in range(depth):
            xsrc = x_in if l == 0 else xs
            import os
            PH = os.environ.get("PHASES", "A,GLA,GDN,MOBA,C1,C2").split(",")
            if "A" in PH: phase_A(l, xsrc)
            if "GLA" in PH: phase_GLA(l)
            if "GDN" in PH: phase_GDN(l)
            if "MOBA" in PH: phase_MOBA(l)
            if "C1" in PH: phase_C1(l, xsrc)
            if "C2" in PH: phase_C2(l, y_out if l == depth - 1 else xs)

        semkeys = S_.emit()
        sems = [es.enter_context(nc.semaphore(f"s{i}")) for i in range(len(semkeys))]
        block = es.enter_context(nc.Block())
        S_.run(semkeys, sems, block)
    return nc


def _consts(S):
    j = np.arange(128)[:, None]
    i = np.arange(128)[None, :]
    c = {}
    c["c_identb"] = np.eye(128, dtype=np.float32).astype(BF)
    c["c_identf"] = np.eye(128, dtype=np.float32)
    c["c_masku"] = (j <= i).astype(np.float32)
    c["c_negu"] = np.where(j <= i, 0.0, -BIG).astype(np.float32)
    c["c_posl"] = np.where(i < j, 0.0, BIG).astype(np.float32)
    c["c_triu"] = (j <= i).astype(np.float32)
    c["c_trisu"] = (j > i).astype(np.float32)
    c["c_bones"] = ((j // 64) == (i // 64)).astype(np.float32)
    half = 32
    inv = (np.float32(10000.0) ** (-np.arange(half, dtype=np.float32) / np.float32(half))).astype(np.float32)
    pos = np.arange(S, dtype=np.float32)
    ang = (pos[:, None] * inv[None, :]).astype(np.float32)
    cos = np.cos(ang).astype(np.float32).T
    sin = np.sin(ang).astype(np.float32).T
    p = np.arange(128)
    cosP = cos[p % 32]
    sinP = sin[p % 32] * np.where((p % 64) < 32, -1.0, 1.0)[:, None]
    c["c_rope"] = np.stack([cosP * 0.125, sinP * 0.125, cosP, sinP]).astype(np.float32)
    c["c_onehot"] = (np.arange(64)[:, None] == (np.arange(S)[None, :] // 256)).astype(np.float32).astype(BF)
    own = np.arange(32)[:, None]
    jb = np.arange(32)[None, :]
    mbt = np.where(jb < own, 0.0, np.where(jb == own, BIG, -BIG)).astype(np.float32)
    c["c_mb"] = np.broadcast_to(mbt.reshape(1, 32 * 32), (128, 32 * 32)).copy()
    cm = np.zeros((4, 128, 512), np.float32)
    key = np.arange(128)[:, None]
    q = np.arange(512)[None, :]
    for r in range(4):
        cm[r] = ((r * 128 + key) <= q).astype(np.float32)
    c["c_cm"] = cm.astype(BF)
    return c


def _prep_weights(inp, depth):
    w = {}
    w["win"] = np.ascontiguousarray(inp["w_in"][:depth][:, :, WIN_IDX]).astype(BF)
    wga = np.zeros((depth, 33, 256), np.float32)
    wga[:, 0:16, :] = inp["gla_w_gate"][:depth]
    wga[:, 32, :] = inp["gla_b_gate"][:depth]
    w["wga"] = wga.astype(BF)
    w["wo"] = np.asarray(inp["w_o"][:depth]).astype(BF)
    w["wg"] = np.asarray(inp["ffn_w_gate"][:depth]).astype(BF)
    w["wu"] = np.asarray(inp["ffn_w_up"][:depth]).astype(BF)
    w["wd"] = np.asarray(inp["ffn_w_down"][:depth]).astype(BF)
    w["gains"] = np.stack([inp["norm_mix_pre"][:depth], inp["norm_mix_post"][:depth], inp["norm_ffn_pre"][:depth],
                           inp["norm_ffn_post"][:depth]], axis=1).astype(np.float32)
    w["hn"] = np.stack([inp["gla_norm"][:depth], inp["gdn_norm"][:depth]], axis=1).astype(np.float32)
    w["conv"] = np.ascontiguousarray(np.transpose(inp["gdn_conv"][:depth], (0, 2, 1))).astype(np.float32)
    w["gsc"] = np.stack([inp["gdn_a_log"][:depth], inp["gdn_dt_bias"][:depth]], axis=1).astype(np.float32)
    return w


def run(inputs, n_cores=8, dbg=False, depth=None):
    inp = {k: np.asarray(v) for k, v in inputs.items()}
    x = inp["x"]
    B, S, _ = x.shape
    depth = depth or inp["w_in"].shape[0]
    nc = build(S, depth, dbg=dbg)
    shared = {}
    shared.update(_consts(S))
    shared.update(_prep_weights(inp, depth))
    in_maps = []
    for c in range(n_cores):
        m = dict(shared)
        m["x"] = np.ascontiguousarray(x[c % B]).astype(np.float32)
        in_maps.append(m)
    res = run_bass_kernel_spmd(nc, in_maps, core_ids=list(range(n_cores)))
    return res


def kernel(**inputs):
    res = run(inputs)
    B = np.asarray(inputs["x"]).shape[0]
    out = np.stack([res.results[b]["y"] for b in range(B)], axis=0)
    return out.astype(np.float32)
```

```python
import numpy as np
import ml_dtypes
import concourse.bass as bass
import concourse.mybir as mybir
from concourse.bass_utils import run_bass_kernel_spmd
from contextlib import ExitStack

F32 = mybir.dt.float32
BF16 = mybir.dt.bfloat16
AF = mybir.ActivationFunctionType
ALU = mybir.AluOpType
AX = mybir.AxisListType
BF = ml_dtypes.bfloat16

D = 1024
DFF = 2816
NFC = DFF // 128
BIG = 30000.0
ENGS = ("pe", "act", "dve", "pool", "sp")


class _Op:
    __slots__ = ("eng", "fn", "deps", "dma", "chan", "idx", "sig", "waits", "need")

    def __init__(self, eng, fn, dma, chan, idx):
        self.eng, self.fn, self.dma, self.chan, self.idx = eng, fn, dma, chan, idx
        self.deps = ()
        self.sig = None
        self.need = False
        self.waits = ()


class Sched:
    def __init__(self):
        self.ops = []
        self.lastw = {}
        self.readers = {}
        self.last_on_eng = {}
        self.last_on_chan = {}

    @staticmethod
    def _norm(keys):
        return [k[:3] if (k[:2] in ("pf", "pb") and len(k) > 2 and k[2].isdigit()) else k for k in keys]

    def add(self, eng, fn, reads=(), writes=(), dma=False, chan=None):
        reads = self._norm(reads)
        writes = self._norm(writes)
        op = _Op(eng, fn, dma, chan if dma else None, len(self.ops))
        deps = set()
        for k in reads:
            w = self.lastw.get(k)
            if w is not None:
                deps.add(w)
        for k in writes:
            w = self.lastw.get(k)
            if w is not None:
                deps.add(w)
            for r in self.readers.get(k, ()):
                deps.add(r)
        op.deps = deps
        for k in reads:
            self.readers.setdefault(k, []).append(op.idx)
        for k in writes:
            self.lastw[k] = op.idx
            self.readers[k] = []
        self.ops.append(op)
        if dma:
            self.last_on_chan[chan] = op.idx
        else:
            self.last_on_eng[eng] = op.idx
        return op

    def barrier(self):
        allprev = set(self.last_on_eng.values()) | set(self.last_on_chan.values())
        for e in ENGS:
            op = _Op(e, None, False, None, len(self.ops))
            op.deps = set(allprev)
            self.ops.append(op)
            self.last_on_eng[e] = op.idx
        self.lastw.clear()
        self.readers.clear()

    def emit(self):
        ops = self.ops
        for op in ops:
            for d in op.deps:
                ops[d].need = True
        cnt = {}
        for op in ops:
            if op.fn is None:
                continue
            if op.dma:
                key = ("chan", op.chan)
                cnt[key] = cnt.get(key, 0) + 16
                op.sig = (key, cnt[key])
            elif op.need:
                key = ("eng", op.eng)
                cnt[key] = cnt.get(key, 0) + 1
                op.sig = (key, cnt[key])
        water = {e: {} for e in ENGS}
        for op in ops:
            need = {}
            for d in op.deps:
                dop = ops[d]
                if dop.sig is None:
                    continue
                k, v = dop.sig
                if need.get(k, 0) < v:
                    need[k] = v
            wm = water[op.eng]
            waits = []
            for k, v in need.items():
                if wm.get(k, 0) < v:
                    wm[k] = v
                    waits.append((k, v))
            op.waits = waits
        return sorted(cnt.keys())

    def run(self, semkeys, sems, block):
        ops = self.ops
        semmap = dict(zip(semkeys, sems))

        def stream(engname):
            def body(e):
                for op in ops:
                    if op.eng != engname:
                        continue
                    for k, v in op.waits:
                        e.wait_ge(semmap[k], v)
                    if op.fn is None:
                        continue
                    ins = op.fn(e)
                    if op.sig is not None:
                        ins.then_inc(semmap[op.sig[0]], 16 if op.dma else 1)
            return body

        block.tensor(stream("pe"))
        block.scalar(stream("act"))
        block.vector(stream("dve"))
        block.gpsimd(stream("pool"))
        block.sync(stream("sp"))


def _win_cols():
    o = {}
    idx = []
    base = {"gq": 0, "gk": 256, "gv": 512, "gz": 768, "ga": 1024, "dq": 1040, "dk": 1296, "dv": 1552,
            "dz": 1808, "db": 2064, "da": 2068, "mq": 2072, "mk": 2584, "mv": 3096}

    def put(name, cols):
        o[name] = len(idx)
        idx.extend(cols)

    put("gq", range(base["gq"], base["gq"] + 256))
    put("gk", range(base["gk"], base["gk"] + 256))
    put("gv", range(base["gv"], base["gv"] + 256))
    put("ga", range(base["ga"], base["ga"] + 16))
    put("dq", range(base["dq"], base["dq"] + 256))
    put("dk", range(base["dk"], base["dk"] + 256))
    put("dv", range(base["dv"], base["dv"] + 256))
    put("dba", range(base["db"], base["db"] + 8))
    swap = lambda b: [b + h * 64 + (d + 32) % 64 for h in range(8) for d in range(64)]
    put("mq", range(base["mq"], base["mq"] + 512))
    put("mqs", swap(base["mq"]))
    put("mk", range(base["mk"], base["mk"] + 512))
    put("mks", swap(base["mk"]))
    put("z", list(range(base["gz"], base["gz"] + 256)) + list(range(base["dz"], base["dz"] + 256)))
    put("mv", range(base["mv"], base["mv"] + 512))
    return np.array(idx, dtype=np.int64), o


WIN_IDX, WOFF = _win_cols()
NCOL = len(WIN_IDX)


def build(S, depth, dbg=False):
    NT = S // 128
    NG = S // 512
    NB = S // 256
    nc = bass.Bass("TRN2", target_bir_lowering=False)
    sc_kind = "ExternalOutput" if dbg else "Internal"

    def din(name, shape, dt):
        return nc.dram_tensor(name, shape, dt, kind="ExternalInput").ap()

    def dscr(name, shape, dt):
        return nc.dram_tensor(name, shape, dt, kind=sc_kind).ap()

    x_in = din("x", [S, D], F32)
    win_d = din("win", [depth, D, NCOL], BF16)
    wga_d = din("wga", [depth, 33, 256], BF16)
    wo_d = din("wo", [depth, D, D], BF16)
    wg_d = din("wg", [depth, D, DFF], BF16)
    wu_d = din("wu", [depth, D, DFF], BF16)
    wd_d = din("wd", [depth, DFF, D], BF16)
    gains_d = din("gains", [depth, 4, D], F32)
    hn_d = din("hn", [depth, 2, 64], F32)
    conv_d = din("conv", [depth, 768, 4], F32)
    gsc_d = din("gsc", [depth, 2, 4], F32)
    c_identb = din("c_identb", [128, 128], BF16)
    c_identf = din("c_identf", [128, 128], F32)
    c_masku = din("c_masku", [128, 128], F32)
    c_negu = din("c_negu", [128, 128], F32)
    c_posl = din("c_posl", [128, 128], F32)
    c_triu = din("c_triu", [128, 128], F32)
    c_trisu = din("c_trisu", [128, 128], F32)
    c_bones = din("c_bones", [128, 128], F32)
    c_rope = din("c_rope", [4, 128, S], F32)
    c_onehot = din("c_onehot", [64, S], BF16)
    c_mb = din("c_mb", [128, 32 * 32], F32)
    c_cm = din("c_cm", [4, 128, 512], BF16)
    y_out = nc.dram_tensor("y", [S, D], F32, kind="ExternalOutput").ap()

    xs = dscr("xs", [S, D], F32)
    x1s = dscr("x1s", [S, D], F32)
    h2s = dscr("h2s", [S, D], BF16)
    glaT = dscr("glaT", [1024, S], F32)
    gdnT = dscr("gdnT", [768, S], BF16)
    gsct = dscr("gsct", [S, 24], F32)
    mqT = dscr("mqT", [512, S], BF16)
    mkT = dscr("mkT", [512, S], BF16)
    mksum = dscr("mksum", [512, 32], F32)
    mvs = dscr("mvs", [S, 512], BF16)
    zs = dscr("zs", [S, 512], F32)
    mix = dscr("mix", [S, D], BF16)

    S_ = Sched()
    uid = [0]
    ARENA_BASE = 16640
    ARENA_CAP = 226000
    arena = [ARENA_BASE]

    def sb(name, shape, dt):
        nbytes = int(np.prod(shape[1:])) * (4 if dt == F32 else 2)
        nbytes = (nbytes + 63) // 64 * 64
        off = arena[0]
        arena[0] += nbytes
        assert arena[0] <= ARENA_CAP, (name, arena[0])
        uid[0] += 1
        return nc.alloc_sbuf_tensor_at(f"{name}_{uid[0]}", shape, dt, offset=off)

    es = ExitStack()
    with es:
        PF = [es.enter_context(nc.psum_tensor(f"pf{i}", [128, 512], F32)) for i in range(6)]
        PB = [es.enter_context(nc.psum_tensor(f"pb{i}", [128, 1024], BF16)) for i in range(2)]

        def op(eng, meth, reads, writes, **kw):
            S_.add(eng, lambda e, m=meth, k=kw: getattr(e, m)(**k), reads=reads, writes=writes)

        def mmg(items, reads, writes):
            def f(e, items=items):
                n = len(items)
                for i, (o, l, r) in enumerate(items):
                    ins = e.matmul(out=o, lhsT=l, rhs=r, start=(i == 0), stop=(i == n - 1))
                return ins
            S_.add("pe", f, reads=reads, writes=writes)

        def trg(items, reads, writes):
            def f(e, items=items):
                for (o, i_, idn) in items:
                    ins = e.transpose(out=o, in_=i_, identity=idn)
                return ins
            S_.add("pe", f, reads=reads, writes=writes)

        def dma(q, out, in_, reads, writes, chan, slow=False):
            if slow:
                S_.add(q, lambda e, o=out, i=in_: e.dma_start(out=o, in_=i, allow_slow_non_contiguous=True),
                       reads=reads, writes=writes, dma=True, chan=chan)
            else:
                S_.add(q, lambda e, o=out, i=in_: e.dma_start(out=o, in_=i), reads=reads, writes=writes, dma=True, chan=chan)

        identb = sb("identb", [128, 128], BF16)
        identf = sb("identf", [128, 128], F32)
        epsc = sb("epsc", [128, 1], F32)
        onec = sb("onec", [128, 1], F32)
        dma("sp", identb[:], c_identb, [], ["identb"], "c0")
        dma("sp", identf[:], c_identf, [], ["identf"], "c1")
        op("dve", "memset", [], ["epsc"], ap=epsc[:], constant=1e-6)
        op("dve", "memset", [], ["onec"], ap=onec[:], constant=1.0)
        GLOBAL_TOP = arena[0]

        def rstd_from_ss(ss_ap, out_ap, n, rk, wk):
            op("act", "activation", rk + ["epsc"], wk, out=out_ap, in_=ss_ap, func=AF.Ln, scale=1.0 / n, bias=epsc[:ss_ap.shape[0], :])
            op("act", "activation", wk, wk, out=out_ap, in_=out_ap, func=AF.Exp, scale=-0.5)

        def phase_A(l, xsrc):
            arena[0] = GLOBAL_TOP
            win = sb("win", [128, 8, NCOL], BF16)
            wga = sb("wga", [33, 256], BF16)
            gpre = sb("gpre", [128, D], F32)
            convw = sb("convw", [128, 6, 4], F32)
            dtb = sb("dtb", [128, 4], F32)
            nA = sb("nA", [128, 4], F32)
            triu = sb("triu", [128, 128], F32)
            trisu = sb("trisu", [128, 128], F32)
            bones = sb("bones", [128, 128], F32)
            for c in range(8):
                dma("sp" if c % 2 == 0 else "pool", win[:, c, :], win_d[l, c * 128:(c + 1) * 128, :], [], [f"win{c}"], f"w{c}")
            WIN = [f"win{c}" for c in range(8)]
            dma("sp", wga[:], wga_d[l], [], ["wga"], "c2")
            dma("sp", gpre[:], gains_d[l, 0:1, :].broadcast_to([128, D]), [], ["gpre"], "c3")
            dma("sp", convw[:], conv_d[l].rearrange("(g p) k -> p g k", p=128), [], ["convw"], "c4")
            dma("sp", dtb[:], gsc_d[l, 1:2, :].broadcast_to([128, 4]), [], ["dtb"], "c5")
            dma("sp", nA[:], gsc_d[l, 0:1, :].broadcast_to([128, 4]), [], ["nA"], "c6")
            dma("sp", triu[:], c_triu, [], ["triu"], "c7")
            dma("sp", trisu[:], c_trisu, [], ["trisu"], "c8")
            dma("sp", bones[:], c_bones, [], ["bones"], "c9")
            op("act", "activation", ["nA"], ["nA"], out=nA[:], in_=nA[:], func=AF.Exp)
            op("dve", "tensor_scalar", ["nA"], ["nA"], out=nA[:], in0=nA[:], scalar1=-1.0, scalar2=None, op0=ALU.mult)

            xt = [sb(f"xt{i}", [128, D], F32) for i in range(2)]
            sq = sb("sq", [128, D], F32)
            hb = [sb(f"hb{i}", [128, D], BF16) for i in range(2)]
            ss = [sb(f"ss{i}", [128, 1], F32) for i in range(2)]
            rs = [sb(f"rs{i}", [128, 1], F32) for i in range(2)]
            hT = [sb(f"hT{i}", [128, 8, 512], BF16) for i in range(2)]
            gaT = sb("gaT", [33, 512], BF16)
            op("dve", "memset", [], ["gaT"], ap=gaT[:], constant=0.0)
            op("dve", "memset", ["gaT"], ["gaT"], ap=gaT[32:33, :], constant=1.0)
            stf = [sb(f"stf{i}", [128, 512], F32) for i in range(3)]
            stb = [sb(f"stb{i}", [128, 512], BF16) for i in range(3)]
            xc = [sb(f"xc{i}", [128, 3 + 512], F32) for i in range(6)]
            cy = [sb(f"cy{i}", [128, 512], F32) for i in range(2)]
            ce = [sb(f"ce{i}", [128, 512], F32) for i in range(2)]
            rope_t = [sb(f"rope{i}", [128, 512], F32) for i in range(4)]
            r1 = [sb(f"r1{i}", [128, 512], F32) for i in range(2)]
            r2 = [sb(f"r2{i}", [128, 512], F32) for i in range(2)]
            ksum = sb("ksum", [128, 4, 32], F32)
            sc = [sb(f"sc{i}", [128, 24], F32) for i in range(2)]
            sct = [sb(f"sct{i}", [128, 8], F32) for i in range(2)]
            op("pool", "memset", [], ["ksum"], ap=ksum[:], constant=0.0)
            for i in range(6):
                op("pool", "memset", [], [f"xc{i}"], ap=xc[i][:, 0:3], constant=0.0)
            nst = [0, 0]
            pfi = [0]

            def nextpf():
                pfi[0] = (pfi[0] + 1) % 4
                return pfi[0], PF[pfi[0]], f"pf{pfi[0]}"

            def load_norm(G):
                hTb = hT[G % 2]
                for tt in range(4):
                    t = G * 4 + tt
                    b = t % 2
                    dma("sp", xt[b][:], xsrc[t * 128:(t + 1) * 128, :], [], [f"xt{b}"], f"xt{b}")
                    op("act", "activation", [f"xt{b}"], ["sq", f"ss{b}"], out=sq[:], in_=xt[b][:], func=AF.Square, accum_out=ss[b][:])
                    rstd_from_ss(ss[b][:], rs[b][:], D, [f"ss{b}"], [f"rs{b}"])
                    op("dve", "scalar_tensor_tensor", [f"xt{b}", f"rs{b}", "gpre"], [f"hb{b}"], out=hb[b][:], in0=xt[b][:],
                       scalar=rs[b][:], in1=gpre[:], op0=ALU.mult, op1=ALU.mult)
                    for half in range(2):
                        trg([(PB[half][:, c * 128:(c + 1) * 128], hb[b][:, (half * 4 + c) * 128:(half * 4 + c + 1) * 128], identb[:])
                             for c in range(4)], [f"hb{b}", "identb"], [f"pb{half}"])
                        dst = hTb[:, half * 4:(half + 1) * 4, tt * 128:(tt + 1) * 128]
                        src = PB[half][:, 0:512].rearrange("p (c t) -> p c t", c=4)
                        if half == 0:
                            op("act", "copy", [f"pb{half}"], [f"hT{G % 2}_{tt}"], out=dst, in_=src)
                        else:
                            op("dve", "tensor_copy", [f"pb{half}"], [f"hT{G % 2}_{tt}"], out=dst, in_=src)

            def fm(G, coff, ncols=128):
                i, p, pk = nextpf()
                hTb = hT[G % 2]
                mmg([(p[0:ncols, :], win[:, c, coff:coff + ncols], hTb[:, c, :]) for c in range(8)],
                    WIN + [f"hT{G % 2}_{tt}" for tt in range(4)], [pk])
                return p, pk

            def stage_f():
                nst[0] = (nst[0] + 1) % 3
                return stf[nst[0]], f"stf{nst[0]}"

            def stage_b():
                nst[1] = (nst[1] + 1) % 3
                return stb[nst[1]], f"stb{nst[1]}"

            for G in range(NG):
                g0 = G * 512
                load_norm(G)
                hk = [f"hT{G % 2}_{tt}" for tt in range(4)]
                for j in range(6):
                    p, pk = fm(G, WOFF["gq"] + j * 128)
                    st, sk = stage_f()
                    op("act", "copy", [pk], [sk], out=st[:], in_=p[:])
                    dma("act", glaT[j * 128:(j + 1) * 128, g0:g0 + 512], st[:], [sk], [], "o_" + sk)
                p, pk = fm(G, WOFF["ga"], 16)
                op("act", "copy", [pk], ["gaT"], out=gaT[0:16, :], in_=p[0:16, :])
                for j in range(2):
                    i, p2, pk2 = nextpf()
                    mmg([(p2[:], wga[:, j * 128:(j + 1) * 128], gaT[:, :])], ["wga", "gaT"], [pk2])
                    st, sk = stage_f()
                    op("act", "activation", [pk2], [sk], out=st[:], in_=p2[:], func=AF.Exp, scale=-1.0)
                    op("act", "activation", [sk, "onec"], [sk], out=st[:], in_=st[:], func=AF.Ln, bias=onec[:])
                    dma("act", glaT[768 + j * 128:768 + (j + 1) * 128, g0:g0 + 512], st[:], [sk], [], "o_" + sk)
                for tt in range(4):
                    t = G * 4 + tt
                    b = t % 2
                    i, p, pk = nextpf()
                    mmg([(p[:, 0:8], hT[G % 2][:, c, tt * 128:(tt + 1) * 128], win[:, c, WOFF["dba"]:WOFF["dba"] + 8]) for c in range(8)],
                        WIN + hk, [pk])
                    scb, sck, stt_, stk = sc[b], f"sc{b}", sct[b], f"sct{b}"
                    op("act", "activation", [pk], [stk], out=stt_[:, 0:4], in_=p[:, 0:4], func=AF.Exp, scale=-1.0)
                    op("dve", "tensor_scalar", [stk], [stk], out=stt_[:, 0:4], in0=stt_[:, 0:4], scalar1=1.0, scalar2=None, op0=ALU.add)
                    op("dve", "reciprocal", [stk], [sck], out=scb[:, 4:8], in_=stt_[:, 0:4])
                    op("dve", "tensor_tensor", [pk, "dtb"], [stk], out=stt_[:, 4:8], in0=p[:, 4:8], in1=dtb[:], op=ALU.add)
                    op("act", "activation", [stk], [stk], out=stt_[:, 4:8], in_=stt_[:, 4:8], func=AF.Exp)
                    op("act", "activation", [stk, "onec"], [stk], out=stt_[:, 4:8], in_=stt_[:, 4:8], func=AF.Ln, bias=onec[:])
                    op("dve", "tensor_tensor", [stk, "nA"], [stk], out=stt_[:, 4:8], in0=stt_[:, 4:8], in1=nA[:], op=ALU.mult)
                    i2, pc, pck = nextpf()
                    mmg([(pc[:, 0:4], triu[:], stt_[:, 4:8])], ["triu", stk], [pck])
                    op("act", "copy", [pck], [sck], out=scb[:, 0:4], in_=pc[:, 0:4])
                    op("act", "activation", [pck], [sck], out=scb[:, 8:12], in_=pc[:, 0:4], func=AF.Exp)
                    op("dve", "tensor_scalar", [pck], [sck], out=scb[:, 20:24], in0=pc[:, 0:4], scalar1=-1.0, scalar2=None, op0=ALU.mult)
                    i3, pr, prk = nextpf()
                    mmg([(pr[:, 0:4], trisu[:], stt_[:, 4:8])], ["trisu", stk], [prk])
                    op("act", "activation", [prk], [sck], out=scb[:, 16:20], in_=pr[:, 0:4], func=AF.Exp)
                    op("dve", "tensor_tensor", [sck], [sck], out=scb[:, 12:16], in0=scb[:, 4:8], in1=scb[:, 8:12], op=ALU.mult)
                    dma("sp", gsct[t * 128:(t + 1) * 128, :], scb[:], [sck], [], "o_" + sck)
                for j in range(6):
                    p, pk = fm(G, WOFF["dq"] + j * 128)
                    xk = f"xc{j}"
                    op("act", "copy", [pk], [xk], out=xc[j][:, 3:515], in_=p[:])
                    b = j % 2
                    cyb, cyk, ceb, cek = cy[b], f"cy{b}", ce[b], f"ce{b}"
                    op("dve", "tensor_scalar", [xk, "convw"], [cyk], out=cyb[:], in0=xc[j][:, 3:515], scalar1=convw[:, j, 3:4], scalar2=None, op0=ALU.mult)
                    for i in range(3):
                        op("dve", "scalar_tensor_tensor", [xk, "convw", cyk], [cyk], out=cyb[:], in0=xc[j][:, i:i + 512],
                           scalar=convw[:, j, i:i + 1], in1=cyb[:], op0=ALU.mult, op1=ALU.add)
                    op("pool", "tensor_copy", [xk], [xk], out=xc[j][:, 0:3], in_=xc[j][:, 512:515])
                    op("act", "activation", [cyk], [cek], out=ceb[:], in_=cyb[:], func=AF.Exp, scale=-1.0)
                    op("pool", "tensor_scalar", [cek], [cek], out=ceb[:], in0=ceb[:], scalar1=1.0, scalar2=None, op0=ALU.add)
                    op("dve", "reciprocal", [cek], [cek], out=ceb[:], in_=ceb[:])
                    op("dve", "tensor_tensor", [cek, cyk], [cyk], out=cyb[:], in0=cyb[:], in1=ceb[:], op=ALU.mult)
                    st, sk = stage_b()
                    if j < 4:
                        op("pool", "tensor_tensor", [cyk], [cek], out=ceb[:], in0=cyb[:], in1=cyb[:], op=ALU.mult)
                        i, pn, pnk = nextpf()
                        mmg([(pn[:], bones[:], ceb[:])], ["bones", cek], [pnk])
                        op("act", "activation", [pnk, "epsc"], [cek], out=ceb[:], in_=pn[:], func=AF.Ln, bias=epsc[:])
                        op("act", "activation", [cek], [cek], out=ceb[:], in_=ceb[:], func=AF.Exp, scale=-0.5)
                        op("dve", "scalar_tensor_tensor", [cyk, cek], [sk], out=st[:], in0=cyb[:], scalar=(0.125 if j < 2 else 1.0),
                           in1=ceb[:], op0=ALU.mult, op1=ALU.mult)
                    else:
                        op("dve", "tensor_copy", [cyk], [sk], out=st[:], in_=cyb[:])
                    dma("sp", gdnT[j * 128:(j + 1) * 128, g0:g0 + 512], st[:], [sk], [], "o_" + sk)
                for tbl in range(4):
                    dma("sp", rope_t[tbl][:], c_rope[tbl, :, g0:g0 + 512], [], [f"rope{tbl}"], f"rope{tbl}")
                for isk in range(2):
                    for j in range(4):
                        p, pk = fm(G, WOFF["mk" if isk else "mq"] + j * 128)
                        b = j % 2
                        op("dve", "tensor_tensor", [pk, f"rope{2 * isk}"], [f"r1{b}"], out=r1[b][:], in0=p[:], in1=rope_t[2 * isk][:], op=ALU.mult)
                        p2, pk2 = fm(G, WOFF["mks" if isk else "mqs"] + j * 128)
                        op("dve", "tensor_tensor", [pk2, f"rope{2 * isk + 1}"], [f"r2{b}"], out=r2[b][:], in0=p2[:], in1=rope_t[2 * isk + 1][:], op=ALU.mult)
                        op("pool", "tensor_tensor", [f"r1{b}", f"r2{b}"], [f"r1{b}"], out=r1[b][:], in0=r1[b][:], in1=r2[b][:], op=ALU.add)
                        st, sk = stage_b()
                        op("act", "copy", [f"r1{b}"], [sk], out=st[:], in_=r1[b][:])
                        dst = (mkT if isk else mqT)
                        dma("act", dst[j * 128:(j + 1) * 128, g0:g0 + 512], st[:], [sk], [], "o_" + sk)
                        if isk:
                            op("dve", "tensor_reduce", [f"r1{b}"], ["ksum"], out=ksum[:, j, 2 * G:2 * G + 2],
                               in_=r1[b][:].rearrange("p (n k) -> p n k", n=2), axis=AX.X, op=ALU.add)
                for tt in range(4):
                    t = G * 4 + tt
                    lhs = lambda c, tt=tt: hT[G % 2][:, c, tt * 128:(tt + 1) * 128]
                    i, p, pk = nextpf()
                    mmg([(p[:], lhs(c), win[:, c, WOFF["z"]:WOFF["z"] + 512]) for c in range(8)], WIN + hk, [pk])
                    st, sk = stage_f()
                    b = tt % 2
                    op("act", "activation", [pk], [f"ce{b}"], out=ce[b][:], in_=p[:], func=AF.Exp, scale=-1.0)
                    op("pool", "tensor_scalar", [f"ce{b}"], [f"ce{b}"], out=ce[b][:], in0=ce[b][:], scalar1=1.0, scalar2=None, op0=ALU.add)
                    op("dve", "reciprocal", [f"ce{b}"], [f"ce{b}"], out=ce[b][:], in_=ce[b][:])
                    op("dve", "tensor_tensor", [f"ce{b}", pk], [sk], out=st[:], in0=p[:], in1=ce[b][:], op=ALU.mult)
                    dma("sp", zs[t * 128:(t + 1) * 128, :], st[:], [sk], [], "o_" + sk)
                    i, p, pk = nextpf()
                    mmg([(p[:], lhs(c), win[:, c, WOFF["mv"]:WOFF["mv"] + 512]) for c in range(8)], WIN + hk, [pk])
                    st, sk = stage_b()
                    op("act", "copy", [pk], [sk], out=st[:], in_=p[:])
                    dma("act", mvs[t * 128:(t + 1) * 128, :], st[:], [sk], [], "o_" + sk)
            for j in range(4):
                dma("pool", mksum[j * 128:(j + 1) * 128, :], ksum[:, j, :], ["ksum"], [], "o_ksum")
            S_.barrier()

        def head_out_stage(t, O, Ok, col0, zcol0, normw, zt, ztk, tmp, tmpk, ms, msk, outb, outbk, chan):
            dma("sp", zt[:], zs[t * 128:(t + 1) * 128, zcol0:zcol0 + 256], [], [ztk], "zt" + chan)
            Okl = Ok if isinstance(Ok, list) else [Ok]
            op("act", "activation", Okl, [tmpk], out=tmp[:], in_=O[:], func=AF.Square)
            op("dve", "tensor_reduce", [tmpk], [msk], out=ms[:], in_=tmp[:].rearrange("p (h d) -> p h d", h=4), axis=AX.X, op=ALU.add)
            rstd_from_ss(ms[:], ms[:], 64, [msk], [msk])
            op("dve", "tensor_tensor", Okl + [msk], [tmpk], out=tmp[:].rearrange("p (h d) -> p h d", h=4),
               in0=O[:].rearrange("p (h d) -> p h d", h=4), in1=ms[:].unsqueeze(2).to_broadcast([128, 4, 64]), op=ALU.mult)
            op("pool", "tensor_tensor", [ztk, "normw"], [ztk], out=zt[:].rearrange("p (h d) -> p h d", h=4),
               in0=zt[:].rearrange("p (h d) -> p h d", h=4), in1=normw[:].unsqueeze(1).to_broadcast([128, 4, 64]), op=ALU.mult)
            op("dve", "tensor_tensor", [tmpk, ztk], [outbk], out=outb[:], in0=tmp[:], in1=zt[:], op=ALU.mult)
            dma("pool", mix[t * 128:(t + 1) * 128, col0:col0 + 256], outb[:], [outbk], [], "o_" + chan)

        def phase_GLA(l):
            arena[0] = GLOBAL_TOP
            masku = sb("masku", [128, 128], F32)
            normw = sb("normw", [128, 64], F32)
            dma("sp", masku[:], c_masku, [], ["masku"], "c2")
            dma("sp", normw[:], hn_d[l, 0:1, :].broadcast_to([128, 64]), [], ["normw"], "c3")
            inT = [[sb(f"gin{b}_{k}", [128, 2, 128], F32) for k in range(4)] for b in range(2)]
            ones = sb("ones", [128, 128], F32)
            op("dve", "memset", [], ["ones"], ap=ones[:], constant=1.0)
            cum = [sb(f"cum{g}", [128, 128], F32) for g in range(2)]
            nb = [sb(f"nb{g}", [128, 1], F32) for g in range(2)]
            E = [[sb(f"E{g}_{k}", [128, 128], F32) for k in range(3)] for g in range(2)]
            qt = [sb(f"qt{g}", [128, 128], BF16) for g in range(2)]
            kt = [sb(f"kt{g}", [128, 128], BF16) for g in range(2)]
            kh = [sb(f"kh{g}", [128, 128], BF16) for g in range(2)]
            vb = [sb(f"vb{g}", [128, 128], BF16) for g in range(2)]
            khT = [sb(f"khT{g}", [128, 128], BF16) for g in range(2)]
            vT = [sb(f"vT{g}", [128, 128], BF16) for g in range(2)]
            AT = [sb(f"AT{h}", [128, 128], BF16) for h in range(4)]
            St = [sb(f"St{g}", [128, 64], F32) for g in range(2)]
            Sb = [sb(f"Sb{g}", [128, 64], BF16) for g in range(2)]
            O = [sb(f"O{b}", [128, 256], F32) for b in range(2)]
            zt = [sb(f"zt{b}", [128, 256], F32) for b in range(2)]
            tmp = sb("tmp", [128, 256], F32)
            ms = sb("ms", [128, 4], F32)
            outb = [sb(f"outb{b}", [128, 256], BF16) for b in range(2)]
            for g in range(2):
                op("dve", "memset", [], [f"St{g}"], ap=St[g][:], constant=0.0)
                op("pool", "memset", [], [f"Sb{g}"], ap=Sb[g][:], constant=0.0)
            for t in range(NT):
                b = t % 2
                for k in range(4):
                    dma("sp", inT[b][k][:], glaT[k * 256:(k + 1) * 256, t * 128:(t + 1) * 128].rearrange("(g p) t -> p g t", p=128),
                        [], [f"gin{b}_{k}"], f"gin{b}_{k}")
                for g in range(2):
                    q_, k_, v_, sp_ = (inT[b][k][:, g, :] for k in range(4))
                    rk = [f"gin{b}_{k}" for k in range(4)]
                    ck = f"cum{g}"
                    op("dve", "tensor_tensor_scan", [rk[3], "ones"], [ck], out=cum[g][:], data0=ones[:], data1=sp_, initial=0.0,
                       op0=ALU.mult, op1=ALU.add)
                    op("dve", "tensor_scalar", [ck], [f"nb{g}"], out=nb[g][:], in0=cum[g][:, 127:128], scalar1=-1.0 / 16, scalar2=None, op0=ALU.mult)
                    op("act", "activation", [ck], [f"E{g}_0"], out=E[g][0][:], in_=cum[g][:], func=AF.Exp, scale=-1.0 / 16)
                    op("act", "activation", [ck], [f"E{g}_1"], out=E[g][1][:], in_=cum[g][:], func=AF.Exp, scale=1.0 / 16)
                    op("act", "activation", [ck, f"nb{g}"], [f"E{g}_2"], out=E[g][2][:], in_=cum[g][:], func=AF.Exp, scale=1.0 / 16, bias=nb[g][:])
                    op("dve", "scalar_tensor_tensor", [rk[0], f"E{g}_0"], [f"qt{g}"], out=qt[g][:], in0=q_, scalar=0.125, in1=E[g][0][:],
                       op0=ALU.mult, op1=ALU.mult)
                    op("dve", "tensor_tensor", [rk[1], f"E{g}_1"], [f"kt{g}"], out=kt[g][:], in0=k_, in1=E[g][1][:], op=ALU.mult)
                    op("pool", "tensor_tensor", [rk[1], f"E{g}_2"], [f"khT{g}"], out=khT[g][:], in0=k_, in1=E[g][2][:], op=ALU.mult)
                    op("pool", "tensor_copy", [rk[2]], [f"vT{g}"], out=vT[g][:], in_=v_)
                    trg([(PB[0][:, 0:128], khT[g][:], identb[:]), (PB[0][:, 128:256], vT[g][:], identb[:])],
                        [f"khT{g}", f"vT{g}", "identb"], ["pb0"])
                    op("act", "copy", ["pb0"], [f"kh{g}"], out=kh[g][:], in_=PB[0][:, 0:128])
                    op("act", "copy", ["pb0"], [f"vb{g}"], out=vb[g][:], in_=PB[0][:, 128:256])
                    for hh in range(2):
                        h = 2 * g + hh
                        r = slice(hh * 64, hh * 64 + 64)
                        pa, pak = PF[hh], f"pf{hh}"
                        mmg([(pa[:, 0:128], kt[g][r, :], qt[g][r, :])], [f"kt{g}", f"qt{g}"], [pak])
                        op("dve", "tensor_tensor", [pak, "masku"], [f"AT{h}"], out=AT[h][:], in0=pa[:, 0:128], in1=masku[:], op=ALU.mult)
                        mmg([(PF[2][:, h * 64:(h + 1) * 64], qt[g][r, :], Sb[g][r, :]),
                             (PF[2][:, h * 64:(h + 1) * 64], AT[h][:], vb[g][:, r])],
                            [f"qt{g}", f"Sb{g}", f"AT{h}", f"vb{g}"], [f"pf2_{h}"])
                        mmg([(PF[3][r, g * 64:(g + 1) * 64], kh[g][:, r], vb[g][:, r])], [f"kh{g}", f"vb{g}"], [f"pf3_{g}_{hh}"])
                    op("dve", "scalar_tensor_tensor", [f"St{g}", f"E{g}_0", f"pf3_{g}_0", f"pf3_{g}_1"], [f"St{g}"], out=St[g][:], in0=St[g][:],
                       scalar=E[g][0][:, 127:128], in1=PF[3][:, g * 64:(g + 1) * 64], op0=ALU.mult, op1=ALU.add)
                    op("act", "copy", [f"St{g}"], [f"Sb{g}"], out=Sb[g][:], in_=St[g][:])
                op("act", "copy", [f"pf2_{h}" for h in range(4)], [f"O{b}"], out=O[b][:], in_=PF[2][:, 0:256])
                head_out_stage(t, O[b], f"O{b}", 0, 0, normw, zt[b], f"zt{b}", tmp, "tmp", ms, "ms", outb[b], f"outb{b}", f"gla{b}")
            S_.barrier()

        def phase_GDN(l):
            arena[0] = GLOBAL_TOP
            negu = sb("negu", [128, 128], F32)
            posl = sb("posl", [128, 128], F32)
            normw = sb("normw", [128, 64], F32)
            onesr = sb("onesr", [1, 128], F32)
            dma("sp", negu[:], c_negu, [], ["negu"], "c2")
            dma("sp", posl[:], c_posl, [], ["posl"], "c3")
            dma("sp", normw[:], hn_d[l, 1:2, :].broadcast_to([128, 64]), [], ["normw"], "c4")
            op("dve", "memset", [], ["onesr"], ap=onesr[:], constant=1.0)
            egl = [sb(f"egl{g}", [128, NT], F32) for g in range(2)]
            for g in range(2):
                for hh in range(2):
                    h = 2 * g + hh
                    src = gsct.rearrange("(t p) c -> p t c", p=128)[127:128, :, 8 + h]
                    dma("sp", egl[g][hh * 64:(hh + 1) * 64, :], src.broadcast_to([64, NT]), [], [f"egl{g}"], f"egl{g}{hh}", slow=True)
            inT = [sb(f"din{b}", [128, 6, 128], BF16) for b in range(2)]
            SC = [sb(f"SC{b}", [128, 24], F32) for b in range(2)]
            kbe = [sb(f"kbe{h}", [128, 64], BF16) for h in range(4)]
            khat = [sb(f"khat{h}", [128, 64], BF16) for h in range(4)]
            vbt = [sb(f"vbt{h}", [128, 64], BF16) for h in range(4)]
            gcr = [sb(f"gcr{h}", [1, 128], F32) for h in range(4)]
            DT = [sb(f"DT{h}", [128, 128], F32) for h in range(4)]
            DL = [sb(f"DL{h}", [128, 128], F32) for h in range(4)]
            attnT = [sb(f"attnT{h}", [128, 128], BF16) for h in range(4)]
            Am = [[sb(f"Am{h}_{i}", [128, 128], BF16) for i in range(2)] for h in range(4)]
            Cm = [[sb(f"Cm{h}_{i}", [128, 128], BF16) for i in range(2)] for h in range(4)]
            Ym = [[sb(f"Ym{h}_{i}", [128, 128], BF16) for i in range(2)] for h in range(4)]
            u = [sb(f"u{h}", [128, 64], F32) for h in range(4)]
            wT = [sb(f"wT{g}", [128, 128], BF16) for g in range(2)]
            vn = [sb(f"vn{h}", [128, 64], BF16) for h in range(4)]
            o1 = [sb(f"o1{h}", [128, 64], F32) for h in range(4)]
            St = [sb(f"St{g}", [128, 64], F32) for g in range(2)]
            Sb = [sb(f"Sb{g}", [128, 64], BF16) for g in range(2)]
            O = [sb(f"O{b}", [128, 256], F32) for b in range(2)]
            zt = [sb(f"zt{b}", [128, 256], F32) for b in range(2)]
            tmp = sb("tmp", [128, 256], F32)
            ms = sb("ms", [128, 4], F32)
            outb = [sb(f"outb{b}", [128, 256], BF16) for b in range(2)]
            for g in range(2):
                op("dve", "memset", [], [f"St{g}"], ap=St[g][:], constant=0.0)
                op("pool", "memset", [], [f"Sb{g}"], ap=Sb[g][:], constant=0.0)
            pfi = [0]

            def npf():
                pfi[0] = (pfi[0] + 1) % 6
                return PF[pfi[0]], f"pf{pfi[0]}"

            for t in range(NT):
                b = t % 2
                dma("sp", inT[b][:], gdnT[:, t * 128:(t + 1) * 128].rearrange("(g p) t -> p g t", p=128), [], [f"din{b}"], f"din{b}")
                dma("sp", SC[b][:], gsct[t * 128:(t + 1) * 128, :], [], [f"SC{b}"], f"SC{b}")
                ink, sck = f"din{b}", f"SC{b}"
                col = lambda q, h: SC[b][:, 4 * q + h:4 * q + h + 1]

                def qT(h): return inT[b][(h % 2) * 64:(h % 2) * 64 + 64, 0 + h // 2, :]
                def kT(h): return inT[b][(h % 2) * 64:(h % 2) * 64 + 64, 2 + h // 2, :]
                def vT(h): return inT[b][(h % 2) * 64:(h % 2) * 64 + 64, 4 + h // 2, :]

                for h in range(4):
                    hh = h % 2
                    pbk = f"pb{hh}"
                    trg([(PB[hh][:, 0:64], kT(h), identb[hh * 64:hh * 64 + 64, hh * 64:hh * 64 + 64]),
                         (PB[hh][:, 64:128], vT(h), identb[hh * 64:hh * 64 + 64, hh * 64:hh * 64 + 64])], [ink, "identb"], [pbk])
                    op("dve", "tensor_scalar", [pbk, sck], [f"kbe{h}"], out=kbe[h][:], in0=PB[hh][:, 0:64], scalar1=col(3, h), scalar2=None, op0=ALU.mult)
                    op("act", "activation", [pbk, sck], [f"khat{h}"], out=khat[h][:], in_=PB[hh][:, 0:64], func=AF.Copy, scale=col(4, h))
                    op("act", "activation", [pbk, sck], [f"vbt{h}"], out=vbt[h][:], in_=PB[hh][:, 64:128], func=AF.Copy, scale=col(1, h))
                    p, pk = npf()
                    mmg([(p[0:1, 0:128], col(0, h), identf[:])], [sck, "identf"], [pk])
                    op("act", "copy", [pk], [f"gcr{h}"], out=gcr[h][:], in_=p[0:1, 0:128])
                    p, pk = npf()
                    mmg([(p[:, 0:128], onesr[:], gcr[h][:]), (p[:, 0:128], identf[:], negu[:])], ["onesr", f"gcr{h}", "identf", "negu"], [pk])
                    op("act", "activation", [pk, sck], [f"DT{h}"], out=DT[h][:], in_=p[:, 0:128], func=AF.Exp, bias=col(5, h))
                    p, pk = npf()
                    mmg([(p[:, 0:128], onesr[:], gcr[h][:]), (p[:, 0:128], identf[:], posl[:])], ["onesr", f"gcr{h}", "identf", "posl"], [pk])
                    op("act", "activation", [pk, sck], [f"DL{h}"], out=DL[h][:], in_=p[:, 0:128], func=AF.Exp, scale=-1.0, bias=col(0, h))
                    p, pk = npf()
                    mmg([(p[:, 0:128], kT(h), qT(h))], [ink], [pk])
                    op("dve", "tensor_tensor", [pk, f"DT{h}"], [f"attnT{h}"], out=attnT[h][:], in0=p[:, 0:128], in1=DT[h][:], op=ALU.mult)
                    p, pk = npf()
                    mmg([(p[:, 0:128], kT(h), kT(h))], [ink], [pk])
                    op("dve", "scalar_tensor_tensor", [pk, sck, f"DL{h}"], [f"Cm{h}_0"], out=Cm[h][0][:], in0=p[:, 0:128], scalar=col(1, h),
                       in1=DL[h][:], op0=ALU.mult, op1=ALU.mult)
                    trg([(PB[hh][:, 128:256], Cm[h][0][:], identb[:])], [f"Cm{h}_0", "identb"], [pbk + "b"])
                    op("act", "copy", [pbk + "b"], [f"Am{h}_0"], out=Am[h][0][:], in_=PB[hh][:, 128:256])
                    op("pool", "tensor_tensor", [f"Am{h}_0", "identb"], [f"Ym{h}_0"], out=Ym[h][0][:], in0=identb[:], in1=Am[h][0][:], op=ALU.subtract)
                for k in range(1, 7):
                    a0, a1 = (k - 1) % 2, k % 2
                    for h in range(4):
                        p, pk = npf()
                        mmg([(p[:, 0:128], Am[h][a0][:], Cm[h][a0][:])], [f"Am{h}_{a0}", f"Cm{h}_{a0}"], [pk])
                        op("act" if h % 2 == 0 else "dve", "copy" if h % 2 == 0 else "tensor_copy", [pk], [f"Cm{h}_{a1}"], out=Cm[h][a1][:], in_=p[:, 0:128])
                        if k < 6:
                            p2, pk2 = npf()
                            mmg([(p2[:, 0:128], Cm[h][a0][:], Am[h][a0][:])], [f"Am{h}_{a0}", f"Cm{h}_{a0}"], [pk2])
                            op("dve" if h % 2 == 0 else "act", "tensor_copy" if h % 2 == 0 else "copy", [pk2], [f"Am{h}_{a1}"], out=Am[h][a1][:], in_=p2[:, 0:128])
                    for h in range(4):
                        p, pk = npf()
                        mmg([(p[:, 0:128], identb[:], Ym[h][a0][:]), (p[:, 0:128], Cm[h][a1][:], Ym[h][a0][:])],
                            ["identb", f"Ym{h}_{a0}", f"Cm{h}_{a1}"], [pk])
                        op("act" if h % 2 == 0 else "dve", "copy" if h % 2 == 0 else "tensor_copy", [pk], [f"Ym{h}_{a1}"], out=Ym[h][a1][:], in_=p[:, 0:128])
                YF = 0
                for h in range(4):
                    g, hh = h // 2, h % 2
                    r = slice(hh * 64, hh * 64 + 64)
                    p, pk = npf()
                    mmg([(p[:, 0:64], Ym[h][YF][:], vbt[h][:])], [f"Ym{h}_{YF}", f"vbt{h}"], [pk])
                    op("act", "copy", [pk], [f"u{h}"], out=u[h][:], in_=p[:, 0:64])
                    p, pk = npf()
                    mmg([(p[r, 0:128], kbe[h][:], Ym[h][YF][:])], [f"Ym{h}_{YF}", f"kbe{h}"], [pk])
                    op("act", "copy", [pk], [f"wT{g}_{hh}"], out=wT[g][r, :], in_=p[r, 0:128])
                for h in range(4):
                    g, hh = h // 2, h % 2
                    r = slice(hh * 64, hh * 64 + 64)
                    p, pk = npf()
                    mmg([(p[:, 0:64], wT[g][r, :], Sb[g][r, :])], [f"wT{g}_{hh}", f"Sb{g}"], [pk])
                    op("dve", "tensor_tensor", [f"u{h}", pk], [f"vn{h}"], out=vn[h][:], in0=u[h][:], in1=p[:, 0:64], op=ALU.subtract)
                    p, pk = npf()
                    mmg([(p[:, 0:64], qT(h), Sb[g][r, :])], [ink, f"Sb{g}"], [pk])
                    op("act", "activation", [pk, sck], [f"o1{h}"], out=o1[h][:], in_=p[:, 0:64], func=AF.Copy, scale=col(2, h))
                    p, pk = npf()
                    mmg([(p[:, 0:64], attnT[h][:], vn[h][:])], [f"attnT{h}", f"vn{h}"], [pk])
                    op("dve", "tensor_tensor", [f"o1{h}", pk], [f"O{b}_{h}"], out=O[b][:, h * 64:(h + 1) * 64], in0=o1[h][:], in1=p[:, 0:64], op=ALU.add)
                for g in range(2):
                    p, pk = npf()
                    for hh in range(2):
                        h = 2 * g + hh
                        r = slice(hh * 64, hh * 64 + 64)
                        mmg([(p[r, 0:64], khat[h][:], vn[h][:])], [f"khat{h}", f"vn{h}"], [pk])
                    op("dve", "scalar_tensor_tensor", [f"St{g}", f"egl{g}", pk], [f"St{g}"], out=St[g][:], in0=St[g][:],
                       scalar=egl[g][:, t:t + 1], in1=p[:, 0:64], op0=ALU.mult, op1=ALU.add)
                    op("act", "copy", [f"St{g}"], [f"Sb{g}"], out=Sb[g][:], in_=St[g][:])
                head_out_stage(t, O[b], [f"O{b}_{h}" for h in range(4)], 256, 256, normw, zt[b], f"zt{b}", tmp, "tmp", ms, "ms", outb[b], f"outb{b}", f"gdn{b}")
            S_.barrier()


        def phase_MOBA(l):
            arena[0] = GLOBAL_TOP
            qa = [sb(f"qa{i}", [128, S], BF16) for i in range(2)]
            ka = [sb(f"ka{i}", [128, S], BF16) for i in range(2)]
            Va = [sb(f"Va{i}", [128, NT, 65], BF16) for i in range(2)]
            ksf = [sb(f"ksf{i}", [64, 32], F32) for i in range(2)]
            ksb = [sb(f"ksb{i}", [64, 32], BF16) for i in range(2)]
            mb = sb("mb", [128, 32 * 32], F32)
            cm = sb("cm", [128, 4, 512], BF16)
            dma("sp", mb[:], c_mb, [], ["mb"], "c2")
            dma("sp", cm[:], c_cm.rearrange("r p q -> p r q"), [], ["cm"], "c3")
            for i in range(2):
                dma("sp", ka[i][64:128, :], c_onehot, [], [f"ka{i}_oh"], f"c4{i}")
                dma("sp", qa[i][96:128, :], c_onehot[32:64, :], [], [f"qa{i}_z"], f"c5{i}")
                op("pool", "memset", [], [f"Va{i}_1"], ap=Va[i][:, :, 64:65], constant=1.0)
            gm = [sb(f"gm{i}", [128, 32], F32) for i in range(2)]
            top8 = [sb(f"top8{i}", [128, 8], F32) for i in range(2)]
            bia = [sb(f"bia{i}", [128, 32], BF16) for i in range(2)]
            Pt = [sb(f"Pt{i}", [128, 512], BF16) for i in range(4)]
            osb = [sb(f"osb{i}", [128, 512], F32) for i in range(2)]
            for i in range(2):
                op("pool", "memset", [], [f"osb{i}"], ap=osb[i][:], constant=0.0)
            rden = [sb(f"rden{i}", [128, 1], F32) for i in range(2)]
            ob = [sb(f"ob{i}", [128, 64], BF16) for i in range(2)]
            of = [sb(f"of{i}", [128, 65], F32) for i in range(2)]
            def load_head(h):
                i = h % 2
                dma("sp", qa[i][0:64, :], mqT[h * 64:(h + 1) * 64, :], [], [f"qa{i}"], f"qa{i}")
                dma("sp", ka[i][0:64, :], mkT[h * 64:(h + 1) * 64, :], [], [f"ka{i}"], f"ka{i}")
                dma("sp", Va[i][:, :, 0:64], mvs[:, h * 64:(h + 1) * 64].rearrange("(t p) d -> p t d", p=128), [], [f"Va{i}"], f"Va{i}")
                dma("sp", ksf[i][:], mksum[h * 64:(h + 1) * 64, :], [], [f"ksf{i}"], f"ksf{i}")
                op("dve", "tensor_copy", [f"ksf{i}"], [f"ksb{i}"], out=ksb[i][:], in_=ksf[i][:])

            def gate_tile(h, t):
                i = h % 2
                j = t % 2
                own = t // 2
                mmg([(PF[4][:, j * 32:(j + 1) * 32], qa[i][0:64, t * 128:(t + 1) * 128], ksb[i][:, :])], [f"qa{i}", f"ksb{i}"], [f"pf4_{j}"])
                op("dve", "tensor_tensor", [f"pf4_{j}", "mb"], [f"gm{j}"], out=gm[j][:], in0=PF[4][:, j * 32:(j + 1) * 32],
                   in1=mb[:, own * 32:(own + 1) * 32], op=ALU.add)
                op("dve", "max", [f"gm{j}"], [f"top8{j}"], out=top8[j][:], in_=gm[j][:])
                op("dve", "tensor_scalar", [f"top8{j}"], [f"top8{j}"], out=top8[j][:, 3:4], in0=top8[j][:, 3:4], scalar1=-BIG / 2, scalar2=None, op0=ALU.max)
                op("dve", "tensor_scalar", [f"gm{j}", f"top8{j}"], [f"gm{j}"], out=gm[j][:], in0=gm[j][:], scalar1=top8[j][:, 3:4], scalar2=BIG,
                   op0=ALU.is_ge, op1=ALU.mult)
                op("dve", "tensor_scalar", [f"gm{j}"], [f"bia{j}"], out=bia[j][:], in0=gm[j][:], scalar1=-BIG, scalar2=None, op0=ALU.add)
                trg([(PB[j][64:96, 0:128], bia[j][:], identb[:])], [f"bia{j}", "identb"], [f"pb{j}"])
                op("act", "copy", [f"pb{j}"], [f"qa{i}_b{t}"], out=qa[i][64:96, t * 128:(t + 1) * 128], in_=PB[j][64:96, 0:128])

            def out_stage(h, G):
                oi = G % 2
                op("act", "copy", [f"pf{2 + oi}"], [f"osb{oi}"], out=osb[oi][0:65, :], in_=PF[2 + oi][0:65, :])
                for tt in range(4):
                    t = G * 4 + tt
                    j = tt % 2
                    mmg([(PF[4][:, 128:256], osb[oi][:, tt * 128:(tt + 1) * 128], identf[:])], [f"osb{oi}", "identf"], ["pf4"])
                    op("dve", "tensor_copy", ["pf4"], [f"of{j}"], out=of[j][:], in_=PF[4][:, 128:193])
                    op("dve", "reciprocal", [f"of{j}"], [f"rden{j}"], out=rden[j][:], in_=of[j][:, 64:65])
                    op("dve", "tensor_scalar", [f"of{j}", f"rden{j}"], [f"ob{j}"], out=ob[j][:], in0=of[j][:, 0:64],
                       scalar1=rden[j][:], scalar2=None, op0=ALU.mult)
                    dma("sp", mix[t * 128:(t + 1) * 128, 512 + h * 64:512 + (h + 1) * 64], ob[j][:], [f"ob{j}"], [], f"o_ob{j}")

            steps = [(G, kt_) for G in range(NG) for kt_ in range(4 * G + 4)]
            NH = 8
            load_head(0)
            for t in range(NT):
                gate_tile(0, t)
            for h in range(NH):
                i = h % 2
                pend_gate = list(range(NT)) if h + 1 < NH else []
                if h + 1 < NH:
                    load_head(h + 1)

                def emit_st(sidx, i=i):
                    G, kt_ = steps[sidx]
                    pi = (0, 1, 5)[sidx % 3]
                    qk = [f"qa{i}", f"qa{i}_z"] + [f"qa{i}_b{t}" for t in range(G * 4, G * 4 + 4)]
                    mmg([(PF[pi][:], ka[i][:, kt_ * 128:(kt_ + 1) * 128], qa[i][:, G * 512:(G + 1) * 512])],
                        [f"ka{i}", f"ka{i}_oh"] + qk, [f"pf{pi}"])

                emit_st(0)
                if len(steps) > 1:
                    emit_st(1)
                pend_out = []
                for sidx, (G, kt_) in enumerate(steps):
                    if sidx + 2 < len(steps):
                        emit_st(sidx + 2)
                    pi = (0, 1, 5)[sidx % 3]
                    pti = sidx % 4
                    oi = G % 2
                    nk = 4 * G + 4
                    op("act", "activation", [f"pf{pi}"], [f"Pt{pti}"], out=Pt[pti][:], in_=PF[pi][:], func=AF.Exp)
                    if kt_ >= 4 * G:
                        op("pool", "tensor_tensor", [f"Pt{pti}", "cm"], [f"Pt{pti}"], out=Pt[pti][:], in0=Pt[pti][:], in1=cm[:, kt_ - 4 * G, :], op=ALU.mult)

                    def f(e, o=PF[2 + oi][0:65, :], l_=Va[i][:, kt_, :], r_=Pt[pti][:], st=(kt_ == 0), sp=(kt_ == nk - 1)):
                        return e.matmul(out=o, lhsT=l_, rhs=r_, start=st, stop=sp)
                    S_.add("pe", f, reads=[f"Va{i}", f"Va{i}_1", f"Pt{pti}"], writes=[f"pf{2 + oi}"])
                    for po in list(pend_out):
                        if sidx >= po[1]:
                            out_stage(h, po[0])
                            pend_out.remove(po)
                    if kt_ == nk - 1:
                        pend_out.append((G, sidx + 2))
                    if sidx % 8 == 4 and pend_gate:
                        gate_tile(h + 1, pend_gate.pop(0))
                for po in pend_out:
                    out_stage(h, po[0])
                while pend_gate:
                    gate_tile(h + 1, pend_gate.pop(0))
            S_.barrier()

        def phase_C1(l, xsrc):
            arena[0] = GLOBAL_TOP
            wo = sb("wo", [128, 8, D], BF16)
            gpost = sb("gpost", [128, D], F32)
            gpre2 = sb("gpre2", [128, D], F32)
            for c in range(8):
                dma("sp" if c % 2 == 0 else "pool", wo[:, c, :], wo_d[l, c * 128:(c + 1) * 128, :], [], [f"wo{c}"], f"w{c}")
            WO = [f"wo{c}" for c in range(8)]
            dma("sp", gpost[:], gains_d[l, 1:2, :].broadcast_to([128, D]), [], ["gpost"], "c2")
            dma("sp", gpre2[:], gains_d[l, 2:3, :].broadcast_to([128, D]), [], ["gpre2"], "c3")
            mt = [sb(f"mt{i}", [128, D], BF16) for i in range(2)]
            mT = [sb(f"mT{i}", [128, 8, 128], BF16) for i in range(2)]
            xt = [sb(f"xt{i}", [128, D], F32) for i in range(2)]
            yn = [sb(f"yn{i}", [128, D], F32) for i in range(2)]
            sq = sb("sq", [128, D], F32)
            ss = [sb(f"ss{i}", [128, 2], F32) for i in range(2)]
            rs = [sb(f"rs{i}", [128, 1], F32) for i in range(2)]
            hb = [sb(f"hb{i}", [128, D], BF16) for i in range(2)]
            for t in range(NT):
                b = t % 2
                rows = slice(t * 128, (t + 1) * 128)
                dma("sp", mt[b][:], mix[rows, :], [], [f"mt{b}"], f"mt{b}")
                dma("sp", xt[b][:], xsrc[rows, :], [], [f"xt{b}"], f"xt{b}")
                for half in range(2):
                    trg([(PB[half][:, c * 128:(c + 1) * 128], mt[b][:, (half * 4 + c) * 128:(half * 4 + c + 1) * 128], identb[:]) for c in range(4)],
                        [f"mt{b}", "identb"], [f"pb{half}"])
                    src = PB[half][:, 0:512].rearrange("p (c t) -> p c t", c=4)
                    if half == 0:
                        op("act", "copy", [f"pb{half}"], [f"mT{b}_{half}"], out=mT[b][:, 0:4, :], in_=src)
                    else:
                        op("dve", "tensor_copy", [f"pb{half}"], [f"mT{b}_{half}"], out=mT[b][:, 4:8, :], in_=src)
                for n in range(2):
                    pi = (2 * t + n) % 4
                    mmg([(PF[pi][:], mT[b][:, c, :], wo[:, c, n * 512:(n + 1) * 512]) for c in range(8)], WO + [f"mT{b}_0", f"mT{b}_1"], [f"pf{pi}"])
                    op("act", "activation", [f"pf{pi}"], ["sq", f"ss{b}_{n}"], out=sq[:, n * 512:(n + 1) * 512], in_=PF[pi][:], func=AF.Square,
                       accum_out=ss[b][:, n:n + 1])
                op("dve", "tensor_tensor", [f"ss{b}_0", f"ss{b}_1"], [f"rs{b}"], out=rs[b][:], in0=ss[b][:, 0:1], in1=ss[b][:, 1:2], op=ALU.add)
                rstd_from_ss(rs[b][:], rs[b][:], D, [f"rs{b}"], [f"rs{b}"])
                for n in range(2):
                    pi = (2 * t + n) % 4
                    op("dve", "scalar_tensor_tensor", [f"pf{pi}", f"rs{b}", "gpost"], [f"yn{b}_{n}"], out=yn[b][:, n * 512:(n + 1) * 512], in0=PF[pi][:],
                       scalar=rs[b][:], in1=gpost[:, n * 512:(n + 1) * 512], op0=ALU.mult, op1=ALU.mult)
                op("pool", "tensor_tensor", [f"yn{b}_0", f"yn{b}_1", f"xt{b}"], [f"yn{b}"], out=yn[b][:], in0=yn[b][:], in1=xt[b][:], op=ALU.add)
                dma("pool", x1s[rows, :], yn[b][:], [f"yn{b}"], [], f"o_yn{b}")
                op("act", "activation", [f"yn{b}"], ["sq", f"ss{b}_0"], out=sq[:], in_=yn[b][:], func=AF.Square, accum_out=ss[b][:, 0:1])
                rstd_from_ss(ss[b][:, 0:1], rs[b][:], D, [f"ss{b}_0"], [f"rs{b}"])
                op("dve", "scalar_tensor_tensor", [f"yn{b}", f"rs{b}", "gpre2"], [f"hb{b}"], out=hb[b][:], in0=yn[b][:], scalar=rs[b][:], in1=gpre2[:],
                   op0=ALU.mult, op1=ALU.mult)
                dma("pool", h2s[rows, :], hb[b][:], [f"hb{b}"], [], f"o_hb{b}")
            S_.barrier()

        def phase_C2(l, dst):
            arena[0] = GLOBAL_TOP
            wg = sb("wg", [128, 8, DFF], BF16)
            wu = sb("wu", [128, 8, DFF], BF16)
            wd = sb("wd", [128, NFC, D], BF16)
            gpost = sb("gpost", [128, D], F32)
            for c in range(8):
                dma("sp", wg[:, c, :], wg_d[l, c * 128:(c + 1) * 128, :], [], [f"wg{c}"], f"w{c}")
                dma("pool", wu[:, c, :], wu_d[l, c * 128:(c + 1) * 128, :], [], [f"wu{c}"], f"wu{c}")
            for f_ in range(NFC):
                dma("sp" if f_ % 2 else "pool", wd[:, f_, :], wd_d[l, f_ * 128:(f_ + 1) * 128, :], [], [f"wd{f_}"], f"wd{f_ % 4}")
            WG = [f"wg{c}" for c in range(8)]
            WU = [f"wu{c}" for c in range(8)]
            WD = [f"wd{f_}" for f_ in range(NFC)]
            dma("sp", gpost[:], gains_d[l, 3:4, :].broadcast_to([128, D]), [], ["gpost"], "c2")
            ht = [sb(f"ht{i}", [128, D], BF16) for i in range(2)]
            hT = sb("hT", [128, 8, 512], BF16)
            aT = sb("aT", [128, NFC, 512], BF16)
            th = [sb(f"th{i}", [128, 512], F32) for i in range(2)]
            us = [sb(f"us{i}", [128, 512], F32) for i in range(2)]
            xt = [sb(f"xt{i}", [128, D], F32) for i in range(2)]
            yn = [sb(f"yn{i}", [128, D], F32) for i in range(2)]
            sq = sb("sq", [128, 512], F32)
            ss = [sb(f"ss{i}", [128, 2], F32) for i in range(2)]
            rs = [sb(f"rs{i}", [128, 1], F32) for i in range(2)]
            for G in range(NG):
                for tt in range(4):
                    t = G * 4 + tt
                    b = t % 2
                    dma("sp", ht[b][:], h2s[t * 128:(t + 1) * 128, :], [], [f"ht{b}"], f"ht{b}")
                    for half in range(2):
                        trg([(PB[half][:, c * 128:(c + 1) * 128], ht[b][:, (half * 4 + c) * 128:(half * 4 + c + 1) * 128], identb[:]) for c in range(4)],
                            [f"ht{b}", "identb"], [f"pb{half}"])
                        src = PB[half][:, 0:512].rearrange("p (c t) -> p c t", c=4)
                        dst_ = hT[:, half * 4:(half + 1) * 4, tt * 128:(tt + 1) * 128]
                        if half == 0:
                            op("act", "copy", [f"pb{half}"], [f"hT_{tt}"], out=dst_, in_=src)
                        else:
                            op("dve", "tensor_copy", [f"pb{half}"], [f"hT_{tt}"], out=dst_, in_=src)
                hk = [f"hT_{tt}" for tt in range(4)]
                for f_ in range(NFC):
                    b = f_ % 2
                    pg, pu = PF[2 * b], PF[2 * b + 1]
                    mmg([(pg[:], wg[:, c, f_ * 128:(f_ + 1) * 128], hT[:, c, :]) for c in range(8)], WG + hk, [f"pf{2 * b}"])
                    mmg([(pu[:], wu[:, c, f_ * 128:(f_ + 1) * 128], hT[:, c, :]) for c in range(8)], WU + hk, [f"pf{2 * b + 1}"])
                    op("act", "activation", [f"pf{2 * b}"], [f"th{b}"], out=th[b][:], in_=pg[:], func=AF.Tanh, scale=0.5)
                    op("act", "copy", [f"pf{2 * b + 1}"], [f"us{b}"], out=us[b][:], in_=pu[:])
                    op("dve", "scalar_tensor_tensor", [f"th{b}", f"pf{2 * b}"], [f"th{b}"], out=th[b][:], in0=th[b][:], scalar=1.0, in1=pg[:],
                       op0=ALU.add, op1=ALU.mult)
                    op("dve", "scalar_tensor_tensor", [f"th{b}", f"us{b}"], [f"aT_{f_}"], out=aT[:, f_, :], in0=th[b][:], scalar=0.5, in1=us[b][:],
                       op0=ALU.mult, op1=ALU.mult)
                ak = [f"aT_{f_}" for f_ in range(NFC)]
                for tt in range(4):
                    t = G * 4 + tt
                    b = t % 2
                    rows = slice(t * 128, (t + 1) * 128)
                    dma("sp", xt[b][:], x1s[rows, :], [], [f"xt{b}"], f"xt{b}")
                    for n in range(2):
                        pi = 4 + n
                        mmg([(PF[pi][:], aT[:, f_, tt * 128:(tt + 1) * 128], wd[:, f_, n * 512:(n + 1) * 512]) for f_ in range(NFC)], WD + ak, [f"pf{pi}"])
                        op("act", "activation", [f"pf{pi}"], ["sq", f"ss{b}_{n}"], out=sq[:], in_=PF[pi][:], func=AF.Square, accum_out=ss[b][:, n:n + 1])
                    op("dve", "tensor_tensor", [f"ss{b}_0", f"ss{b}_1"], [f"rs{b}"], out=rs[b][:], in0=ss[b][:, 0:1], in1=ss[b][:, 1:2], op=ALU.add)
                    rstd_from_ss(rs[b][:], rs[b][:], D, [f"rs{b}"], [f"rs{b}"])
                    for n in range(2):
                        pi = 4 + n
                        op("dve", "scalar_tensor_tensor", [f"pf{pi}", f"rs{b}", "gpost"], [f"yn{b}_{n}"], out=yn[b][:, n * 512:(n + 1) * 512], in0=PF[pi][:],
                           scalar=rs[b][:], in1=gpost[:, n * 512:(n + 1) * 512], op0=ALU.mult, op1=ALU.mult)
                    op("pool", "tensor_tensor", [f"yn{b}_0", f"yn{b}_1", f"xt{b}"], [f"yn{b}"], out=yn[b][:], in0=yn[b][:], in1=xt[b][:], op=ALU.add)
                    dma("pool", dst[rows, :], yn[b][:], [f"yn{b}"], [], f"o_yn{b}")
            S_.barrier()

        for l in range(depth):
            xsrc = x_in if l == 0 else xs
            import os
            PH = os.environ.get("PHASES", "A,GLA,GDN,MOBA,C1,C2").split(",")
            if "A" in PH: phase_A(l, xsrc)
            if "GLA" in PH: phase_GLA(l)
            if "GDN" in PH: phase_GDN(l)
            if "MOBA" in PH: phase_MOBA(l)
            if "C1" in PH: phase_C1(l, xsrc)
            if "C2" in PH: phase_C2(l, y_out if l == depth - 1 else xs)

        semkeys = S_.emit()
        sems = [es.enter_context(nc.semaphore(f"s{i}")) for i in range(len(semkeys))]
        block = es.enter_context(nc.Block())
        S_.run(semkeys, sems, block)
    return nc


def _consts(S):
    j = np.arange(128)[:, None]
    i = np.arange(128)[None, :]
    c = {}
    c["c_identb"] = np.eye(128, dtype=np.float32).astype(BF)
    c["c_identf"] = np.eye(128, dtype=np.float32)
    c["c_masku"] = (j <= i).astype(np.float32)
    c["c_negu"] = np.where(j <= i, 0.0, -BIG).astype(np.float32)
    c["c_posl"] = np.where(i < j, 0.0, BIG).astype(np.float32)
    c["c_triu"] = (j <= i).astype(np.float32)
    c["c_trisu"] = (j > i).astype(np.float32)
    c["c_bones"] = ((j // 64) == (i // 64)).astype(np.float32)
    half = 32
    inv = (np.float32(10000.0) ** (-np.arange(half, dtype=np.float32) / np.float32(half))).astype(np.float32)
    pos = np.arange(S, dtype=np.float32)
    ang = (pos[:, None] * inv[None, :]).astype(np.float32)
    cos = np.cos(ang).astype(np.float32).T
    sin = np.sin(ang).astype(np.float32).T
    p = np.arange(128)
    cosP = cos[p % 32]
    sinP = sin[p % 32] * np.where((p % 64) < 32, -1.0, 1.0)[:, None]
    c["c_rope"] = np.stack([cosP * 0.125, sinP * 0.125, cosP, sinP]).astype(np.float32)
    c["c_onehot"] = (np.arange(64)[:, None] == (np.arange(S)[None, :] // 256)).astype(np.float32).astype(BF)
    own = np.arange(32)[:, None]
    jb = np.arange(32)[None, :]
    mbt = np.where(jb < own, 0.0, np.where(jb == own, BIG, -BIG)).astype(np.float32)
    c["c_mb"] = np.broadcast_to(mbt.reshape(1, 32 * 32), (128, 32 * 32)).copy()
    cm = np.zeros((4, 128, 512), np.float32)
    key = np.arange(128)[:, None]
    q = np.arange(512)[None, :]
    for r in range(4):
        cm[r] = ((r * 128 + key) <= q).astype(np.float32)
    c["c_cm"] = cm.astype(BF)
    return c


def _prep_weights(inp, depth):
    w = {}
    w["win"] = np.ascontiguousarray(inp["w_in"][:depth][:, :, WIN_IDX]).astype(BF)
    wga = np.zeros((depth, 33, 256), np.float32)
    wga[:, 0:16, :] = inp["gla_w_gate"][:depth]
    wga[:, 32, :] = inp["gla_b_gate"][:depth]
    w["wga"] = wga.astype(BF)
    w["wo"] = np.asarray(inp["w_o"][:depth]).astype(BF)
    w["wg"] = np.asarray(inp["ffn_w_gate"][:depth]).astype(BF)
    w["wu"] = np.asarray(inp["ffn_w_up"][:depth]).astype(BF)
    w["wd"] = np.asarray(inp["ffn_w_down"][:depth]).astype(BF)
    w["gains"] = np.stack([inp["norm_mix_pre"][:depth], inp["norm_mix_post"][:depth], inp["norm_ffn_pre"][:depth],
                           inp["norm_ffn_post"][:depth]], axis=1).astype(np.float32)
    w["hn"] = np.stack([inp["gla_norm"][:depth], inp["gdn_norm"][:depth]], axis=1).astype(np.float32)
    w["conv"] = np.ascontiguousarray(np.transpose(inp["gdn_conv"][:depth], (0, 2, 1))).astype(np.float32)
    w["gsc"] = np.stack([inp["gdn_a_log"][:depth], inp["gdn_dt_bias"][:depth]], axis=1).astype(np.float32)
    return w


def run(inputs, n_cores=8, dbg=False, depth=None):
    inp = {k: np.asarray(v) for k, v in inputs.items()}
    x = inp["x"]
    B, S, _ = x.shape
    depth = depth or inp["w_in"].shape[0]
    nc = build(S, depth, dbg=dbg)
    shared = {}
    shared.update(_consts(S))
    shared.update(_prep_weights(inp, depth))
    in_maps = []
    for c in range(n_cores):
        m = dict(shared)
        m["x"] = np.ascontiguousarray(x[c % B]).astype(np.float32)
        in_maps.append(m)
    res = run_bass_kernel_spmd(nc, in_maps, core_ids=list(range(n_cores)))
    return res


def kernel(**inputs):
    res = run(inputs)
    B = np.asarray(inputs["x"]).shape[0]
    out = np.stack([res.results[b]["y"] for b in range(B)], axis=0)
    return out.astype(np.float32)
```

```python
import numpy as np
import ml_dtypes
import concourse.bass as bass
import concourse.mybir as mybir
from concourse.bass_utils import run_bass_kernel_spmd
from contextlib import ExitStack

F32 = mybir.dt.float32
BF16 = mybir.dt.bfloat16
AF = mybir.ActivationFunctionType
ALU = mybir.AluOpType
AX = mybir.AxisListType
BF = ml_dtypes.bfloat16

D = 1024
DFF = 2816
NFC = DFF // 128
BIG = 30000.0
ENGS = ("pe", "act", "dve", "pool", "sp")


class _Op:
    __slots__ = ("eng", "fn", "deps", "dma", "chan", "idx", "sig", "waits", "need")

    def __init__(self, eng, fn, dma, chan, idx):
        self.eng, self.fn, self.dma, self.chan, self.idx = eng, fn, dma, chan, idx
        self.deps = ()
        self.sig = None
        self.need = False
        self.waits = ()


class Sched:
    def __init__(self):
        self.ops = []
        self.lastw = {}
        self.readers = {}
        self.last_on_eng = {}
        self.last_on_chan = {}

    @staticmethod
    def _norm(keys):
        return [k[:3] if (k[:2] in ("pf", "pb") and len(k) > 2 and k[2].isdigit()) else k for k in keys]

    def add(self, eng, fn, reads=(), writes=(), dma=False, chan=None):
        reads = self._norm(reads)
        writes = self._norm(writes)
        op = _Op(eng, fn, dma, chan if dma else None, len(self.ops))
        deps = set()
        for k in reads:
            w = self.lastw.get(k)
            if w is not None:
                deps.add(w)
        for k in writes:
            w = self.lastw.get(k)
            if w is not None:
                deps.add(w)
            for r in self.readers.get(k, ()):
                deps.add(r)
        op.deps = deps
        for k in reads:
            self.readers.setdefault(k, []).append(op.idx)
        for k in writes:
            self.lastw[k] = op.idx
            self.readers[k] = []
        self.ops.append(op)
        if dma:
            self.last_on_chan[chan] = op.idx
        else:
            self.last_on_eng[eng] = op.idx
        return op

    def barrier(self):
        allprev = set(self.last_on_eng.values()) | set(self.last_on_chan.values())
        for e in ENGS:
            op = _Op(e, None, False, None, len(self.ops))
            op.deps = set(allprev)
            self.ops.append(op)
            self.last_on_eng[e] = op.idx
        self.lastw.clear()
        self.readers.clear()

    def emit(self):
        ops = self.ops
        for op in ops:
            for d in op.deps:
                ops[d].need = True
        cnt = {}
        for op in ops:
            if op.fn is None:
                continue
            if op.dma:
                key = ("chan", op.chan)
                cnt[key] = cnt.get(key, 0) + 16
                op.sig = (key, cnt[key])
            elif op.need:
                key = ("eng", op.eng)
                cnt[key] = cnt.get(key, 0) + 1
                op.sig = (key, cnt[key])
        water = {e: {} for e in ENGS}
        for op in ops:
            need = {}
            for d in op.deps:
                dop = ops[d]
                if dop.sig is None:
                    continue
                k, v = dop.sig
                if need.get(k, 0) < v:
                    need[k] = v
            wm = water[op.eng]
            waits = []
            for k, v in need.items():
                if wm.get(k, 0) < v:
                    wm[k] = v
                    waits.append((k, v))
            op.waits = waits
        return sorted(cnt.keys())

    def run(self, semkeys, sems, block):
        ops = self.ops
        semmap = dict(zip(semkeys, sems))

        def stream(engname):
            def body(e):
                for op in ops:
                    if op.eng != engname:
                        continue
                    for k, v in op.waits:
                        e.wait_ge(semmap[k], v)
                    if op.fn is None:
                        continue
                    ins = op.fn(e)
                    if op.sig is not None:
                        ins.then_inc(semmap[op.sig[0]], 16 if op.dma else 1)
            return body

        block.tensor(stream("pe"))
        block.scalar(stream("act"))
        block.vector(stream("dve"))
        block.gpsimd(stream("pool"))
        block.sync(stream("sp"))


def _win_cols():
    o = {}
    idx = []
    base = {"gq": 0, "gk": 256, "gv": 512, "gz": 768, "ga": 1024, "dq": 1040, "dk": 1296, "dv": 1552,
            "dz": 1808, "db": 2064, "da": 2068, "mq": 2072, "mk": 2584, "mv": 3096}

    def put(name, cols):
        o[name] = len(idx)
        idx.extend(cols)

    put("gq", range(base["gq"], base["gq"] + 256))
    put("gk", range(base["gk"], base["gk"] + 256))
    put("gv", range(base["gv"], base["gv"] + 256))
    put("ga", range(base["ga"], base["ga"] + 16))
    put("dq", range(base["dq"], base["dq"] + 256))
    put("dk", range(base["dk"], base["dk"] + 256))
    put("dv", range(base["dv"], base["dv"] + 256))
    put("dba", range(base["db"], base["db"] + 8))
    swap = lambda b: [b + h * 64 + (d + 32) % 64 for h in range(8) for d in range(64)]
    put("mq", range(base["mq"], base["mq"] + 512))
    put("mqs", swap(base["mq"]))
    put("mk", range(base["mk"], base["mk"] + 512))
    put("mks", swap(base["mk"]))
    put("z", list(range(base["gz"], base["gz"] + 256)) + list(range(base["dz"], base["dz"] + 256)))
    put("mv", range(base["mv"], base["mv"] + 512))
    return np.array(idx, dtype=np.int64), o


WIN_IDX, WOFF = _win_cols()
NCOL = len(WIN_IDX)


def build(S, depth, dbg=False):
    NT = S // 128
    NG = S // 512
    NB = S // 256
    nc = bass.Bass("TRN2", target_bir_lowering=False)
    sc_kind = "ExternalOutput" if dbg else "Internal"

    def din(name, shape, dt):
        return nc.dram_tensor(name, shape, dt, kind="ExternalInput").ap()

    def dscr(name, shape, dt):
        return nc.dram_tensor(name, shape, dt, kind=sc_kind).ap()

    x_in = din("x", [S, D], F32)
    win_d = din("win", [depth, D, NCOL], BF16)
    wga_d = din("wga", [depth, 33, 256], BF16)
    wo_d = din("wo", [depth, D, D], BF16)
    wg_d = din("wg", [depth, D, DFF], BF16)
    wu_d = din("wu", [depth, D, DFF], BF16)
    wd_d = din("wd", [depth, DFF, D], BF16)
    gains_d = din("gains", [depth, 4, D], F32)
    hn_d = din("hn", [depth, 2, 64], F32)
    conv_d = din("conv", [depth, 768, 4], F32)
    gsc_d = din("gsc", [depth, 2, 4], F32)
    c_identb = din("c_identb", [128, 128], BF16)
    c_identf = din("c_identf", [128, 128], F32)
    c_masku = din("c_masku", [128, 128], F32)
    c_negu = din("c_negu", [128, 128], F32)
    c_posl = din("c_posl", [128, 128], F32)
    c_triu = din("c_triu", [128, 128], F32)
    c_trisu = din("c_trisu", [128, 128], F32)
    c_bones = din("c_bones", [128, 128], F32)
    c_rope = din("c_rope", [4, 128, S], F32)
    c_onehot = din("c_onehot", [64, S], BF16)
    c_mb = din("c_mb", [128, 32 * 32], F32)
    c_cm = din("c_cm", [4, 128, 512], BF16)
    y_out = nc.dram_tensor("y", [S, D], F32, kind="ExternalOutput").ap()

    xs = dscr("xs", [S, D], F32)
    x1s = dscr("x1s", [S, D], F32)
    h2s = dscr("h2s", [S, D], BF16)
    glaT = dscr("glaT", [1024, S], F32)
    gdnT = dscr("gdnT", [768, S], BF16)
    gsct = dscr("gsct", [S, 24], F32)
    mqT = dscr("mqT", [512, S], BF16)
    mkT = dscr("mkT", [512, S], BF16)
    mksum = dscr("mksum", [512, 32], F32)
    mvs = dscr("mvs", [S, 512], BF16)
    zs = dscr("zs", [S, 512], F32)
    mix = dscr("mix", [S, D], BF16)

    S_ = Sched()
    uid = [0]
    ARENA_BASE = 16640
    ARENA_CAP = 226000
    arena = [ARENA_BASE]

    def sb(name, shape, dt):
        nbytes = int(np.prod(shape[1:])) * (4 if dt == F32 else 2)
        nbytes = (nbytes + 63) // 64 * 64
        off = arena[0]
        arena[0] += nbytes
        assert arena[0] <= ARENA_CAP, (name, arena[0])
        uid[0] += 1
        return nc.alloc_sbuf_tensor_at(f"{name}_{uid[0]}", shape, dt, offset=off)

    es = ExitStack()
    with es:
        PF = [es.enter_context(nc.psum_tensor(f"pf{i}", [128, 512], F32)) for i in range(6)]
        PB = [es.enter_context(nc.psum_tensor(f"pb{i}", [128, 1024], BF16)) for i in range(2)]

        def op(eng, meth, reads, writes, **kw):
            S_.add(eng, lambda e, m=meth, k=kw: getattr(e, m)(**k), reads=reads, writes=writes)

        def mmg(items, reads, writes):
            def f(e, items=items):
                n = len(items)
                for i, (o, l, r) in enumerate(items):
                    ins = e.matmul(out=o, lhsT=l, rhs=r, start=(i == 0), stop=(i == n - 1))
                return ins
            S_.add("pe", f, reads=reads, writes=writes)

        def trg(items, reads, writes):
            def f(e, items=items):
                for (o, i_, idn) in items:
                    ins = e.transpose(out=o, in_=i_, identity=idn)
                return ins
            S_.add("pe", f, reads=reads, writes=writes)

        def dma(q, out, in_, reads, writes, chan, slow=False):
            if slow:
                S_.add(q, lambda e, o=out, i=in_: e.dma_start(out=o, in_=i, allow_slow_non_contiguous=True),
                       reads=reads, writes=writes, dma=True, chan=chan)
            else:
                S_.add(q, lambda e, o=out, i=in_: e.dma_start(out=o, in_=i), reads=reads, writes=writes, dma=True, chan=chan)

        identb = sb("identb", [128, 128], BF16)
        identf = sb("identf", [128, 128], F32)
        epsc = sb("epsc", [128, 1], F32)
        onec = sb("onec", [128, 1], F32)
        dma("sp", identb[:], c_identb, [], ["identb"], "c0")
        dma("sp", identf[:], c_identf, [], ["identf"], "c1")
        op("dve", "memset", [], ["epsc"], ap=epsc[:], constant=1e-6)
        op("dve", "memset", [], ["onec"], ap=onec[:], constant=1.0)
        GLOBAL_TOP = arena[0]

        def rstd_from_ss(ss_ap, out_ap, n, rk, wk):
            op("act", "activation", rk + ["epsc"], wk, out=out_ap, in_=ss_ap, func=AF.Ln, scale=1.0 / n, bias=epsc[:ss_ap.shape[0], :])
            op("act", "activation", wk, wk, out=out_ap, in_=out_ap, func=AF.Exp, scale=-0.5)

        def phase_A(l, xsrc):
            arena[0] = GLOBAL_TOP
            win = sb("win", [128, 8, NCOL], BF16)
            wga = sb("wga", [33, 256], BF16)
            gpre = sb("gpre", [128, D], F32)
            convw = sb("convw", [128, 6, 4], F32)
            dtb = sb("dtb", [128, 4], F32)
            nA = sb("nA", [128, 4], F32)
            triu = sb("triu", [128, 128], F32)
            trisu = sb("trisu", [128, 128], F32)
            bones = sb("bones", [128, 128], F32)
            for c in range(8):
                dma("sp" if c % 2 == 0 else "pool", win[:, c, :], win_d[l, c * 128:(c + 1) * 128, :], [], [f"win{c}"], f"w{c}")
            WIN = [f"win{c}" for c in range(8)]
            dma("sp", wga[:], wga_d[l], [], ["wga"], "c2")
            dma("sp", gpre[:], gains_d[l, 0:1, :].broadcast_to([128, D]), [], ["gpre"], "c3")
            dma("sp", convw[:], conv_d[l].rearrange("(g p) k -> p g k", p=128), [], ["convw"], "c4")
            dma("sp", dtb[:], gsc_d[l, 1:2, :].broadcast_to([128, 4]), [], ["dtb"], "c5")
            dma("sp", nA[:], gsc_d[l, 0:1, :].broadcast_to([128, 4]), [], ["nA"], "c6")
            dma("sp", triu[:], c_triu, [], ["triu"], "c7")
            dma("sp", trisu[:], c_trisu, [], ["trisu"], "c8")
            dma("sp", bones[:], c_bones, [], ["bones"], "c9")
            op("act", "activation", ["nA"], ["nA"], out=nA[:], in_=nA[:], func=AF.Exp)
            op("dve", "tensor_scalar", ["nA"], ["nA"], out=nA[:], in0=nA[:], scalar1=-1.0, scalar2=None, op0=ALU.mult)

            xt = [sb(f"xt{i}", [128, D], F32) for i in range(2)]
            sq = sb("sq", [128, D], F32)
            hb = [sb(f"hb{i}", [128, D], BF16) for i in range(2)]
            ss = [sb(f"ss{i}", [128, 1], F32) for i in range(2)]
            rs = [sb(f"rs{i}", [128, 1], F32) for i in range(2)]
            hT = [sb(f"hT{i}", [128, 8, 512], BF16) for i in range(2)]
            gaT = sb("gaT", [33, 512], BF16)
            op("dve", "memset", [], ["gaT"], ap=gaT[:], constant=0.0)
            op("dve", "memset", ["gaT"], ["gaT"], ap=gaT[32:33, :], constant=1.0)
            stf = [sb(f"stf{i}", [128, 512], F32) for i in range(3)]
            stb = [sb(f"stb{i}", [128, 512], BF16) for i in range(3)]
            xc = [sb(f"xc{i}", [128, 3 + 512], F32) for i in range(6)]
            cy = [sb(f"cy{i}", [128, 512], F32) for i in range(6)]
            ce = [sb(f"ce{i}", [128, 512], F32) for i in range(6)]
            gst = [sb(f"gst{i}", [128, 512], BF16) for i in range(6)]
            rope_t = [sb(f"rope{i}", [128, 512], F32) for i in range(4)]
            r1 = [sb(f"r1{i}", [128, 512], F32) for i in range(4)]
            r2 = [sb(f"r2{i}", [128, 512], F32) for i in range(4)]
            ksum = sb("ksum", [128, 4, 32], F32)
            sc = [sb(f"sc{i}", [128, 24], F32) for i in range(2)]
            sct = [sb(f"sct{i}", [128, 8], F32) for i in range(2)]
            op("pool", "memset", [], ["ksum"], ap=ksum[:], constant=0.0)
            for i in range(6):
                op("pool", "memset", [], [f"xc{i}"], ap=xc[i][:, 0:3], constant=0.0)
            nst = [0, 0]
            pfi = [0]

            def nextpf():
                pfi[0] = (pfi[0] + 1) % 4
                return pfi[0], PF[pfi[0]], f"pf{pfi[0]}"

            def load_norm(G):
                hTb = hT[G % 2]
                for tt in range(4):
                    t = G * 4 + tt
                    b = t % 2
                    dma("sp", xt[b][:], xsrc[t * 128:(t + 1) * 128, :], [], [f"xt{b}"], f"xt{b}")
                    op("act", "activation", [f"xt{b}"], ["sq", f"ss{b}"], out=sq[:], in_=xt[b][:], func=AF.Square, accum_out=ss[b][:])
                    rstd_from_ss(ss[b][:], rs[b][:], D, [f"ss{b}"], [f"rs{b}"])
                    op("dve", "scalar_tensor_tensor", [f"xt{b}", f"rs{b}", "gpre"], [f"hb{b}"], out=hb[b][:], in0=xt[b][:],
                       scalar=rs[b][:], in1=gpre[:], op0=ALU.mult, op1=ALU.mult)
                    for half in range(2):
                        trg([(PB[half][:, c * 128:(c + 1) * 128], hb[b][:, (half * 4 + c) * 128:(half * 4 + c + 1) * 128], identb[:])
                             for c in range(4)], [f"hb{b}", "identb"], [f"pb{half}"])
                        dst = hTb[:, half * 4:(half + 1) * 4, tt * 128:(tt + 1) * 128]
                        src = PB[half][:, 0:512].rearrange("p (c t) -> p c t", c=4)
                        if half == 0:
                            op("act", "copy", [f"pb{half}"], [f"hT{G % 2}_{tt}"], out=dst, in_=src)
                        else:
                            op("dve", "tensor_copy", [f"pb{half}"], [f"hT{G % 2}_{tt}"], out=dst, in_=src)

            def fm(G, coff, ncols=128):
                i, p, pk = nextpf()
                hTb = hT[G % 2]
                mmg([(p[0:ncols, :], win[:, c, coff:coff + ncols], hTb[:, c, :]) for c in range(8)],
                    WIN + [f"hT{G % 2}_{tt}" for tt in range(4)], [pk])
                return p, pk

            def stage_f():
                nst[0] = (nst[0] + 1) % 3
                return stf[nst[0]], f"stf{nst[0]}"

            def stage_b():
                nst[1] = (nst[1] + 1) % 3
                return stb[nst[1]], f"stb{nst[1]}"

            for G in range(NG):
                g0 = G * 512
                load_norm(G)
                hk = [f"hT{G % 2}_{tt}" for tt in range(4)]
                for j in range(6):
                    p, pk = fm(G, WOFF["gq"] + j * 128)
                    st, sk = stage_f()
                    op("act", "copy", [pk], [sk], out=st[:], in_=p[:])
                    dma("act", glaT[j * 128:(j + 1) * 128, g0:g0 + 512], st[:], [sk], [], "o_" + sk)
                p, pk = fm(G, WOFF["ga"], 16)
                op("act", "copy", [pk], ["gaT"], out=gaT[0:16, :], in_=p[0:16, :])
                for j in range(2):
                    i, p2, pk2 = nextpf()
                    mmg([(p2[:], wga[:, j * 128:(j + 1) * 128], gaT[:, :])], ["wga", "gaT"], [pk2])
                    st, sk = stage_f()
                    op("act", "activation", [pk2], [sk], out=st[:], in_=p2[:], func=AF.Exp, scale=-1.0)
                    op("act", "activation", [sk, "onec"], [sk], out=st[:], in_=st[:], func=AF.Ln, bias=onec[:])
                    dma("act", glaT[768 + j * 128:768 + (j + 1) * 128, g0:g0 + 512], st[:], [sk], [], "o_" + sk)
                for tt in range(4):
                    t = G * 4 + tt
                    b = t % 2
                    i, p, pk = nextpf()
                    mmg([(p[:, 0:8], hT[G % 2][:, c, tt * 128:(tt + 1) * 128], win[:, c, WOFF["dba"]:WOFF["dba"] + 8]) for c in range(8)],
                        WIN + hk, [pk])
                    scb, sck, stt_, stk = sc[b], f"sc{b}", sct[b], f"sct{b}"
                    op("act", "activation", [pk], [stk], out=stt_[:, 0:4], in_=p[:, 0:4], func=AF.Exp, scale=-1.0)
                    op("dve", "tensor_scalar", [stk], [stk], out=stt_[:, 0:4], in0=stt_[:, 0:4], scalar1=1.0, scalar2=None, op0=ALU.add)
                    op("dve", "reciprocal", [stk], [sck], out=scb[:, 4:8], in_=stt_[:, 0:4])
                    op("dve", "tensor_tensor", [pk, "dtb"], [stk], out=stt_[:, 4:8], in0=p[:, 4:8], in1=dtb[:], op=ALU.add)
                    op("act", "activation", [stk], [stk], out=stt_[:, 4:8], in_=stt_[:, 4:8], func=AF.Exp)
                    op("act", "activation", [stk, "onec"], [stk], out=stt_[:, 4:8], in_=stt_[:, 4:8], func=AF.Ln, bias=onec[:])
                    op("dve", "tensor_tensor", [stk, "nA"], [stk], out=stt_[:, 4:8], in0=stt_[:, 4:8], in1=nA[:], op=ALU.mult)
                    i2, pc, pck = nextpf()
                    mmg([(pc[:, 0:4], triu[:], stt_[:, 4:8])], ["triu", stk], [pck])
                    op("act", "copy", [pck], [sck], out=scb[:, 0:4], in_=pc[:, 0:4])
                    op("act", "activation", [pck], [sck], out=scb[:, 8:12], in_=pc[:, 0:4], func=AF.Exp)
                    op("dve", "tensor_scalar", [pck], [sck], out=scb[:, 20:24], in0=pc[:, 0:4], scalar1=-1.0, scalar2=None, op0=ALU.mult)
                    i3, pr, prk = nextpf()
                    mmg([(pr[:, 0:4], trisu[:], stt_[:, 4:8])], ["trisu", stk], [prk])
                    op("act", "activation", [prk], [sck], out=scb[:, 16:20], in_=pr[:, 0:4], func=AF.Exp)
                    op("dve", "tensor_tensor", [sck], [sck], out=scb[:, 12:16], in0=scb[:, 4:8], in1=scb[:, 8:12], op=ALU.mult)
                    dma("sp", gsct[t * 128:(t + 1) * 128, :], scb[:], [sck], [], "o_" + sck)
                J6 = range(6)
                for j in J6:
                    p, pk = fm(G, WOFF["dq"] + j * 128)
                    op("act", "copy", [pk], [f"xc{j}"], out=xc[j][:, 3:515], in_=p[:])
                for j in J6:
                    op("dve", "tensor_scalar", [f"xc{j}", "convw"], [f"cy{j}"], out=cy[j][:], in0=xc[j][:, 3:515], scalar1=convw[:, j, 3:4], scalar2=None, op0=ALU.mult)
                for i in range(3):
                    for j in J6:
                        op("dve", "scalar_tensor_tensor", [f"xc{j}", "convw", f"cy{j}"], [f"cy{j}"], out=cy[j][:], in0=xc[j][:, i:i + 512],
                           scalar=convw[:, j, i:i + 1], in1=cy[j][:], op0=ALU.mult, op1=ALU.add)
                for j in J6:
                    op("pool", "tensor_copy", [f"xc{j}"], [f"xc{j}"], out=xc[j][:, 0:3], in_=xc[j][:, 512:515])
                    op("act", "activation", [f"cy{j}"], [f"ce{j}"], out=ce[j][:], in_=cy[j][:], func=AF.Exp, scale=-1.0)
                for j in J6:
                    op("pool", "tensor_scalar", [f"ce{j}"], [f"ce{j}"], out=ce[j][:], in0=ce[j][:], scalar1=1.0, scalar2=None, op0=ALU.add)
                for j in J6:
                    op("dve", "reciprocal", [f"ce{j}"], [f"ce{j}"], out=ce[j][:], in_=ce[j][:])
                for j in J6:
                    op("dve", "tensor_tensor", [f"ce{j}", f"cy{j}"], [f"cy{j}"], out=cy[j][:], in0=cy[j][:], in1=ce[j][:], op=ALU.mult)
                for j in range(4):
                    op("pool", "tensor_tensor", [f"cy{j}"], [f"ce{j}"], out=ce[j][:], in0=cy[j][:], in1=cy[j][:], op=ALU.mult)
                pns = {}
                for j in range(4):
                    i_, pn_, pnk = nextpf()
                    pns[j] = (pn_, pnk)
                    mmg([(pn_[:], bones[:], ce[j][:])], ["bones", f"ce{j}"], [pnk])
                    op("act", "activation", [pnk, "epsc"], [f"ce{j}"], out=ce[j][:], in_=pn_[:], func=AF.Ln, bias=epsc[:])
                for j in range(4):
                    op("act", "activation", [f"ce{j}"], [f"ce{j}"], out=ce[j][:], in_=ce[j][:], func=AF.Exp, scale=-0.5)
                for j in J6:
                    if j < 4:
                        op("dve", "scalar_tensor_tensor", [f"cy{j}", f"ce{j}"], [f"gst{j}"], out=gst[j][:], in0=cy[j][:], scalar=(0.125 if j < 2 else 1.0),
                           in1=ce[j][:], op0=ALU.mult, op1=ALU.mult)
                    else:
                        op("dve", "tensor_copy", [f"cy{j}"], [f"gst{j}"], out=gst[j][:], in_=cy[j][:])
                    dma("sp", gdnT[j * 128:(j + 1) * 128, g0:g0 + 512], gst[j][:], [f"gst{j}"], [], f"o_gst{j}")
                for tbl in range(4):
                    dma("sp", rope_t[tbl][:], c_rope[tbl, :, g0:g0 + 512], [], [f"rope{tbl}"], f"rope{tbl}")
                for isk in range(2):
                    for j in range(4):
                        p, pk = fm(G, WOFF["mk" if isk else "mq"] + j * 128)
                        op("dve", "tensor_tensor", [pk, f"rope{2 * isk}"], [f"r1{j}"], out=r1[j][:], in0=p[:], in1=rope_t[2 * isk][:], op=ALU.mult)
                        p2, pk2 = fm(G, WOFF["mks" if isk else "mqs"] + j * 128)
                        op("dve", "tensor_tensor", [pk2, f"rope{2 * isk + 1}"], [f"r2{j}"], out=r2[j][:], in0=p2[:], in1=rope_t[2 * isk + 1][:], op=ALU.mult)
                    for j in range(4):
                        op("pool", "tensor_tensor", [f"r1{j}", f"r2{j}"], [f"r1{j}"], out=r1[j][:], in0=r1[j][:], in1=r2[j][:], op=ALU.add)
                    for j in range(4):
                        st, sk = stage_b()
                        op("act", "copy", [f"r1{j}"], [sk], out=st[:], in_=r1[j][:])
                        dst = (mkT if isk else mqT)
                        dma("act", dst[j * 128:(j + 1) * 128, g0:g0 + 512], st[:], [sk], [], "o_" + sk)
                        if isk:
                            op("dve", "tensor_reduce", [f"r1{j}"], ["ksum"], out=ksum[:, j, 2 * G:2 * G + 2],
                               in_=r1[j][:].rearrange("p (n k) -> p n k", n=2), axis=AX.X, op=ALU.add)
                for tt in range(4):
                    t = G * 4 + tt
                    lhs = lambda c, tt=tt: hT[G % 2][:, c, tt * 128:(tt + 1) * 128]
                    i, p, pk = nextpf()
                    mmg([(p[:], lhs(c), win[:, c, WOFF["z"]:WOFF["z"] + 512]) for c in range(8)], WIN + hk, [pk])
                    st, sk = stage_f()
                    b = tt % 2
                    op("act", "activation", [pk], [f"ce{b}"], out=ce[b][:], in_=p[:], func=AF.Exp, scale=-1.0)
                    op("pool", "tensor_scalar", [f"ce{b}"], [f"ce{b}"], out=ce[b][:], in0=ce[b][:], scalar1=1.0, scalar2=None, op0=ALU.add)
                    op("dve", "reciprocal", [f"ce{b}"], [f"ce{b}"], out=ce[b][:], in_=ce[b][:])
                    op("dve", "tensor_tensor", [f"ce{b}", pk], [sk], out=st[:], in0=p[:], in1=ce[b][:], op=ALU.mult)
                    dma("sp", zs[t * 128:(t + 1) * 128, :], st[:], [sk], [], "o_" + sk)
                    i, p, pk = nextpf()
                    mmg([(p[:], lhs(c), win[:, c, WOFF["mv"]:WOFF["mv"] + 512]) for c in range(8)], WIN + hk, [pk])
                    st, sk = stage_b()
                    op("act", "copy", [pk], [sk], out=st[:], in_=p[:])
                    dma("act", mvs[t * 128:(t + 1) * 128, :], st[:], [sk], [], "o_" + sk)
            for j in range(4):
                dma("pool", mksum[j * 128:(j + 1) * 128, :], ksum[:, j, :], ["ksum"], [], "o_ksum")
            S_.barrier()

        def head_out_stage(t, O, Ok, col0, zcol0, normw, zt, ztk, tmp, tmpk, ms, msk, outb, outbk, chan):
            dma("sp", zt[:], zs[t * 128:(t + 1) * 128, zcol0:zcol0 + 256], [], [ztk], "zt" + chan)
            Okl = Ok if isinstance(Ok, list) else [Ok]
            op("act", "activation", Okl, [tmpk], out=tmp[:], in_=O[:], func=AF.Square)
            op("dve", "tensor_reduce", [tmpk], [msk], out=ms[:], in_=tmp[:].rearrange("p (h d) -> p h d", h=4), axis=AX.X, op=ALU.add)
            rstd_from_ss(ms[:], ms[:], 64, [msk], [msk])
            op("dve", "tensor_tensor", Okl + [msk], [tmpk], out=tmp[:].rearrange("p (h d) -> p h d", h=4),
               in0=O[:].rearrange("p (h d) -> p h d", h=4), in1=ms[:].unsqueeze(2).to_broadcast([128, 4, 64]), op=ALU.mult)
            op("pool", "tensor_tensor", [ztk, "normw"], [ztk], out=zt[:].rearrange("p (h d) -> p h d", h=4),
               in0=zt[:].rearrange("p (h d) -> p h d", h=4), in1=normw[:].unsqueeze(1).to_broadcast([128, 4, 64]), op=ALU.mult)
            op("dve", "tensor_tensor", [tmpk, ztk], [outbk], out=outb[:], in0=tmp[:], in1=zt[:], op=ALU.mult)
            dma("pool", mix[t * 128:(t + 1) * 128, col0:col0 + 256], outb[:], [outbk], [], "o_" + chan)

        def phase_GLA(l):
            arena[0] = GLOBAL_TOP
            masku = sb("masku", [128, 128], F32)
            normw = sb("normw", [128, 64], F32)
            dma("sp", masku[:], c_masku, [], ["masku"], "c2")
            dma("sp", normw[:], hn_d[l, 0:1, :].broadcast_to([128, 64]), [], ["normw"], "c3")
            inT = [[sb(f"gin{b}_{k}", [128, 2, 128], F32) for k in range(4)] for b in range(2)]
            ones = sb("ones", [128, 128], F32)
            op("dve", "memset", [], ["ones"], ap=ones[:], constant=1.0)
            cum = [sb(f"cum{g}", [128, 128], F32) for g in range(2)]
            nb = [sb(f"nb{g}", [128, 1], F32) for g in range(2)]
            E = [[sb(f"E{g}_{k}", [128, 128], F32) for k in range(3)] for g in range(2)]
            qt = [sb(f"qt{g}", [128, 128], BF16) for g in range(2)]
            kt = [sb(f"kt{g}", [128, 128], BF16) for g in range(2)]
            kh = [sb(f"kh{g}", [128, 128], BF16) for g in range(2)]
            vb = [sb(f"vb{g}", [128, 128], BF16) for g in range(2)]
            khT = [sb(f"khT{g}", [128, 128], BF16) for g in range(2)]
            vT = [sb(f"vT{g}", [128, 128], BF16) for g in range(2)]
            AT = [sb(f"AT{h}", [128, 128], BF16) for h in range(4)]
            St = [sb(f"St{g}", [128, 64], F32) for g in range(2)]
            Sb = [sb(f"Sb{g}", [128, 64], BF16) for g in range(2)]
            O = [sb(f"O{b}", [128, 256], F32) for b in range(2)]
            zt = [sb(f"zt{b}", [128, 256], F32) for b in range(2)]
            tmp = sb("tmp", [128, 256], F32)
            ms = sb("ms", [128, 4], F32)
            outb = [sb(f"outb{b}", [128, 256], BF16) for b in range(2)]
            for g in range(2):
                op("dve", "memset", [], [f"St{g}"], ap=St[g][:], constant=0.0)
                op("pool", "memset", [], [f"Sb{g}"], ap=Sb[g][:], constant=0.0)
            for t in range(NT):
                b = t % 2
                for k in range(4):
                    dma("sp", inT[b][k][:], glaT[k * 256:(k + 1) * 256, t * 128:(t + 1) * 128].rearrange("(g p) t -> p g t", p=128),
                        [], [f"gin{b}_{k}"], f"gin{b}_{k}")
                for g in range(2):
                    q_, k_, v_, sp_ = (inT[b][k][:, g, :] for k in range(4))
                    rk = [f"gin{b}_{k}" for k in range(4)]
                    ck = f"cum{g}"
                    op("dve", "tensor_tensor_scan", [rk[3], "ones"], [ck], out=cum[g][:], data0=ones[:], data1=sp_, initial=0.0,
                       op0=ALU.mult, op1=ALU.add)
                    op("dve", "tensor_scalar", [ck], [f"nb{g}"], out=nb[g][:], in0=cum[g][:, 127:128], scalar1=-1.0 / 16, scalar2=None, op0=ALU.mult)
                    op("act", "activation", [ck], [f"E{g}_0"], out=E[g][0][:], in_=cum[g][:], func=AF.Exp, scale=-1.0 / 16)
                    op("act", "activation", [ck], [f"E{g}_1"], out=E[g][1][:], in_=cum[g][:], func=AF.Exp, scale=1.0 / 16)
                    op("act", "activation", [ck, f"nb{g}"], [f"E{g}_2"], out=E[g][2][:], in_=cum[g][:], func=AF.Exp, scale=1.0 / 16, bias=nb[g][:])
                    op("dve", "scalar_tensor_tensor", [rk[0], f"E{g}_0"], [f"qt{g}"], out=qt[g][:], in0=q_, scalar=0.125, in1=E[g][0][:],
                       op0=ALU.mult, op1=ALU.mult)
                    op("dve", "tensor_tensor", [rk[1], f"E{g}_1"], [f"kt{g}"], out=kt[g][:], in0=k_, in1=E[g][1][:], op=ALU.mult)
                    op("pool", "tensor_tensor", [rk[1], f"E{g}_2"], [f"khT{g}"], out=khT[g][:], in0=k_, in1=E[g][2][:], op=ALU.mult)
                    op("pool", "tensor_copy", [rk[2]], [f"vT{g}"], out=vT[g][:], in_=v_)
                    trg([(PB[0][:, 0:128], khT[g][:], identb[:]), (PB[0][:, 128:256], vT[g][:], identb[:])],
                        [f"khT{g}", f"vT{g}", "identb"], ["pb0"])
                    op("act", "copy", ["pb0"], [f"kh{g}"], out=kh[g][:], in_=PB[0][:, 0:128])
                    op("act", "copy", ["pb0"], [f"vb{g}"], out=vb[g][:], in_=PB[0][:, 128:256])
                    for hh in range(2):
                        h = 2 * g + hh
                        r = slice(hh * 64, hh * 64 + 64)
                        pa, pak = PF[hh], f"pf{hh}"
                        mmg([(pa[:, 0:128], kt[g][r, :], qt[g][r, :])], [f"kt{g}", f"qt{g}"], [pak])
                        op("dve", "tensor_tensor", [pak, "masku"], [f"AT{h}"], out=AT[h][:], in0=pa[:, 0:128], in1=masku[:], op=ALU.mult)
                        mmg([(PF[2][:, h * 64:(h + 1) * 64], qt[g][r, :], Sb[g][r, :]),
                             (PF[2][:, h * 64:(h + 1) * 64], AT[h][:], vb[g][:, r])],
                            [f"qt{g}", f"Sb{g}", f"AT{h}", f"vb{g}"], [f"pf2_{h}"])
                        mmg([(PF[3][r, g * 64:(g + 1) * 64], kh[g][:, r], vb[g][:, r])], [f"kh{g}", f"vb{g}"], [f"pf3_{g}_{hh}"])
                    op("dve", "scalar_tensor_tensor", [f"St{g}", f"E{g}_0", f"pf3_{g}_0", f"pf3_{g}_1"], [f"St{g}"], out=St[g][:], in0=St[g][:],
                       scalar=E[g][0][:, 127:128], in1=PF[3][:, g * 64:(g + 1) * 64], op0=ALU.mult, op1=ALU.add)
                    op("act", "copy", [f"St{g}"], [f"Sb{g}"], out=Sb[g][:], in_=St[g][:])
                op("act", "copy", [f"pf2_{h}" for h in range(4)], [f"O{b}"], out=O[b][:], in_=PF[2][:, 0:256])
                head_out_stage(t, O[b], f"O{b}", 0, 0, normw, zt[b], f"zt{b}", tmp, "tmp", ms, "ms", outb[b], f"outb{b}", f"gla{b}")
            S_.barrier()

        def phase_GDN(l):
            arena[0] = GLOBAL_TOP
            negu = sb("negu", [128, 128], F32)
            posl = sb("posl", [128, 128], F32)
            normw = sb("normw", [128, 64], F32)
            onesr = sb("onesr", [1, 128], F32)
            dma("sp", negu[:], c_negu, [], ["negu"], "c2")
            dma("sp", posl[:], c_posl, [], ["posl"], "c3")
            dma("sp", normw[:], hn_d[l, 1:2, :].broadcast_to([128, 64]), [], ["normw"], "c4")
            op("dve", "memset", [], ["onesr"], ap=onesr[:], constant=1.0)
            egl = [sb(f"egl{g}", [128, NT], F32) for g in range(2)]
            for g in range(2):
                for hh in range(2):
                    h = 2 * g + hh
                    src = gsct.rearrange("(t p) c -> p t c", p=128)[127:128, :, 8 + h]
                    dma("sp", egl[g][hh * 64:(hh + 1) * 64, :], src.broadcast_to([64, NT]), [], [f"egl{g}"], f"egl{g}{hh}", slow=True)
            inT = [sb(f"din{b}", [128, 6, 128], BF16) for b in range(2)]
            SC = [sb(f"SC{b}", [128, 24], F32) for b in range(2)]
            kbe = [sb(f"kbe{h}", [128, 64], BF16) for h in range(4)]
            khat = [sb(f"khat{h}", [128, 64], BF16) for h in range(4)]
            vbt = [sb(f"vbt{h}", [128, 64], BF16) for h in range(4)]
            gcr = [sb(f"gcr{h}", [1, 128], F32) for h in range(4)]
            DT = [sb(f"DT{h}", [128, 128], F32) for h in range(4)]
            DL = [sb(f"DL{h}", [128, 128], F32) for h in range(4)]
            attnT = [sb(f"attnT{h}", [128, 128], BF16) for h in range(4)]
            Am = [[sb(f"Am{h}_{i}", [128, 128], BF16) for i in range(2)] for h in range(4)]
            Cm = [[sb(f"Cm{h}_{i}", [128, 128], BF16) for i in range(2)] for h in range(4)]
            Ym = [[sb(f"Ym{h}_{i}", [128, 128], BF16) for i in range(2)] for h in range(4)]
            u = [sb(f"u{h}", [128, 64], F32) for h in range(4)]
            wT = [sb(f"wT{g}", [128, 128], BF16) for g in range(2)]
            vn = [sb(f"vn{h}", [128, 64], BF16) for h in range(4)]
            o1 = [sb(f"o1{h}", [128, 64], F32) for h in range(4)]
            St = [sb(f"St{g}", [128, 64], F32) for g in range(2)]
            Sb = [sb(f"Sb{g}", [128, 64], BF16) for g in range(2)]
            O = [sb(f"O{b}", [128, 256], F32) for b in range(2)]
            zt = [sb(f"zt{b}", [128, 256], F32) for b in range(2)]
            tmp = sb("tmp", [128, 256], F32)
            ms = sb("ms", [128, 4], F32)
            outb = [sb(f"outb{b}", [128, 256], BF16) for b in range(2)]
            for g in range(2):
                op("dve", "memset", [], [f"St{g}"], ap=St[g][:], constant=0.0)
                op("pool", "memset", [], [f"Sb{g}"], ap=Sb[g][:], constant=0.0)
            pfi = [0]

            def npf():
                pfi[0] = (pfi[0] + 1) % 6
                return PF[pfi[0]], f"pf{pfi[0]}"

            for t in range(NT):
                b = t % 2
                dma("sp", inT[b][:], gdnT[:, t * 128:(t + 1) * 128].rearrange("(g p) t -> p g t", p=128), [], [f"din{b}"], f"din{b}")
                dma("sp", SC[b][:], gsct[t * 128:(t + 1) * 128, :], [], [f"SC{b}"], f"SC{b}")
                ink, sck = f"din{b}", f"SC{b}"
                col = lambda q, h: SC[b][:, 4 * q + h:4 * q + h + 1]

                def qT(h): return inT[b][(h % 2) * 64:(h % 2) * 64 + 64, 0 + h // 2, :]
                def kT(h): return inT[b][(h % 2) * 64:(h % 2) * 64 + 64, 2 + h // 2, :]
                def vT(h): return inT[b][(h % 2) * 64:(h % 2) * 64 + 64, 4 + h // 2, :]

                for h in range(4):
                    hh = h % 2
                    pbk = f"pb{hh}"
                    trg([(PB[hh][:, 0:64], kT(h), identb[hh * 64:hh * 64 + 64, hh * 64:hh * 64 + 64]),
                         (PB[hh][:, 64:128], vT(h), identb[hh * 64:hh * 64 + 64, hh * 64:hh * 64 + 64])], [ink, "identb"], [pbk])
                    op("dve", "tensor_scalar", [pbk, sck], [f"kbe{h}"], out=kbe[h][:], in0=PB[hh][:, 0:64], scalar1=col(3, h), scalar2=None, op0=ALU.mult)
                    op("act", "activation", [pbk, sck], [f"khat{h}"], out=khat[h][:], in_=PB[hh][:, 0:64], func=AF.Copy, scale=col(4, h))
                    op("act", "activation", [pbk, sck], [f"vbt{h}"], out=vbt[h][:], in_=PB[hh][:, 64:128], func=AF.Copy, scale=col(1, h))
                    p, pk = npf()
                    mmg([(p[0:1, 0:128], col(0, h), identf[:])], [sck, "identf"], [pk])
                    op("act", "copy", [pk], [f"gcr{h}"], out=gcr[h][:], in_=p[0:1, 0:128])
                    p, pk = npf()
                    mmg([(p[:, 0:128], onesr[:], gcr[h][:]), (p[:, 0:128], identf[:], negu[:])], ["onesr", f"gcr{h}", "identf", "negu"], [pk])
                    op("act", "activation", [pk, sck], [f"DT{h}"], out=DT[h][:], in_=p[:, 0:128], func=AF.Exp, bias=col(5, h))
                    p, pk = npf()
                    mmg([(p[:, 0:128], onesr[:], gcr[h][:]), (p[:, 0:128], identf[:], posl[:])], ["onesr", f"gcr{h}", "identf", "posl"], [pk])
                    op("act", "activation", [pk, sck], [f"DL{h}"], out=DL[h][:], in_=p[:, 0:128], func=AF.Exp, scale=-1.0, bias=col(0, h))
                    p, pk = npf()
                    mmg([(p[:, 0:128], kT(h), qT(h))], [ink], [pk])
                    op("dve", "tensor_tensor", [pk, f"DT{h}"], [f"attnT{h}"], out=attnT[h][:], in0=p[:, 0:128], in1=DT[h][:], op=ALU.mult)
                    p, pk = npf()
                    mmg([(p[:, 0:128], kT(h), kT(h))], [ink], [pk])
                    op("dve", "scalar_tensor_tensor", [pk, sck, f"DL{h}"], [f"Cm{h}_0"], out=Cm[h][0][:], in0=p[:, 0:128], scalar=col(1, h),
                       in1=DL[h][:], op0=ALU.mult, op1=ALU.mult)
                    trg([(PB[hh][:, 128:256], Cm[h][0][:], identb[:])], [f"Cm{h}_0", "identb"], [pbk + "b"])
                    op("act", "copy", [pbk + "b"], [f"Am{h}_0"], out=Am[h][0][:], in_=PB[hh][:, 128:256])
                    op("pool", "tensor_tensor", [f"Am{h}_0", "identb"], [f"Ym{h}_0"], out=Ym[h][0][:], in0=identb[:], in1=Am[h][0][:], op=ALU.subtract)
                for k in range(1, 7):
                    a0, a1 = (k - 1) % 2, k % 2
                    for h in range(4):
                        p, pk = npf()
                        mmg([(p[:, 0:128], Am[h][a0][:], Cm[h][a0][:])], [f"Am{h}_{a0}", f"Cm{h}_{a0}"], [pk])
                        op("act" if h % 2 == 0 else "dve", "copy" if h % 2 == 0 else "tensor_copy", [pk], [f"Cm{h}_{a1}"], out=Cm[h][a1][:], in_=p[:, 0:128])
                        if k < 6:
                            p2, pk2 = npf()
                            mmg([(p2[:, 0:128], Cm[h][a0][:], Am[h][a0][:])], [f"Am{h}_{a0}", f"Cm{h}_{a0}"], [pk2])
                            op("dve" if h % 2 == 0 else "act", "tensor_copy" if h % 2 == 0 else "copy", [pk2], [f"Am{h}_{a1}"], out=Am[h][a1][:], in_=p2[:, 0:128])
                    for h in range(4):
                        p, pk = npf()
                        mmg([(p[:, 0:128], identb[:], Ym[h][a0][:]), (p[:, 0:128], Cm[h][a1][:], Ym[h][a0][:])],
                            ["identb", f"Ym{h}_{a0}", f"Cm{h}_{a1}"], [pk])
                        op("act" if h % 2 == 0 else "dve", "copy" if h % 2 == 0 else "tensor_copy", [pk], [f"Ym{h}_{a1}"], out=Ym[h][a1][:], in_=p[:, 0:128])
                YF = 0
                for h in range(4):
                    g, hh = h // 2, h % 2
                    r = slice(hh * 64, hh * 64 + 64)
                    p, pk = npf()
                    mmg([(p[:, 0:64], Ym[h][YF][:], vbt[h][:])], [f"Ym{h}_{YF}", f"vbt{h}"], [pk])
                    op("act", "copy", [pk], [f"u{h}"], out=u[h][:], in_=p[:, 0:64])
                    p, pk = npf()
                    mmg([(p[r, 0:128], kbe[h][:], Ym[h][YF][:])], [f"Ym{h}_{YF}", f"kbe{h}"], [pk])
                    op("act", "copy", [pk], [f"wT{g}_{hh}"], out=wT[g][r, :], in_=p[r, 0:128])
                for h in range(4):
                    g, hh = h // 2, h % 2
                    r = slice(hh * 64, hh * 64 + 64)
                    p, pk = npf()
                    mmg([(p[:, 0:64], wT[g][r, :], Sb[g][r, :])], [f"wT{g}_{hh}", f"Sb{g}"], [pk])
                    op("dve", "tensor_tensor", [f"u{h}", pk], [f"vn{h}"], out=vn[h][:], in0=u[h][:], in1=p[:, 0:64], op=ALU.subtract)
                    p, pk = npf()
                    mmg([(p[:, 0:64], qT(h), Sb[g][r, :])], [ink, f"Sb{g}"], [pk])
                    op("act", "activation", [pk, sck], [f"o1{h}"], out=o1[h][:], in_=p[:, 0:64], func=AF.Copy, scale=col(2, h))
                    p, pk = npf()
                    mmg([(p[:, 0:64], attnT[h][:], vn[h][:])], [f"attnT{h}", f"vn{h}"], [pk])
                    op("dve", "tensor_tensor", [f"o1{h}", pk], [f"O{b}_{h}"], out=O[b][:, h * 64:(h + 1) * 64], in0=o1[h][:], in1=p[:, 0:64], op=ALU.add)
                for g in range(2):
                    p, pk = npf()
                    for hh in range(2):
                        h = 2 * g + hh
                        r = slice(hh * 64, hh * 64 + 64)
                        mmg([(p[r, 0:64], khat[h][:], vn[h][:])], [f"khat{h}", f"vn{h}"], [pk])
                    op("dve", "scalar_tensor_tensor", [f"St{g}", f"egl{g}", pk], [f"St{g}"], out=St[g][:], in0=St[g][:],
                       scalar=egl[g][:, t:t + 1], in1=p[:, 0:64], op0=ALU.mult, op1=ALU.add)
                    op("act", "copy", [f"St{g}"], [f"Sb{g}"], out=Sb[g][:], in_=St[g][:])
                head_out_stage(t, O[b], [f"O{b}_{h}" for h in range(4)], 256, 256, normw, zt[b], f"zt{b}", tmp, "tmp", ms, "ms", outb[b], f"outb{b}", f"gdn{b}")
            S_.barrier()


        def phase_MOBA(l):
            arena[0] = GLOBAL_TOP
            qa = [sb(f"qa{i}", [128, S], BF16) for i in range(2)]
            ka = [sb(f"ka{i}", [128, S], BF16) for i in range(2)]
            Va = [sb(f"Va{i}", [128, NT, 65], BF16) for i in range(2)]
            ksf = [sb(f"ksf{i}", [64, 32], F32) for i in range(2)]
            ksb = [sb(f"ksb{i}", [64, 32], BF16) for i in range(2)]
            mb = sb("mb", [128, 32 * 32], F32)
            cm = sb("cm", [128, 4, 512], BF16)
            dma("sp", mb[:], c_mb, [], ["mb"], "c2")
            dma("sp", cm[:], c_cm.rearrange("r p q -> p r q"), [], ["cm"], "c3")
            for i in range(2):
                dma("sp", ka[i][64:128, :], c_onehot, [], [f"ka{i}_oh"], f"c4{i}")
                dma("sp", qa[i][96:128, :], c_onehot[32:64, :], [], [f"qa{i}_z"], f"c5{i}")
                op("pool", "memset", [], [f"Va{i}_1"], ap=Va[i][:, :, 64:65], constant=1.0)
            gm = [sb(f"gm{i}", [128, 32], F32) for i in range(2)]
            top8 = [sb(f"top8{i}", [128, 8], F32) for i in range(2)]
            bia = [sb(f"bia{i}", [128, 32], BF16) for i in range(2)]
            NPT = 7
            Pt = [sb(f"Pt{i}", [128, 512], BF16) for i in range(NPT)]
            osb = [sb(f"osb{i}", [128, 512], F32) for i in range(2)]
            for i in range(2):
                op("pool", "memset", [], [f"osb{i}"], ap=osb[i][:], constant=0.0)
            rden = [sb(f"rden{i}", [128, 1], F32) for i in range(2)]
            ob = [sb(f"ob{i}", [128, 64], BF16) for i in range(2)]
            of = [sb(f"of{i}", [128, 65], F32) for i in range(2)]
            def load_head(h):
                i = h % 2
                dma("sp", qa[i][0:64, :], mqT[h * 64:(h + 1) * 64, :], [], [f"qa{i}"], f"qa{i}")
                dma("sp", ka[i][0:64, :], mkT[h * 64:(h + 1) * 64, :], [], [f"ka{i}"], f"ka{i}")
                dma("sp", Va[i][:, :, 0:64], mvs[:, h * 64:(h + 1) * 64].rearrange("(t p) d -> p t d", p=128), [], [f"Va{i}"], f"Va{i}")
                dma("sp", ksf[i][:], mksum[h * 64:(h + 1) * 64, :], [], [f"ksf{i}"], f"ksf{i}")
                op("dve", "tensor_copy", [f"ksf{i}"], [f"ksb{i}"], out=ksb[i][:], in_=ksf[i][:])

            def gate_tile(h, t):
                i = h % 2
                j = t % 2
                own = t // 2
                mmg([(PF[4][:, j * 32:(j + 1) * 32], qa[i][0:64, t * 128:(t + 1) * 128], ksb[i][:, :])], [f"qa{i}", f"ksb{i}"], [f"pf4_{j}"])
                op("dve", "tensor_tensor", [f"pf4_{j}", "mb"], [f"gm{j}"], out=gm[j][:], in0=PF[4][:, j * 32:(j + 1) * 32],
                   in1=mb[:, own * 32:(own + 1) * 32], op=ALU.add)
                op("dve", "max", [f"gm{j}"], [f"top8{j}"], out=top8[j][:], in_=gm[j][:])
                op("dve", "tensor_scalar", [f"top8{j}"], [f"top8{j}"], out=top8[j][:, 3:4], in0=top8[j][:, 3:4], scalar1=-BIG / 2, scalar2=None, op0=ALU.max)
                op("dve", "tensor_scalar", [f"gm{j}", f"top8{j}"], [f"gm{j}"], out=gm[j][:], in0=gm[j][:], scalar1=top8[j][:, 3:4], scalar2=BIG,
                   op0=ALU.is_ge, op1=ALU.mult)
                op("dve", "tensor_scalar", [f"gm{j}"], [f"bia{j}"], out=bia[j][:], in0=gm[j][:], scalar1=-BIG, scalar2=None, op0=ALU.add)
                trg([(PB[0][64:96, 0:128], bia[j][:], identb[:])], [f"bia{j}", "identb"], ["pb0"])
                op("act", "copy", ["pb0"], [f"qa{i}_b{t}"], out=qa[i][64:96, t * 128:(t + 1) * 128], in_=PB[0][64:96, 0:128])

            def out_copy(G):
                oi = G % 2
                op("act", "copy", ["pf2"], [f"osb{oi}"], out=osb[oi][0:65, :], in_=PF[2][0:65, :])

            def out_stage(h, G):
                oi = G % 2
                for tt in range(4):
                    t = G * 4 + tt
                    j = tt % 2
                    mmg([(PF[4][:, 128:256], osb[oi][:, tt * 128:(tt + 1) * 128], identf[:])], [f"osb{oi}", "identf"], ["pf4"])
                    op("dve", "tensor_copy", ["pf4"], [f"of{j}"], out=of[j][:], in_=PF[4][:, 128:193])
                    op("dve", "reciprocal", [f"of{j}"], [f"rden{j}"], out=rden[j][:], in_=of[j][:, 64:65])
                    op("dve", "tensor_scalar", [f"of{j}", f"rden{j}"], [f"ob{j}"], out=ob[j][:], in0=of[j][:, 0:64],
                       scalar1=rden[j][:], scalar2=None, op0=ALU.mult)
                    dma("sp", mix[t * 128:(t + 1) * 128, 512 + h * 64:512 + (h + 1) * 64], ob[j][:], [f"ob{j}"], [], f"o_ob{j}")

            steps = [(G, kt_) for G in range(NG) for kt_ in range(4 * G + 4)]
            NH = 8
            STB = [(PF[0], "pf0"), (PF[1], "pf1"), (PF[5], "pf5"), (PF[3], "pf3")]
            try:
                pb1f = PB[1][:].bitcast(F32)
                assert tuple(pb1f.shape) == (128, 512)
                STB.append((pb1f, "pb1"))
            except Exception as ex:
                print("bitcast unavailable", ex)
            NSB = len(STB)
            load_head(0)
            for t in range(NT):
                gate_tile(0, t)
            for h in range(NH):
                i = h % 2
                pend_gate = list(range(NT)) if h + 1 < NH else []
                if h + 1 < NH:
                    load_head(h + 1)

                def emit_st(sidx, i=i):
                    G, kt_ = steps[sidx]
                    stp, stk = STB[sidx % NSB]
                    qk = [f"qa{i}", f"qa{i}_z"] + [f"qa{i}_b{t}" for t in range(G * 4, G * 4 + 4)]
                    mmg([(stp[:, 0:512], ka[i][:, kt_ * 128:(kt_ + 1) * 128], qa[i][:, G * 512:(G + 1) * 512])],
                        [f"ka{i}", f"ka{i}_oh"] + qk, [stk])

                for s0 in range(min(NSB - 1, len(steps))):
                    emit_st(s0)
                pend_out = []
                for sidx, (G, kt_) in enumerate(steps):
                    if sidx + NSB - 1 < len(steps):
                        emit_st(sidx + NSB - 1)
                    stp, stk = STB[sidx % NSB]
                    pti = sidx % NPT
                    oi = G % 2
                    nk = 4 * G + 4
                    op("act", "activation", [stk], [f"Pt{pti}"], out=Pt[pti][:], in_=stp[:, 0:512], func=AF.Exp)
                    if kt_ >= 4 * G:
                        op("pool", "tensor_tensor", [f"Pt{pti}", "cm"], [f"Pt{pti}"], out=Pt[pti][:], in0=Pt[pti][:], in1=cm[:, kt_ - 4 * G, :], op=ALU.mult)

                    def f(e, o=PF[2][0:65, :], l_=Va[i][:, kt_, :], r_=Pt[pti][:], st=(kt_ == 0), sp=(kt_ == nk - 1)):
                        return e.matmul(out=o, lhsT=l_, rhs=r_, start=st, stop=sp)
                    S_.add("pe", f, reads=[f"Va{i}", f"Va{i}_1", f"Pt{pti}"], writes=["pf2"])
                    for po in list(pend_out):
                        if sidx >= po[1]:
                            out_stage(h, po[0])
                            pend_out.remove(po)
                    if kt_ == nk - 1:
                        out_copy(G)
                        pend_out.append((G, sidx + 2))
                    if sidx % 8 == 4 and pend_gate:
                        gate_tile(h + 1, pend_gate.pop(0))
                for po in pend_out:
                    out_stage(h, po[0])
                while pend_gate:
                    gate_tile(h + 1, pend_gate.pop(0))
            S_.barrier()

        def phase_C1(l, xsrc):
            arena[0] = GLOBAL_TOP
            wo = sb("wo", [128, 8, D], BF16)
            gpost = sb("gpost", [128, D], F32)
            gpre2 = sb("gpre2", [128, D], F32)
            for c in range(8):
                dma("sp" if c % 2 == 0 else "pool", wo[:, c, :], wo_d[l, c * 128:(c + 1) * 128, :], [], [f"wo{c}"], f"w{c}")
            WO = [f"wo{c}" for c in range(8)]
            dma("sp", gpost[:], gains_d[l, 1:2, :].broadcast_to([128, D]), [], ["gpost"], "c2")
            dma("sp", gpre2[:], gains_d[l, 2:3, :].broadcast_to([128, D]), [], ["gpre2"], "c3")
            mt = [sb(f"mt{i}", [128, D], BF16) for i in range(2)]
            mT = [sb(f"mT{i}", [128, 8, 128], BF16) for i in range(2)]
            xt = [sb(f"xt{i}", [128, D], F32) for i in range(2)]
            yn = [sb(f"yn{i}", [128, D], F32) for i in range(2)]
            sq = sb("sq", [128, D], F32)
            ss = [sb(f"ss{i}", [128, 2], F32) for i in range(2)]
            rs = [sb(f"rs{i}", [128, 1], F32) for i in range(2)]
            hb = [sb(f"hb{i}", [128, D], BF16) for i in range(2)]
            for t in range(NT):
                b = t % 2
                rows = slice(t * 128, (t + 1) * 128)
                dma("sp", mt[b][:], mix[rows, :], [], [f"mt{b}"], f"mt{b}")
                dma("sp", xt[b][:], xsrc[rows, :], [], [f"xt{b}"], f"xt{b}")
                for half in range(2):
                    trg([(PB[half][:, c * 128:(c + 1) * 128], mt[b][:, (half * 4 + c) * 128:(half * 4 + c + 1) * 128], identb[:]) for c in range(4)],
                        [f"mt{b}", "identb"], [f"pb{half}"])
                    src = PB[half][:, 0:512].rearrange("p (c t) -> p c t", c=4)
                    if half == 0:
                        op("act", "copy", [f"pb{half}"], [f"mT{b}_{half}"], out=mT[b][:, 0:4, :], in_=src)
                    else:
                        op("dve", "tensor_copy", [f"pb{half}"], [f"mT{b}_{half}"], out=mT[b][:, 4:8, :], in_=src)
                for n in range(2):
                    pi = (2 * t + n) % 4
                    mmg([(PF[pi][:], mT[b][:, c, :], wo[:, c, n * 512:(n + 1) * 512]) for c in range(8)], WO + [f"mT{b}_0", f"mT{b}_1"], [f"pf{pi}"])
                    op("act", "activation", [f"pf{pi}"], ["sq", f"ss{b}_{n}"], out=sq[:, n * 512:(n + 1) * 512], in_=PF[pi][:], func=AF.Square,
                       accum_out=ss[b][:, n:n + 1])
                op("dve", "tensor_tensor", [f"ss{b}_0", f"ss{b}_1"], [f"rs{b}"], out=rs[b][:], in0=ss[b][:, 0:1], in1=ss[b][:, 1:2], op=ALU.add)
                rstd_from_ss(rs[b][:], rs[b][:], D, [f"rs{b}"], [f"rs{b}"])
                for n in range(2):
                    pi = (2 * t + n) % 4
                    op("dve", "scalar_tensor_tensor", [f"pf{pi}", f"rs{b}", "gpost"], [f"yn{b}_{n}"], out=yn[b][:, n * 512:(n + 1) * 512], in0=PF[pi][:],
                       scalar=rs[b][:], in1=gpost[:, n * 512:(n + 1) * 512], op0=ALU.mult, op1=ALU.mult)
                op("pool", "tensor_tensor", [f"yn{b}_0", f"yn{b}_1", f"xt{b}"], [f"yn{b}"], out=yn[b][:], in0=yn[b][:], in1=xt[b][:], op=ALU.add)
                dma("pool", x1s[rows, :], yn[b][:], [f"yn{b}"], [], f"o_yn{b}")
                op("act", "activation", [f"yn{b}"], ["sq", f"ss{b}_0"], out=sq[:], in_=yn[b][:], func=AF.Square, accum_out=ss[b][:, 0:1])
                rstd_from_ss(ss[b][:, 0:1], rs[b][:], D, [f"ss{b}_0"], [f"rs{b}"])
                op("dve", "scalar_tensor_tensor", [f"yn{b}", f"rs{b}", "gpre2"], [f"hb{b}"], out=hb[b][:], in0=yn[b][:], scalar=rs[b][:], in1=gpre2[:],
                   op0=ALU.mult, op1=ALU.mult)
                dma("pool", h2s[rows, :], hb[b][:], [f"hb{b}"], [], f"o_hb{b}")
            S_.barrier()

        def phase_C2(l, dst):
            arena[0] = GLOBAL_TOP
            wg = sb("wg", [128, 8, DFF], BF16)
            wu = sb("wu", [128, 8, DFF], BF16)
            wd = sb("wd", [128, NFC, D], BF16)
            gpost = sb("gpost", [128, D], F32)
            for c in range(8):
                dma("sp", wg[:, c, :], wg_d[l, c * 128:(c + 1) * 128, :], [], [f"wg{c}"], f"w{c}")
                dma("pool", wu[:, c, :], wu_d[l, c * 128:(c + 1) * 128, :], [], [f"wu{c}"], f"wu{c}")
            for f_ in range(NFC):
                dma("sp" if f_ % 2 else "pool", wd[:, f_, :], wd_d[l, f_ * 128:(f_ + 1) * 128, :], [], [f"wd{f_}"], f"wd{f_ % 4}")
            WG = [f"wg{c}" for c in range(8)]
            WU = [f"wu{c}" for c in range(8)]
            WD = [f"wd{f_}" for f_ in range(NFC)]
            dma("sp", gpost[:], gains_d[l, 3:4, :].broadcast_to([128, D]), [], ["gpost"], "c2")
            ht = [sb(f"ht{i}", [128, D], BF16) for i in range(2)]
            hT = sb("hT", [128, 8, 512], BF16)
            aT = sb("aT", [128, NFC, 512], BF16)
            th = [sb(f"th{i}", [128, 512], F32) for i in range(2)]
            us = [sb(f"us{i}", [128, 512], F32) for i in range(2)]
            xt = [sb(f"xt{i}", [128, D], F32) for i in range(2)]
            yn = [sb(f"yn{i}", [128, D], F32) for i in range(2)]
            sq = sb("sq", [128, 512], F32)
            ss = [sb(f"ss{i}", [128, 2], F32) for i in range(2)]
            rs = [sb(f"rs{i}", [128, 1], F32) for i in range(2)]
            for G in range(NG):
                for tt in range(4):
                    t = G * 4 + tt
                    b = t % 2
                    dma("sp", ht[b][:], h2s[t * 128:(t + 1) * 128, :], [], [f"ht{b}"], f"ht{b}")
                    for half in range(2):
                        trg([(PB[half][:, c * 128:(c + 1) * 128], ht[b][:, (half * 4 + c) * 128:(half * 4 + c + 1) * 128], identb[:]) for c in range(4)],
                            [f"ht{b}", "identb"], [f"pb{half}"])
                        src = PB[half][:, 0:512].rearrange("p (c t) -> p c t", c=4)
                        dst_ = hT[:, half * 4:(half + 1) * 4, tt * 128:(tt + 1) * 128]
                        if half == 0:
                            op("act", "copy", [f"pb{half}"], [f"hT_{tt}"], out=dst_, in_=src)
                        else:
                            op("dve", "tensor_copy", [f"pb{half}"], [f"hT_{tt}"], out=dst_, in_=src)
                hk = [f"hT_{tt}" for tt in range(4)]
                for f_ in range(NFC):
                    b = f_ % 2
                    pg, pu = PF[2 * b], PF[2 * b + 1]
                    mmg([(pg[:], wg[:, c, f_ * 128:(f_ + 1) * 128], hT[:, c, :]) for c in range(8)], WG + hk, [f"pf{2 * b}"])
                    mmg([(pu[:], wu[:, c, f_ * 128:(f_ + 1) * 128], hT[:, c, :]) for c in range(8)], WU + hk, [f"pf{2 * b + 1}"])
                    op("act", "activation", [f"pf{2 * b}"], [f"th{b}"], out=th[b][:], in_=pg[:], func=AF.Tanh, scale=0.5)
                    op("act", "copy", [f"pf{2 * b + 1}"], [f"us{b}"], out=us[b][:], in_=pu[:])
                    op("dve", "scalar_tensor_tensor", [f"th{b}", f"pf{2 * b}"], [f"th{b}"], out=th[b][:], in0=th[b][:], scalar=1.0, in1=pg[:],
                       op0=ALU.add, op1=ALU.mult)
                    op("dve", "scalar_tensor_tensor", [f"th{b}", f"us{b}"], [f"aT_{f_}"], out=aT[:, f_, :], in0=th[b][:], scalar=0.5, in1=us[b][:],
                       op0=ALU.mult, op1=ALU.mult)
                ak = [f"aT_{f_}" for f_ in range(NFC)]
                for tt in range(4):
                    t = G * 4 + tt
                    b = t % 2
                    rows = slice(t * 128, (t + 1) * 128)
                    dma("sp", xt[b][:], x1s[rows, :], [], [f"xt{b}"], f"xt{b}")
                    for n in range(2):
                        pi = 4 + n
                        mmg([(PF[pi][:], aT[:, f_, tt * 128:(tt + 1) * 128], wd[:, f_, n * 512:(n + 1) * 512]) for f_ in range(NFC)], WD + ak, [f"pf{pi}"])
                        op("act", "activation", [f"pf{pi}"], ["sq", f"ss{b}_{n}"], out=sq[:], in_=PF[pi][:], func=AF.Square, accum_out=ss[b][:, n:n + 1])
                    op("dve", "tensor_tensor", [f"ss{b}_0", f"ss{b}_1"], [f"rs{b}"], out=rs[b][:], in0=ss[b][:, 0:1], in1=ss[b][:, 1:2], op=ALU.add)
                    rstd_from_ss(rs[b][:], rs[b][:], D, [f"rs{b}"], [f"rs{b}"])
                    for n in range(2):
                        pi = 4 + n
                        op("dve", "scalar_tensor_tensor", [f"pf{pi}", f"rs{b}", "gpost"], [f"yn{b}_{n}"], out=yn[b][:, n * 512:(n + 1) * 512], in0=PF[pi][:],
                           scalar=rs[b][:], in1=gpost[:, n * 512:(n + 1) * 512], op0=ALU.mult, op1=ALU.mult)
                    op("pool", "tensor_tensor", [f"yn{b}_0", f"yn{b}_1", f"xt{b}"], [f"yn{b}"], out=yn[b][:], in0=yn[b][:], in1=xt[b][:], op=ALU.add)
                    dma("pool", dst[rows, :], yn[b][:], [f"yn{b}"], [], f"o_yn{b}")
            S_.barrier()

        for l in range(depth):
            xsrc = x_in if l == 0 else xs
            import os
            PH = os.environ.get("PHASES", "A,GLA,GDN,MOBA,C1,C2").split(",")
            if "A" in PH: phase_A(l, xsrc)
            if "GLA" in PH: phase_GLA(l)
            if "GDN" in PH: phase_GDN(l)
            if "MOBA" in PH: phase_MOBA(l)
            if "C1" in PH: phase_C1(l, xsrc)
            if "C2" in PH: phase_C2(l, y_out if l == depth - 1 else xs)

        semkeys = S_.emit()
        sems = [es.enter_context(nc.semaphore(f"s{i}")) for i in range(len(semkeys))]
        block = es.enter_context(nc.Block())
        S_.run(semkeys, sems, block)
    return nc


def _consts(S):
    j = np.arange(128)[:, None]
    i = np.arange(128)[None, :]
    c = {}
    c["c_identb"] = np.eye(128, dtype=np.float32).astype(BF)
    c["c_identf"] = np.eye(128, dtype=np.float32)
    c["c_masku"] = (j <= i).astype(np.float32)
    c["c_negu"] = np.where(j <= i, 0.0, -BIG).astype(np.float32)
    c["c_posl"] = np.where(i < j, 0.0, BIG).astype(np.float32)
    c["c_triu"] = (j <= i).astype(np.float32)
    c["c_trisu"] = (j > i).astype(np.float32)
    c["c_bones"] = ((j // 64) == (i // 64)).astype(np.float32)
    half = 32
    inv = (np.float32(10000.0) ** (-np.arange(half, dtype=np.float32) / np.float32(half))).astype(np.float32)
    pos = np.arange(S, dtype=np.float32)
    ang = (pos[:, None] * inv[None, :]).astype(np.float32)
    cos = np.cos(ang).astype(np.float32).T
    sin = np.sin(ang).astype(np.float32).T
    p = np.arange(128)
    cosP = cos[p % 32]
    sinP = sin[p % 32] * np.where((p % 64) < 32, -1.0, 1.0)[:, None]
    c["c_rope"] = np.stack([cosP * 0.125, sinP * 0.125, cosP, sinP]).astype(np.float32)
    c["c_onehot"] = (np.arange(64)[:, None] == (np.arange(S)[None, :] // 256)).astype(np.float32).astype(BF)
    own = np.arange(32)[:, None]
    jb = np.arange(32)[None, :]
    mbt = np.where(jb < own, 0.0, np.where(jb == own, BIG, -BIG)).astype(np.float32)
    c["c_mb"] = np.broadcast_to(mbt.reshape(1, 32 * 32), (128, 32 * 32)).copy()
    cm = np.zeros((4, 128, 512), np.float32)
    key = np.arange(128)[:, None]
    q = np.arange(512)[None, :]
    for r in range(4):
        cm[r] = ((r * 128 + key) <= q).astype(np.float32)
    c["c_cm"] = cm.astype(BF)
    return c


def _prep_weights(inp, depth):
    w = {}
    w["win"] = np.ascontiguousarray(inp["w_in"][:depth][:, :, WIN_IDX]).astype(BF)
    wga = np.zeros((depth, 33, 256), np.float32)
    wga[:, 0:16, :] = inp["gla_w_gate"][:depth]
    wga[:, 32, :] = inp["gla_b_gate"][:depth]
    w["wga"] = wga.astype(BF)
    w["wo"] = np.asarray(inp["w_o"][:depth]).astype(BF)
    w["wg"] = np.asarray(inp["ffn_w_gate"][:depth]).astype(BF)
    w["wu"] = np.asarray(inp["ffn_w_up"][:depth]).astype(BF)
    w["wd"] = np.asarray(inp["ffn_w_down"][:depth]).astype(BF)
    w["gains"] = np.stack([inp["norm_mix_pre"][:depth], inp["norm_mix_post"][:depth], inp["norm_ffn_pre"][:depth],
                           inp["norm_ffn_post"][:depth]], axis=1).astype(np.float32)
    w["hn"] = np.stack([inp["gla_norm"][:depth], inp["gdn_norm"][:depth]], axis=1).astype(np.float32)
    w["conv"] = np.ascontiguousarray(np.transpose(inp["gdn_conv"][:depth], (0, 2, 1))).astype(np.float32)
    w["gsc"] = np.stack([inp["gdn_a_log"][:depth], inp["gdn_dt_bias"][:depth]], axis=1).astype(np.float32)
    return w


def run(inputs, n_cores=8, dbg=False, depth=None):
    inp = {k: np.asarray(v) for k, v in inputs.items()}
    x = inp["x"]
    B, S, _ = x.shape
    depth = depth or inp["w_in"].shape[0]
    nc = build(S, depth, dbg=dbg)
    shared = {}
    shared.update(_consts(S))
    shared.update(_prep_weights(inp, depth))
    in_maps = []
    for c in range(n_cores):
        m = dict(shared)
        m["x"] = np.ascontiguousarray(x[c % B]).astype(np.float32)
        in_maps.append(m)
    res = run_bass_kernel_spmd(nc, in_maps, core_ids=list(range(n_cores)))
    return res


def kernel(**inputs):
    res = run(inputs)
    B = np.asarray(inputs["x"]).shape[0]
    out = np.stack([res.results[b]["y"] for b in range(B)], axis=0)
    return out.astype(np.float32)
```
